# Optimizing a Trainium2 kernel written in Bass

```python
import math
import jax, jax.numpy as jnp
from jax import lax
import numpy as np

D_MODEL = 1024
BATCH = 4
SEQ = 4096
DEPTH = 2

N_Q_HEADS = 8
N_KV_HEADS = 2
HEAD_DIM = 128
Q_GROUP = N_Q_HEADS // N_KV_HEADS
ATTN_WIDTH = N_Q_HEADS * HEAD_DIM
KV_WIDTH = N_KV_HEADS * HEAD_DIM
ROPE_AXIS_DIM = HEAD_DIM // 2
ROPE_THETA = 10000.0
GRID_W = 64
Q_BLOCK = 128

LRU_WIDTH = D_MODEL
LRU_BLOCKS = 8
LRU_BLOCK_W = LRU_WIDTH // LRU_BLOCKS
CONV_WIDTH = 4
CONV_PAD_LEFT = 2
CONV_PAD_RIGHT = 1
LRU_C = 8.0

N_BRANCHES = 2
IN_WIDTH = ATTN_WIDTH + 2 * KV_WIDTH + 2 * LRU_WIDTH + N_BRANCHES * D_MODEL

D_FF = 2816
N_EXPERTS = 8
TOP_K = 2
EXPERT_FF = 3584
N_DENSE = (DEPTH + 1) // 2
N_MOE = DEPTH // 2

N_MOD = 6
NORM_EPS = 1e-6

kernel_name = "hybrid_rglru_gqa_axialrope_moe_encoder"


def rms_norm(x, gain=None):
    xf = x.astype(jnp.float32)
    y = xf * lax.rsqrt(jnp.mean(xf * xf, axis=-1, keepdims=True) + NORM_EPS)
    if gain is not None:
        y = y * gain.astype(jnp.float32)
    return y.astype(x.dtype)


def modulate(x, shift, scale):
    return rms_norm(x) * (1 + scale) + shift


def axial_rope_angles(seq_len):
    rows = seq_len // GRID_W
    row = jnp.repeat(jnp.arange(rows, dtype=jnp.float32), GRID_W)
    col = jnp.tile(jnp.arange(GRID_W, dtype=jnp.float32), rows)
    n_freq = ROPE_AXIS_DIM // 2
    inv_freq = jnp.exp(-math.log(ROPE_THETA) * (2.0 * jnp.arange(n_freq, dtype=jnp.float32) / ROPE_AXIS_DIM))
    ang = jnp.stack([row[:, None] * inv_freq, col[:, None] * inv_freq], axis=1)
    return jnp.cos(ang), jnp.sin(ang)


def apply_axial_rope(x, cos, sin):
    xr = x.astype(jnp.float32).reshape(x.shape[:-1] + (2, 2, ROPE_AXIS_DIM // 2))
    x1 = xr[..., 0, :]
    x2 = xr[..., 1, :]
    c = cos[None, :, None]
    s = sin[None, :, None]
    out = jnp.stack([x1 * c - x2 * s, x2 * c + x1 * s], axis=-2)
    return out.reshape(x.shape).astype(x.dtype)


def gqa_attention(q, k, v):
    B, S = q.shape[0], q.shape[1]
    nqb = S // Q_BLOCK
    qb = q.reshape(B, nqb, Q_BLOCK, N_KV_HEADS, Q_GROUP, HEAD_DIM).transpose(1, 0, 2, 3, 4, 5)
    scale = HEAD_DIM ** -0.5

    def one_block(q_blk):
        s = jnp.einsum('bqkgd,bskd->bkgqs', q_blk, k, preferred_element_type=jnp.float32) * scale
        p = jax.nn.softmax(s, axis=-1)
        return jnp.einsum('bkgqs,bskd->bqkgd', p.astype(v.dtype), v)

    o = lax.map(one_block, qb)
    return o.transpose(1, 0, 2, 3, 4, 5).reshape(B, S, ATTN_WIDTH)


def centred_depthwise_conv(x, w, b):
    kern = w.astype(x.dtype)[:, None, :]
    y = lax.conv_general_dilated(x, kern, window_strides=(1,),
                                 padding=[(CONV_PAD_LEFT, CONV_PAD_RIGHT)],
                                 dimension_numbers=('NWC', 'WIO', 'NWC'),
                                 feature_group_count=x.shape[-1])
    return y + b.astype(x.dtype)


def block_diag_linear(x, w, b):
    B, S, _ = x.shape
    xb = x.reshape(B, S, LRU_BLOCKS, LRU_BLOCK_W)
    y = jnp.einsum('bsni,nij->bsnj', xb, w.astype(jnp.float32)).reshape(B, S, LRU_WIDTH)
    return y + b.astype(jnp.float32)


def rg_lru(x, w_a, b_a, w_x, b_x, lam, reverse):
    xf = x.astype(jnp.float32)
    r = jax.nn.sigmoid(block_diag_linear(xf, w_a, b_a))
    i = jax.nn.sigmoid(block_diag_linear(xf, w_x, b_x))
    log_a = -LRU_C * r * jax.nn.softplus(-lam.astype(jnp.float32))
    a = jnp.exp(log_a)
    u = jnp.sqrt(-jnp.expm1(2.0 * log_a)) * (i * xf)

    def step(h, au):
        a_t, u_t = au
        h = a_t * h + u_t
        return h, h

    h0 = jnp.zeros((x.shape[0], LRU_WIDTH), jnp.float32)
    _, hs = lax.scan(step, h0, (a.transpose(1, 0, 2), u.transpose(1, 0, 2)), reverse=reverse)
    return hs.transpose(1, 0, 2)


def swiglu(h, w_gate, w_up, w_down):
    return (jax.nn.silu(h @ w_gate) * (h @ w_up)) @ w_down


def moe_swiglu(h, router_w, router_b, w_gate, w_up, w_down):
    B, S, D = h.shape
    t = h.reshape(B * S, D)
    logits = (t @ router_w).astype(jnp.float32) + router_b.astype(jnp.float32)
    top_vals, top_idx = lax.top_k(logits, TOP_K)
    top_w = jax.nn.softmax(top_vals, axis=-1)
    combine = jnp.sum(jax.nn.one_hot(top_idx, N_EXPERTS, dtype=jnp.float32) * top_w[..., None], axis=1)
    out = jnp.zeros((B * S, D), jnp.float32)
    for e in range(N_EXPERTS):
        y = swiglu(t, w_gate[e], w_up[e], w_down[e])
        out = out + combine[:, e:e + 1] * y.astype(jnp.float32)
    return out.astype(h.dtype).reshape(B, S, D)


def hybrid_mixer(h, cos, sin, w_in, q_gain, k_gain, conv_w, conv_b,
                 lru_w_a, lru_b_a, lru_w_x, lru_b_x, lru_lambda,
                 w_o_attn, w_o_lru, w_out):
    B, S, _ = h.shape
    proj = h @ w_in
    cuts = np.cumsum([ATTN_WIDTH, KV_WIDTH, KV_WIDTH, LRU_WIDTH, LRU_WIDTH]).tolist()
    q, k, v, xr, xg, gates = jnp.split(proj, cuts, axis=-1)

    q = rms_norm(q.reshape(B, S, N_Q_HEADS, HEAD_DIM), q_gain)
    k = rms_norm(k.reshape(B, S, N_KV_HEADS, HEAD_DIM), k_gain)
    v = v.reshape(B, S, N_KV_HEADS, HEAD_DIM)
    q = apply_axial_rope(q, cos, sin)
    k = apply_axial_rope(k, cos, sin)
    y_attn = gqa_attention(q, k, v) @ w_o_attn

    xc = centred_depthwise_conv(xr, conv_w, conv_b)
    h_fwd = rg_lru(xc, lru_w_a[0], lru_b_a[0], lru_w_x[0], lru_b_x[0], lru_lambda[0], reverse=False)
    h_bwd = rg_lru(xc, lru_w_a[1], lru_b_a[1], lru_w_x[1], lru_b_x[1], lru_lambda[1], reverse=True)
    y_lru = ((h_fwd + h_bwd).astype(h.dtype) * jax.nn.gelu(xg, approximate=True)) @ w_o_lru

    g_attn, g_lru = jnp.split(jax.nn.sigmoid(gates), N_BRANCHES, axis=-1)
    merged = g_attn * y_attn + g_lru * y_lru
    return merged @ w_out


def setup_inputs(seed: int = 0) -> dict:
    key = jax.random.key(seed)
    ks = iter(jax.random.split(key, 40))
    f32 = jnp.float32

    def nrm(shape, fan_in, mult=1.0):
        return jax.random.normal(next(ks), shape, f32) * (mult * fan_in ** -0.5)

    def small(shape, s=0.01):
        return jax.random.normal(next(ks), shape, f32) * s

    def gain(shape):
        return 1.0 + jax.random.normal(next(ks), shape, f32) * 0.02

    x = jax.random.normal(next(ks), (BATCH, SEQ, D_MODEL), f32)
    c = jax.random.normal(next(ks), (BATCH, D_MODEL), f32)
    w_mod = nrm((DEPTH, D_MODEL, N_MOD * D_MODEL), D_MODEL, 0.5)
    b_mod = small((DEPTH, N_MOD * D_MODEL))
    w_in = nrm((DEPTH, D_MODEL, IN_WIDTH), D_MODEL)
    q_norm_gain = gain((DEPTH, HEAD_DIM))
    k_norm_gain = gain((DEPTH, HEAD_DIM))
    conv_w = nrm((DEPTH, CONV_WIDTH, LRU_WIDTH), CONV_WIDTH)
    conv_b = small((DEPTH, LRU_WIDTH))
    lru_w_a = nrm((DEPTH, 2, LRU_BLOCKS, LRU_BLOCK_W, LRU_BLOCK_W), LRU_BLOCK_W)
    lru_b_a = small((DEPTH, 2, LRU_WIDTH))
    lru_w_x = nrm((DEPTH, 2, LRU_BLOCKS, LRU_BLOCK_W, LRU_BLOCK_W), LRU_BLOCK_W)
    lru_b_x = small((DEPTH, 2, LRU_WIDTH))
    a0 = jax.random.uniform(next(ks), (DEPTH, 2, LRU_WIDTH), f32, 0.9, 0.999)
    s0 = a0 ** (1.0 / LRU_C)
    lru_lambda = jnp.log(s0) - jnp.log1p(-s0)
    w_o_attn = nrm((DEPTH, ATTN_WIDTH, D_MODEL), ATTN_WIDTH)
    w_o_lru = nrm((DEPTH, LRU_WIDTH, D_MODEL), LRU_WIDTH)
    w_out = nrm((DEPTH, D_MODEL, D_MODEL), D_MODEL)
    ffn_w_gate = nrm((N_DENSE, D_MODEL, D_FF), D_MODEL)
    ffn_w_up = nrm((N_DENSE, D_MODEL, D_FF), D_MODEL)
    ffn_w_down = nrm((N_DENSE, D_FF, D_MODEL), D_FF)
    router_w = nrm((N_MOE, D_MODEL, N_EXPERTS), D_MODEL)
    router_b = small((N_MOE, N_EXPERTS))
    moe_w_gate = nrm((N_MOE, N_EXPERTS, D_MODEL, EXPERT_FF), D_MODEL)
    moe_w_up = nrm((N_MOE, N_EXPERTS, D_MODEL, EXPERT_FF), D_MODEL)
    moe_w_down = nrm((N_MOE, N_EXPERTS, EXPERT_FF, D_MODEL), EXPERT_FF)
    final_gain = gain((D_MODEL,))
    return {"x": x, "c": c, "w_mod": w_mod, "b_mod": b_mod, "w_in": w_in,
            "q_norm_gain": q_norm_gain, "k_norm_gain": k_norm_gain,
            "conv_w": conv_w, "conv_b": conv_b,
            "lru_w_a": lru_w_a, "lru_b_a": lru_b_a, "lru_w_x": lru_w_x, "lru_b_x": lru_b_x,
            "lru_lambda": lru_lambda, "w_o_attn": w_o_attn, "w_o_lru": w_o_lru, "w_out": w_out,
            "ffn_w_gate": ffn_w_gate, "ffn_w_up": ffn_w_up, "ffn_w_down": ffn_w_down,
            "router_w": router_w, "router_b": router_b,
            "moe_w_gate": moe_w_gate, "moe_w_up": moe_w_up, "moe_w_down": moe_w_down,
            "final_gain": final_gain}


def reference(x, c, w_mod, b_mod, w_in, q_norm_gain, k_norm_gain, conv_w, conv_b,
              lru_w_a, lru_b_a, lru_w_x, lru_b_x, lru_lambda, w_o_attn, w_o_lru, w_out,
              ffn_w_gate, ffn_w_up, ffn_w_down, router_w, router_b,
              moe_w_gate, moe_w_up, moe_w_down, final_gain):
    cos, sin = axial_rope_angles(x.shape[1])
    c_act = jax.nn.silu(c)
    for l in range(DEPTH):
        mod = (c_act @ w_mod[l] + b_mod[l])[:, None, :]
        sh1, sc1, g1, sh2, sc2, g2 = jnp.split(mod, N_MOD, axis=-1)

        h = modulate(x, sh1, sc1)
        mix = hybrid_mixer(h, cos, sin, w_in[l], q_norm_gain[l], k_norm_gain[l],
                           conv_w[l], conv_b[l], lru_w_a[l], lru_b_a[l], lru_w_x[l], lru_b_x[l],
                           lru_lambda[l], w_o_attn[l], w_o_lru[l], w_out[l])
        x = x + g1 * mix

        h = modulate(x, sh2, sc2)
        if l % 2 == 0:
            j = l // 2
            ff = swiglu(h, ffn_w_gate[j], ffn_w_up[j], ffn_w_down[j])
        else:
            j = l // 2
            ff = moe_swiglu(h, router_w[j], router_b[j], moe_w_gate[j], moe_w_up[j], moe_w_down[j])
        x = x + g2 * ff
    return rms_norm(x, final_gain)
```

```python
import math
import os
_PARTS = os.environ.get('PA_PARTS', 'nkvrg')
import numpy as np
import concourse.bass as bass
import concourse.mybir as mybir
from concourse.bass_utils import run_bass_kernel_spmd
from contextlib import ExitStack

F32 = mybir.dt.float32
BF16 = mybir.dt.bfloat16
ALU = mybir.AluOpType
AF = mybir.ActivationFunctionType

ENGS = ("pe", "act", "dve", "pool", "sp")

T = 4096
D = 1024
G = 512
NGRP = T // G
DFF = 2816
NF0 = DFF // 128
EFF = 3584
NE = 8
IN_W = 5632
EPS = 1e-6
OQ, OK_, OV, OXR, OXG, OGA, OGL = 0, 1024, 1280, 1536, 2560, 3584, 4608

V_BMOD = 0
V_QG = 96
V_CONVW = 104
V_CONVB = 184
V_LRU = 200
V_FG = 296
V_RB = 304
V_C = 312
NV = 320


class _Rec:
    def __getattr__(self, name):
        def f(*a, **k):
            self.call = (name, a, k)
            return self
        return f


class FW:
    def __init__(self, nc, stack, n_dma_sems=48):
        self.nc = nc
        self.stack = stack
        self.sem = {k: stack.enter_context(nc.semaphore("s_" + k)) for k in ENGS}
        self.cnt = {k: 0 for k in ENGS}
        self.pending = {k: [] for k in ENGS}
        self.seen = {k: {} for k in ENGS}
        self.prog = {k: [] for k in ENGS}
        self.dsems = [stack.enter_context(nc.semaphore("d%d" % i)) for i in range(n_dma_sems)]
        self.dval = [0] * n_dma_sems
        self.n_hw = n_dma_sems - 20
        self.dnext_hw = 0
        self.dnext_sw = 0
        self.semobj = {}
        for k in ENGS:
            self.semobj["s_" + k] = self.sem[k]
        for i, s in enumerate(self.dsems):
            self.semobj["d%d" % i] = s
        self.lastw = {}
        self.readers = {}
        self.ninstr = 0

    def sb(self, st, name, shape, dt):
        self.nsb = getattr(self, "nsb", 0) + 1
        return st.enter_context(self.nc.sbuf_tensor("sb%d_%s" % (self.nsb, name), list(shape), dt))

    def _need(self, eng, reads, writes):
        need = {}

        def add(sv):
            if sv is None:
                return
            s, v = sv
            if eng == "pe" and s == "s_pe":
                return
            if need.get(s, 0) < v:
                need[s] = v
        for k in reads:
            add(self.lastw.get(k))
        for k in writes:
            add(self.lastw.get(k))
            for r in self.readers.get(k, ()):
                add(r)
        out = []
        seen = self.seen[eng]
        for s, v in need.items():
            if seen.get(s, 0) >= v:
                continue
            seen[s] = v
            out.append((s, v))
        return out

    def _emit_waits(self, eng, waits):
        for s, v in waits:
            so = self.semobj[s]
            self.prog[eng].append(lambda e, so=so, v=v: e.wait_ge(so, v))

    def op(self, eng, fn, reads=(), writes=(), inc=True):
        pr = [k for k in reads if k.startswith("pb")]
        if pr:
            reads = [k for k in reads if not k.startswith("pb")]
            writes = list(writes) + pr
        rec = _Rec()
        fn(rec)
        mname, margs, mkw = rec.call
        fn = lambda e, mname=mname, margs=margs, mkw=mkw: getattr(e, mname)(*margs, **mkw)
        waits = self._need(eng, reads, writes)
        self._emit_waits(eng, waits)
        self.pending[eng].append((tuple(reads), tuple(writes)))
        self.ninstr += 1
        if inc:
            self.cnt[eng] += 1
            v = self.cnt[eng]
            s = "s_" + eng
            so = self.sem[eng]
            self.prog[eng].append(lambda e, fn=fn, so=so: fn(e).then_inc(so, 1))
            for rd, wr in self.pending[eng]:
                for k in rd:
                    self.readers.setdefault(k, []).append((s, v))
                for k in wr:
                    self.lastw[k] = (s, v)
                    self.readers[k] = []
            self.pending[eng] = []
        else:
            self.prog[eng].append(lambda e, fn=fn: fn(e))

    def dma(self, out, in_, reads=(), writes=(), q="sp", **kw):
        if q == "pool":
            i = self.n_hw + self.dnext_sw
            self.dnext_sw = (self.dnext_sw + 1) % (len(self.dsems) - self.n_hw)
        else:
            i = self.dnext_hw
            self.dnext_hw = (self.dnext_hw + 1) % self.n_hw
        s = "d%d" % i
        waits = self._need(q, reads, writes)
        prev = self.dval[i]
        if prev > 0 and self.seen[q].get(s, 0) < prev:
            self.seen[q][s] = prev
            waits.append((s, prev))
        self._emit_waits(q, waits)
        self.dval[i] += 16
        v = self.dval[i]
        so = self.dsems[i]
        self.prog[q].append(
            lambda e, out=out, in_=in_, so=so, kw=kw: e.dma_start(out=out, in_=in_, **kw).then_inc(so, 16))
        self.ninstr += 1
        for k in reads:
            self.readers.setdefault(k, []).append((s, v))
        for k in writes:
            self.lastw[k] = (s, v)
            self.readers[k] = []

    def wait_all(self, eng, keys):
        self._emit_waits(eng, self._need(eng, keys, ()))

    def barrier(self):
        allv = [("s_" + k, self.cnt[k]) for k in ENGS if self.cnt[k] > 0]
        allv += [("d%d" % i, v) for i, v in enumerate(self.dval) if v > 0]
        for eng in ENGS:
            w = []
            for s, v in allv:
                if self.seen[eng].get(s, 0) < v:
                    self.seen[eng][s] = v
                    w.append((s, v))
            self._emit_waits(eng, w)

    def finish(self):
        nc = self.nc
        prog = self.prog
        with nc.Block() as block:
            @block.tensor
            def _(e):
                for f in prog["pe"]:
                    f(e)

            @block.scalar
            def _(e):
                for f in prog["act"]:
                    f(e)

            @block.vector
            def _(e):
                for f in prog["dve"]:
                    f(e)

            @block.gpsimd
            def _(e):
                for f in prog["pool"]:
                    f(e)

            @block.sync
            def _(e):
                for f in prog["sp"]:
                    f(e)


class _Stop(Exception):
    pass


def build_nc(n_layers=2, debug=False, stop_after=None):
    nc = bass.Bass("TRN2", target_bir_lowering=False)

    def ck(name):
        if stop_after == name:
            raise _Stop()

    def din(name, shape, dt=F32):
        return nc.dram_tensor(name, list(shape), dt, kind="ExternalInput").ap()

    x_in = din("x", [T, D])
    vecs_in = din("vecs", [128, NV])
    cos_in = din("cosT", [128, T])
    sin_in = din("sinT", [128, T])
    w_mod = din("w_mod", [2, D, 6 * D])
    w_in = din("w_in", [2, D, IN_W])
    lru_wa = din("lru_wa", [2, 2, 8, 128, 128])
    lru_wx = din("lru_wx", [2, 2, 8, 128, 128])
    w_oa = din("w_o_attn", [2, D, D])
    w_ol = din("w_o_lru", [2, D, D])
    w_out = din("w_out", [2, D, D])
    f_wg = din("ffn_w_gate", [1, D, DFF])
    f_wu = din("ffn_w_up", [1, D, DFF])
    f_wd = din("ffn_w_down", [1, DFF, D])
    if n_layers > 1:
        r_w = din("router_w", [1, D, NE])
        m_wg = din("moe_w_gate", [1, NE, D, EFF])
        m_wu = din("moe_w_up", [1, NE, D, EFF])
        m_wd = din("moe_w_down", [1, NE, EFF, D])
    out_rows = T if n_layers == 1 else T // 2
    out_ap = nc.dram_tensor("out", [out_rows, D], F32, kind="ExternalOutput").ap()

    def dscr(name, shape, dt):
        return nc.dram_tensor(name, list(shape), dt).ap()

    XT = dscr("XT", [8, 128, T], F32)
    X1T = dscr("X1T", [8, 128, T], F32)
    HT = dscr("HT", [8, 128, T], BF16)
    XR = dscr("XR", [8, 128, T], BF16)
    XG = dscr("XG", [8, 128, T], BF16)
    YL = dscr("YL", [8, 128, T], BF16)
    AO = dscr("AO", [8, 128, T], BF16)

    with ExitStack() as st:
        fw = FW(nc, st)
        pb = [st.enter_context(nc.psum_tensor("pb%d" % i, [128, 512], F32)) for i in range(8)]
        PB = ["pb%d" % i for i in range(8)]
        ident = fw.sb(st, "ident", [128, 128], F32)
        ones_bf = fw.sb(st, "ones_bf", [128, 128], BF16)
        ones_f = fw.sb(st, "ones_f", [128, 128], F32)
        vecs = fw.sb(st, "vecs", [128, NV], F32)
        modT = fw.sb(st, "modT", [128, 2 * 48], F32)
        sc1p = fw.sb(st, "sc1p", [128, 2 * 48], F32)
        cneg = fw.sb(st, "cneg", [128, 32], F32)
        epsc = fw.sb(st, "epsc", [128, 1], F32)
        cact = fw.sb(st, "cact", [128, 8], F32)

        fw.op("dve", lambda e: e.memset(ident[:], 0.0), writes=["ident"])
        fw.op("pool", lambda e: e.affine_select(out=ident[:], in_=ident[:], compare_op=ALU.not_equal, fill=1.0,
                                                base=0, pattern=[[-1, 128]], channel_multiplier=1),
              reads=["ident"], writes=["ident"])
        fw.op("dve", lambda e: e.memset(ones_bf[:], 1.0), writes=["ones_bf"])
        fw.op("dve", lambda e: e.memset(ones_f[:], 1.0), writes=["ones_f"])
        fw.op("dve", lambda e: e.memset(epsc[:], EPS), writes=["epsc"])
        fw.dma(vecs[:], vecs_in[:, :], writes=["vecs"])

        open_kv = []
        try:
            fw.op("act", lambda e: e.activation(out=cact[:], in_=vecs[:, V_C:V_C + 8], func=AF.Silu),
                  reads=["vecs"], writes=["cact"])
            with ExitStack() as ph:
                xin = [fw.sb(ph, "xin%d" % i, [128, 4, D], F32) for i in range(2)]
                xts = [fw.sb(ph, "xts%d" % i, [128, 8, G], F32) for i in range(2)]
                for g in range(NGRP):
                    b = g % 2
                    fw.dma(xin[b][:], x_in[g * G:(g + 1) * G, :].rearrange("(j p) d -> p j d", p=128), writes=["xin%d" % b])
                    for c in range(8):
                        bank = 1 + (c % 4)
                        for j in range(4):
                            fw.op("pe", lambda e: e.transpose(
                                pb[bank][:, j * 128:(j + 1) * 128], xin[b][:, j, c * 128:(c + 1) * 128], ident[:]),
                                reads=["xin%d" % b, "ident"], writes=[PB[bank]], inc=(j == 3))
                        if c % 2 == 0:
                            fw.op("act", lambda e: e.activation(out=xts[b][:, c, :], in_=pb[bank][:], func=AF.Copy),
                                  reads=[PB[bank]], writes=["xts%d" % b])
                        else:
                            fw.op("dve", lambda e: e.tensor_copy(out=xts[b][:, c, :], in_=pb[bank][:]),
                                  reads=[PB[bank]], writes=["xts%d" % b])
                    fw.dma(XT[:, :, g * G:(g + 1) * G].rearrange("c p t -> p c t"), xts[b][:],
                           reads=["xts%d" % b], writes=["XT_g%d" % g])
                wmb = [fw.sb(ph, "wmb%d" % i, [128, 8, 768], BF16) for i in range(2)]
                cab = fw.sb(ph, "cab", [128, 8, 2], BF16)
                for dd in range(2):
                    fw.op("dve", lambda e, dd=dd: e.tensor_copy(out=cab[:, :, dd], in_=cact[:]), reads=["cact"], writes=["cab"])
                ci = 0
                for l in range(n_layers):
                    for cc in range(8):
                        b = ci % 2
                        ci += 1
                        for k in range(8):
                            fw.dma(wmb[b][:, k, :], w_mod[l, k * 128:(k + 1) * 128, cc * 768:(cc + 1) * 768],
                                   writes=["wmb%d" % b], q="pool", max_dma_last_dim=8192)
                        for jj in range(6):
                            j = cc * 6 + jj
                            for k in range(8):
                                fw.op("pe", lambda e, b=b, jj=jj, k=k, j=j, l=l: e.matmul(
                                    pb[0][:, 2 * (l * 48 + j):2 * (l * 48 + j) + 2], lhsT=wmb[b][:, k, jj * 128:(jj + 1) * 128],
                                    rhs=cab[:, k, :], start=(k == 0), stop=(k == 7)),
                                    reads=["wmb%d" % b, "cab"], writes=[PB[0]], inc=(k == 7))
                nl = n_layers * 48
                fw.op("dve", lambda e: e.tensor_tensor(out=modT[:, 0:nl], in0=pb[0][:, 0:2 * nl].rearrange("p (n two) -> p n two", two=2)[:, :, 0],
                                                       in1=vecs[:, V_BMOD:V_BMOD + nl], op=ALU.add), reads=[PB[0], "vecs"], writes=["modT"])
                fw.op("dve", lambda e: e.tensor_scalar_add(out=sc1p[:, 0:nl], in0=modT[:, 0:nl], scalar1=1.0),
                      reads=["modT"], writes=["sc1p"])
                tmpc = fw.sb(ph, "tmpc", [128, 32], F32)
                for l in range(2):
                    for d in range(2):
                        src = vecs[:, V_LRU + l * 48 + d * 24 + 16:V_LRU + l * 48 + d * 24 + 24]
                        dst = tmpc[:, (l * 2 + d) * 8:(l * 2 + d) * 8 + 8]
                        fw.op("act", lambda e, src=src, dst=dst: e.activation(out=dst, in_=src, func=AF.Exp, scale=-1.0),
                              reads=["vecs"], writes=["tmpc"])
                fw.op("act", lambda e: e.activation(out=tmpc[:], in_=tmpc[:], func=AF.Ln, bias=1.0),
                      reads=["tmpc"], writes=["tmpc"])
                fw.op("dve", lambda e: e.tensor_scalar_mul(out=cneg[:], in0=tmpc[:], scalar1=-8.0),
                      reads=["tmpc"], writes=["cneg"])

            fw.barrier()
            ck("0")

            def load_w(dst3, src2, key, kchunks=8):
                for k in range(kchunks):
                    fw.dma(dst3[:, k, :], src2[k * 128:(k + 1) * 128, :], writes=[key], q="pool", max_dma_last_dim=8192)

            def norm_mod(xt, xkey, l, which, hT, hkey, tmp, tmpkey, sq, sqkey, rs, rskey, bank, hf=None, hfkey=None, part=0):
                if part in (0, 1):
                    fw.op("act", lambda e: e.activation(out=sq[:], in_=xt[:], func=AF.Square), reads=[xkey], writes=[sqkey])
                if part == 1:
                    return
                for c in range(8):
                    fw.op("pe", lambda e, c=c: e.matmul(pb[bank][:], lhsT=ones_bf[:], rhs=sq[:, c * G:(c + 1) * G],
                                                        start=(c == 0), stop=(c == 7)),
                          reads=["ones_bf", sqkey], writes=[PB[bank]], inc=(c == 7))
                fw.op("act", lambda e: e.activation(out=rs[:], in_=pb[bank][:], func=AF.Ln, scale=1.0 / D, bias=epsc[:]),
                      reads=[PB[bank], "epsc"], writes=[rskey])
                fw.op("act", lambda e: e.activation(out=rs[:], in_=rs[:], func=AF.Exp, scale=-0.5),
                      reads=[rskey], writes=[rskey])
                sh0 = l * 48 + which * 24
                for c in range(8):
                    tv = tmp[:, (c % 2) * G:(c % 2 + 1) * G]
                    tk_ = tmpkey + str(c % 2)
                    fw.op("dve", lambda e, c=c: e.tensor_tensor(out=tv, in0=xt[:, c * G:(c + 1) * G],
                                                                in1=rs[:], op=ALU.mult),
                          reads=[xkey, rskey], writes=[tk_])
                    fw.op("act", lambda e, c=c: e.activation(out=hT[:, c * G:(c + 1) * G], in_=tv,
                                                             func=AF.Identity, scale=sc1p[:, sh0 + 8 + c:sh0 + 9 + c],
                                                             bias=modT[:, sh0 + c:sh0 + c + 1]),
                          reads=[tk_, "sc1p", "modT"], writes=[hkey])
                    if hf is not None:
                        fw.op("dve", lambda e, c=c: e.tensor_scalar(out=hf[:, c * G:(c + 1) * G], in0=tv,
                                                                    scalar1=sc1p[:, sh0 + 8 + c:sh0 + 9 + c],
                                                                    scalar2=modT[:, sh0 + c:sh0 + c + 1],
                                                                    op0=ALU.mult, op1=ALU.add),
                              reads=[tk_, "sc1p", "modT"], writes=[hfkey])

            def proj(bank, w3, col0, ncol, hT, hkey, wkey, tok0=0, ntok=G, kch=8, last_inc=True):
                for c in range(kch):
                    fw.op("pe", lambda e, c=c: e.matmul(pb[bank][0:ncol, 0:ntok], lhsT=w3[:, c, col0:col0 + ncol],
                                                        rhs=hT[:, c * G + tok0:c * G + tok0 + ntok],
                                                        start=(c == 0), stop=(c == kch - 1)),
                          reads=[wkey, hkey], writes=[PB[bank]], inc=(c == kch - 1))

            for l in range(n_layers):
                NGo = NGRP if l == 0 else NGRP // 2
                NTo = NGo * G
                vb = lambda off, n=1: vecs[:, off:off + n]
                kvst = ExitStack()
                kvst.__enter__()
                open_kv.append(kvst)
                KT = fw.sb(kvst, "KT%d" % l, [128, 2, T], BF16)
                Vt = fw.sb(kvst, "V%d" % l, [128, T // 128, 256], BF16)
                with ExitStack() as ph:
                    wA = fw.sb(ph, "wA", [128, 8, 2816], BF16)
                    if 'W' not in os.environ.get('PA_SKIP', ''):
                        load_w(wA[:, :, 0:2560], w_in[l, :, OK_:OK_ + 2560], "wA")
                    xA = [fw.sb(ph, "xA%d" % i, [128, 8 * G], F32) for i in range(2)]
                    hA = [fw.sb(ph, "hA%d" % i, [128, 8 * G], BF16) for i in range(2)]
                    tmpA = fw.sb(ph, "tmpA", [128, 2 * G], F32)
                    rsA = fw.sb(ph, "rsA", [128, G], F32)
                    def rot_copy(w, dst0, src0, nheads, key):
                        srcv = w[:, :, src0:src0 + nheads * 128].rearrange("p k (h a b f) -> p k h a b f", h=nheads, a=2, b=2)
                        dstv = w[:, :, dst0:dst0 + nheads * 128].rearrange("p k (h a b f) -> p k h a b f", h=nheads, a=2, b=2)
                        for k in range(8):
                            for bb in range(2):
                                for hh in range(nheads):
                                    fw.op("dve", lambda e, k=k, bb=bb, hh=hh: e.tensor_copy(
                                        out=dstv[:, k, hh, :, bb, :], in_=srcv[:, k, hh, :, 1 - bb, :]),
                                        reads=[key], writes=[key + "r"])
                    if 'R' not in os.environ.get('PA_SKIP', ''):
                        rot_copy(wA, 2560, 0, 2, "wA")
                    ctab = fw.sb(ph, "ctabA", [128, T], F32)
                    stab = fw.sb(ph, "stabA", [128, T], F32)
                    if 'T' not in os.environ.get('PA_SKIP', ''):
                        fw.dma(ctab[:], cos_in[:, :], writes=["ctab"])
                        fw.dma(stab[:], sin_in[:, :], writes=["stab"])
                    sqk = fw.sb(ph, "sqk", [128, G], BF16)
                    rk = fw.sb(ph, "rk", [128, G], F32)
                    t1 = fw.sb(ph, "t1", [128, G], F32)
                    t2 = fw.sb(ph, "t2", [128, G], F32)
                    xrs = [fw.sb(ph, "xrs0", [128, 8, G], BF16)] * 2
                    xgs = [fw.sb(ph, "xgs0", [128, 8, G], BF16)] * 2

                    def qk_norm_rope(bA, bB, bC, g_ap, gp_ap, tok0, out_ap, outkey):
                        qn = int(os.environ.get('QK_N', '8'))
                        opi = [0]
                        def op_(*a, **k):
                            opi[0] += 1
                            if opi[0] <= qn:
                                fw.op(*a, **k)
                        op_("act", lambda e: e.activation(out=sqk[:], in_=pb[bA][:], func=AF.Square),
                              reads=[PB[bA]], writes=["sqk"])
                        op_("pe", lambda e: e.matmul(pb[bC][:], lhsT=ones_bf[:], rhs=sqk[:], start=True, stop=True),
                              reads=["sqk", "ones_bf"], writes=[PB[bC]])
                        op_("act", lambda e: e.activation(out=rk[:], in_=pb[bC][:], func=AF.Ln, scale=1.0 / 128, bias=epsc[:]),
                              reads=[PB[bC], "epsc"], writes=["rk"])
                        op_("act", lambda e: e.activation(out=rk[:], in_=rk[:], func=AF.Exp, scale=-0.5),
                              reads=["rk"], writes=["rk"])
                        op_("dve", lambda e: e.scalar_tensor_tensor(out=t1[:], in0=pb[bA][:], scalar=g_ap,
                                                                      in1=(rk[:] if os.environ.get('QK_ALT') else ctab[:, tok0:tok0 + G]), op0=ALU.mult, op1=ALU.mult),
                              reads=[PB[bA], "vecs", "ctab", "rk"], writes=["t1"])
                        op_("dve", lambda e: e.scalar_tensor_tensor(out=t2[:], in0=pb[bB][:], scalar=gp_ap,
                                                                      in1=stab[:, tok0:tok0 + G], op0=ALU.mult, op1=ALU.mult),
                              reads=[PB[bB], "vecs", "stab"], writes=["t2"])
                        op_("dve", lambda e: e.tensor_tensor(out=t1[:], in0=t1[:], in1=t2[:], op=ALU.add),
                              reads=["t1", "t2"], writes=["t1"])
                        op_("dve", lambda e: e.tensor_tensor(out=out_ap, in0=t1[:], in1=rk[:], op=ALU.mult),
                              reads=["t1", "rk"], writes=[outkey])

                    def load_norm_A(g, part):
                        b = g % 2
                        xk, hk = "xA%d" % b, "hA%d" % b
                        if part in (0, 1):
                            fw.dma(xA[b][:].rearrange("p (c t) -> p c t", c=8),
                                   XT[:, :, g * G:(g + 1) * G].rearrange("c p t -> p c t"),
                                   reads=["XT_g%d" % g], writes=[xk])
                        norm_mod(xA[b], xk, l, 0, hA[b], hk, tmpA, "tmpA", hA[b], hk, rsA, "rsA", 0, part=part)

                    load_norm_A(0, 0)
                    for g in range(NGRP):
                        b = g % 2
                        xk, hk = "xA%d" % b, "hA%d" % b
                        fw.dma(HT[:, :, g * G:(g + 1) * G].rearrange("c p t -> p c t"),
                               hA[b][:].rearrange("p (c t) -> p c t", c=8), reads=[hk], writes=["HT_g%d" % g])
                        for kv in range(2 if 'k' in _PARTS else 0):
                            proj(1, wA, kv * 128, 128, hA[b], hk, "wA")
                            proj(2, wA, 2560 + kv * 128, 128, hA[b], hk, "wAr")
                            qk_norm_rope(1, 2, 3, vb(V_QG + l * 4 + 2), vb(V_QG + l * 4 + 3), g * G,
                                         KT[:, kv, g * G:(g + 1) * G], "KT")
                        if g + 1 < NGRP:
                            load_norm_A(g + 1, 1)
                        for j in range(4 if 'v' in _PARTS else 0):
                            bank = 4 + (j % 2)
                            for c in range(8):
                                fw.op("pe", lambda e, c=c, j=j, bank=bank: e.matmul(
                                    pb[bank][:, 0:256], lhsT=hA[b][:, c * G + j * 128:c * G + (j + 1) * 128],
                                    rhs=wA[:, c, 256:512], start=(c == 0), stop=(c == 7)),
                                    reads=[hk, "wA"], writes=[PB[bank]], inc=(c == 7))
                            fw.op("act", lambda e, j=j, bank=bank: e.activation(out=Vt[:, g * 4 + j, :], in_=pb[bank][:, 0:256],
                                                                                func=AF.Copy),
                                  reads=[PB[bank]], writes=["V"])
                        if g + 1 < NGRP:
                            load_norm_A(g + 1, 2)
                        for blk in range(8 if 'r' in _PARTS else 0):
                            bank = 4 + (blk % 4)
                            proj(bank, wA, 512 + blk * 128, 128, hA[b], hk, "wA")
                            if blk % 2 == 0:
                                fw.op("dve", lambda e, blk=blk, bank=bank: e.tensor_copy(out=xrs[b][:, blk, :], in_=pb[bank][:]),
                                      reads=[PB[bank]], writes=["xrs0"])
                            else:
                                fw.op("act", lambda e, blk=blk, bank=bank: e.activation(out=xrs[b][:, blk, :], in_=pb[bank][:],
                                                                                        func=AF.Copy),
                                      reads=[PB[bank]], writes=["xrs0"])
                        if 'r' in _PARTS:
                            fw.dma(XR[:, :, g * G:(g + 1) * G].rearrange("c p t -> p c t"), xrs[b][:],
                                   reads=["xrs0"], writes=["XR"])
                        if g < NGo and 'g' in _PARTS:
                            for blk in range(8):
                                bank = 4 + (blk % 4)
                                proj(bank, wA, 1536 + blk * 128, 128, hA[b], hk, "wA")
                                fw.op("act", lambda e, blk=blk, bank=bank: e.activation(out=xgs[b][:, blk, :], in_=pb[bank][:],
                                                                                        func=AF.Gelu_apprx_tanh),
                                      reads=[PB[bank]], writes=["xgs0"])
                            fw.dma(XG[:, :, g * G:(g + 1) * G].rearrange("c p t -> p c t"), xgs[b][:],
                                   reads=["xgs0"], writes=["XG"])
                fw.barrier()

                ck("A%d" % l)
                with ExitStack() as ph:
                    wlr = fw.sb(ph, "wlr", [128, 32, 128], BF16)
                    for d in range(2):
                        fw.dma(wlr[:, d * 16:d * 16 + 8, :], lru_wa[l, d].rearrange("n i j -> i n j"), writes=["wlr"],
                               q="pool", max_dma_last_dim=8192)
                        fw.dma(wlr[:, d * 16 + 8:d * 16 + 16, :], lru_wx[l, d].rearrange("n i j -> i n j"), writes=["wlr"],
                               q="pool", max_dma_last_dim=8192)
                    CE = os.environ.get("CONV_ENG", "pool")
                    xrp = [fw.sb(ph, "xrp%d" % i, [128, T + 4], BF16) for i in range(2)]
                    xcb = [fw.sb(ph, "xcb%d" % i, [128, T], BF16) for i in range(2)]
                    hb = [fw.sb(ph, "hb%d" % i, [128, T], BF16) for i in range(2)]
                    rb_ = [fw.sb(ph, "rb%d" % i, [128, T], F32) for i in range(2)]
                    ibs = [fw.sb(ph, "ib%d" % i, [128, T], F32) for i in range(2)]
                    abs_ = [fw.sb(ph, "ab%d" % i, [128, T], F32) for i in range(2)]
                    xgb = fw.sb(ph, "xgb", [128, NTo], BF16)
                    for i in range(2):
                        fw.op("dve", lambda e, i=i: e.memset(xrp[i][:, 0:2], 0.0), writes=["xrp%d" % i])
                        fw.op("dve", lambda e, i=i: e.memset(xrp[i][:, T + 2:T + 4], 0.0), writes=["xrp%d" % i])
                    cw = V_CONVW + l * 40

                    dgc = fw.sb(ph, "dgc", [128, 40, 128], BF16)
                    for blk_ in range(8):
                        for j in range(5):
                            fw.op("dve", lambda e: e.tensor_scalar(
                                out=dgc[:, blk_ * 5 + j, :], in0=ident[:], scalar1=vecs[:, cw + j * 8 + blk_:cw + j * 8 + blk_ + 1],
                                scalar2=None, op0=ALU.mult), reads=["ident", "vecs"], writes=["dgc"])

                    def conv_stage(blk):
                        pb_ = blk % 2
                        xk_, ck_, dk_ = "xrp%d" % pb_, "xcb%d" % pb_, "dgc"
                        fw.dma(xrp[pb_][:, 2:T + 2], XR[blk, :, :], reads=["XR"], writes=[xk_])
                        for g8 in range(NGRP):
                            bank = 6 + (g8 % 2)
                            for j in range(5):
                                fw.op("pe", lambda e, j=j: e.matmul(
                                    pb[bank][:], lhsT=dgc[:, blk * 5 + j, :], rhs=xrp[pb_][:, j + g8 * G:j + (g8 + 1) * G],
                                    start=(j == 0), stop=(j == 4)), reads=[dk_, xk_], writes=[PB[bank]], inc=(j == 4))
                            fw.op("dve", lambda e: e.tensor_scalar(
                                out=xcb[pb_][:, g8 * G:(g8 + 1) * G], in0=pb[bank][:],
                                scalar1=vecs[:, V_CONVB + l * 8 + blk:V_CONVB + l * 8 + blk + 1], scalar2=None, op0=ALU.add),
                                reads=[PB[bank], "vecs"], writes=[ck_])

                    conv_stage(0)
                    for blk in range(8):
                        pb_ = blk % 2
                        ck_ = "xcb%d" % pb_
                        fw.dma(xgb[:], XG[blk, :, 0:NTo], reads=["XG"], writes=["xgb"])
                        for d in range(2):
                            lo = V_LRU + l * 48 + d * 24
                            r, ib, ab = rb_[d], ibs[d], abs_[d]
                            rkey, ikey, akey = "rb%d" % d, "ib%d" % d, "ab%d" % d
                            Td = NTo if d == 0 else T
                            for g8 in range(Td // G):
                                ba, bi = (g8 % 3) * 2, (g8 % 3) * 2 + 1
                                fw.op("pe", lambda e: e.matmul(
                                    pb[ba][:], lhsT=wlr[:, d * 16 + blk, :], rhs=xcb[pb_][:, g8 * G:(g8 + 1) * G], start=True, stop=True),
                                    reads=["wlr", ck_], writes=[PB[ba]])
                                fw.op("pe", lambda e: e.matmul(
                                    pb[bi][:], lhsT=wlr[:, d * 16 + 8 + blk, :], rhs=xcb[pb_][:, g8 * G:(g8 + 1) * G], start=True, stop=True),
                                    reads=["wlr", ck_], writes=[PB[bi]])
                                fw.op("act", lambda e: e.activation(
                                    out=r[:, g8 * G:(g8 + 1) * G], in_=pb[ba][:], func=AF.Sigmoid,
                                    bias=vecs[:, lo + blk:lo + blk + 1]), reads=[PB[ba], "vecs"], writes=[rkey])
                                fw.op("act", lambda e: e.activation(
                                    out=ib[:, g8 * G:(g8 + 1) * G], in_=pb[bi][:], func=AF.Sigmoid,
                                    bias=vecs[:, lo + 8 + blk:lo + 8 + blk + 1]), reads=[PB[bi], "vecs"], writes=[ikey])
                            if d == 0 and blk + 1 < 8:
                                conv_stage(blk + 1)
                            fw.op("dve", lambda e: e.tensor_tensor(out=ib[:, 0:Td], in0=ib[:, 0:Td], in1=xcb[pb_][:, 0:Td], op=ALU.mult),
                                  reads=[ikey, ck_], writes=[ikey])
                            cn = cneg[:, (l * 2 + d) * 8 + blk:(l * 2 + d) * 8 + blk + 1]
                            fw.op("act", lambda e: e.activation(out=ab[:, 0:Td], in_=r[:, 0:Td], func=AF.Exp, scale=cn),
                                  reads=[rkey, "cneg"], writes=[akey])
                            fw.op("act", lambda e: e.activation(out=r[:, 0:Td], in_=ab[:, 0:Td], func=AF.Square),
                                  reads=[akey], writes=[rkey])
                            fw.op("act", lambda e: e.activation(out=r[:, 0:Td], in_=r[:, 0:Td], func=AF.Sqrt, scale=-1.0, bias=1.0),
                                  reads=[rkey], writes=[rkey])
                            fw.op("dve", lambda e: e.tensor_tensor(out=ib[:, 0:Td], in0=ib[:, 0:Td], in1=r[:, 0:Td], op=ALU.mult),
                                  reads=[ikey, rkey], writes=[ikey])
                            if d == 0:
                                fw.op("dve", lambda e: e.tensor_tensor_scan(out=hb[0][:, 0:Td], data0=ab[:, 0:Td], data1=ib[:, 0:Td], initial=0.0,
                                                                            op0=ALU.mult, op1=ALU.add),
                                      reads=[akey, ikey], writes=["hb0"])
                            else:
                                fw.op("dve", lambda e: e.tensor_tensor_scan(out=hb[1][:, ::-1], data0=ab[:, ::-1], data1=ib[:, ::-1],
                                                                            initial=0.0, op0=ALU.mult, op1=ALU.add),
                                      reads=[akey, ikey], writes=["hb1"])
                        fw.op("pool", lambda e: e.tensor_tensor(out=hb[0][:, 0:NTo], in0=hb[0][:, 0:NTo], in1=hb[1][:, 0:NTo],
                                                                op=ALU.add), reads=["hb0", "hb1"], writes=["hb0"])
                        fw.op("pool", lambda e: e.tensor_tensor(out=xgb[:], in0=hb[0][:, 0:NTo], in1=xgb[:], op=ALU.mult),
                              reads=["hb0", "xgb"], writes=["xgb"])
                        fw.dma(YL[blk, :, 0:NTo], xgb[:], reads=["xgb"], writes=["YL"])
                fw.barrier()

                ck("B%d" % l)
                with ExitStack() as ph:
                    wq = fw.sb(ph, "wq", [128, 8, 2048], BF16)
                    load_w(wq[:, :, 0:1024], w_in[l, :, OQ:OQ + 1024], "wq")
                    ctab = fw.sb(ph, "ctabC", [128, NTo], F32)
                    stab = fw.sb(ph, "stabC", [128, NTo], F32)
                    fw.dma(ctab[:], cos_in[:, 0:NTo], writes=["ctab"])
                    fw.dma(stab[:], sin_in[:, 0:NTo], writes=["stab"])
                    srcv = wq[:, :, 0:1024].rearrange("p k (h a b f) -> p k h a b f", h=8, a=2, b=2)
                    dstv = wq[:, :, 1024:2048].rearrange("p k (h a b f) -> p k h a b f", h=8, a=2, b=2)
                    for k in range(8):
                        for bb in range(2):
                            for a in range(2):
                                fw.op("dve", lambda e, k=k, bb=bb, a=a: e.tensor_copy(
                                    out=dstv[:, k, :, a, bb, :], in_=srcv[:, k, :, a, 1 - bb, :]),
                                    reads=["wq"], writes=["wqr"])
                    hC = [fw.sb(ph, "hC%d" % i, [128, 8 * G], BF16) for i in range(2)]
                    qT = fw.sb(ph, "qT", [128, 8, G], BF16)
                    sqk = fw.sb(ph, "sqkC", [128, G], BF16)
                    rk = fw.sb(ph, "rkC", [128, G], F32)
                    t1 = fw.sb(ph, "t1C", [128, G], F32)
                    t2 = fw.sb(ph, "t2C", [128, G], F32)
                    PTb = [fw.sb(ph, "PT%d" % i, [128, G], BF16) for i in range(3)]
                    rec = fw.sb(ph, "rec", [128, G], F32)
                    aos = [fw.sb(ph, "aos%d" % i, [128, 8, G], BF16) for i in range(2)]
                    sc = 1.0 / math.sqrt(128.0)
                    for g in range(min(NGo, int(os.environ.get('C1_NG', '99')))):
                        b = g % 2
                        hk = "hC%d" % b
                        fw.dma(hC[b][:].rearrange("p (c t) -> p c t", c=8),
                               HT[:, :, g * G:(g + 1) * G].rearrange("c p t -> p c t"), reads=["HT_g%d" % g], writes=[hk])
                        for h in range(8):
                            proj(0, wq, h * 128, 128, hC[b], hk, "wq")
                            proj(1, wq, 1024 + h * 128, 128, hC[b], hk, "wqr")
                            fw.op("act", lambda e: e.activation(out=sqk[:], in_=pb[0][:], func=AF.Square),
                                  reads=[PB[0]], writes=["sqk"])
                            fw.op("pe", lambda e: e.matmul(pb[2][:], lhsT=ones_bf[:], rhs=sqk[:], start=True, stop=True),
                                  reads=["sqk", "ones_bf"], writes=[PB[2]])
                            fw.op("act", lambda e: e.activation(out=rk[:], in_=pb[2][:], func=AF.Ln, scale=1.0 / 128, bias=epsc[:]),
                                  reads=[PB[2], "epsc"], writes=["rk"])
                            fw.op("act", lambda e: e.activation(out=rk[:], in_=rk[:], func=AF.Exp, scale=-0.5),
                                  reads=["rk"], writes=["rk"])
                            fw.op("dve", lambda e: e.scalar_tensor_tensor(out=t1[:], in0=pb[0][:], scalar=vb(V_QG + l * 4 + 0),
                                                                          in1=ctab[:, g * G:(g + 1) * G], op0=ALU.mult, op1=ALU.mult),
                                  reads=[PB[0], "vecs", "ctab"], writes=["t1"])
                            fw.op("dve", lambda e: e.scalar_tensor_tensor(out=t2[:], in0=pb[1][:], scalar=vb(V_QG + l * 4 + 1),
                                                                          in1=stab[:, g * G:(g + 1) * G], op0=ALU.mult, op1=ALU.mult),
                                  reads=[PB[1], "vecs", "stab"], writes=["t2"])
                            fw.op("dve", lambda e: e.tensor_tensor(out=t1[:], in0=t1[:], in1=t2[:], op=ALU.add),
                                  reads=["t1", "t2"], writes=["t1"])
                            fw.op("dve", lambda e, h=h: e.tensor_tensor(out=qT[:, h, :], in0=t1[:], in1=rk[:], op=ALU.mult),
                                  reads=["t1", "rk"], writes=["qT%d" % h])
                        for h in range(8):
                            kv = h // 4
                            bo, bs = 4 + (h % 2) * 2, 5 + (h % 2) * 2
                            NKT = T // 128

                            def emit_st(kt):
                                bst = kt % 3
                                fw.op("pe", lambda e: e.matmul(
                                    pb[bst][:], lhsT=KT[:, kv, kt * 128:(kt + 1) * 128], rhs=qT[:, h, :], start=True, stop=True),
                                    reads=["KT", "qT%d" % h], writes=[PB[bst]])
                                fw.op("act", lambda e: e.activation(out=PTb[kt % 3][:], in_=pb[bst][:], func=AF.Exp, scale=sc),
                                      reads=[PB[bst]], writes=["PT%d" % (kt % 3)])

                            emit_st(0)
                            emit_st(1)
                            for kt in range(NKT):
                                if kt + 2 < NKT:
                                    emit_st(kt + 2)
                                ptk = "PT%d" % (kt % 3)
                                last = kt == NKT - 1
                                fw.op("pe", lambda e: e.matmul(
                                    pb[bo][:], lhsT=Vt[:, kt, kv * 128:(kv + 1) * 128], rhs=PTb[kt % 3][:], start=(kt == 0), stop=last),
                                    reads=["V", ptk], writes=[PB[bo]], inc=False)
                                fw.op("pe", lambda e: e.matmul(
                                    pb[bs][:], lhsT=ones_bf[:], rhs=PTb[kt % 3][:], start=(kt == 0), stop=last),
                                    reads=["ones_bf", ptk], writes=[PB[bs]], inc=True)
                            fw.op("dve", lambda e, bs=bs: e.reciprocal(out=rec[:], in_=pb[bs][:]), reads=[PB[bs]], writes=["rec"])
                            fw.op("dve", lambda e, bo=bo, h=h: e.tensor_tensor(out=aos[b][:, h, :], in0=pb[bo][:], in1=rec[:], op=ALU.mult),
                                  reads=[PB[bo], "rec"], writes=["aos%d" % b])
                        fw.dma(AO[:, :, g * G:(g + 1) * G].rearrange("c p t -> p c t"), aos[b][:],
                               reads=["aos%d" % b], writes=["AO"])
                fw.barrier()
                kvst.__exit__(None, None, None)
                open_kv.remove(kvst)

                ck("C1%d" % l)
                with ExitStack() as ph:
                    wg_ = fw.sb(ph, "wgt", [128, 8, 2048], BF16)
                    load_w(wg_, w_in[l, :, OGA:OGA + 2048], "wgt")
                    woa = fw.sb(ph, "woa", [128, 8, D], BF16)
                    wol = fw.sb(ph, "wol", [128, 8, D], BF16)
                    wo = fw.sb(ph, "wo", [128, 8, D], BF16)
                    load_w(woa, w_oa[l], "woa")
                    load_w(wol, w_ol[l], "wol")
                    load_w(wo, w_out[l], "wo")
                    hC = [fw.sb(ph, "hD%d" % i, [128, 8 * G], BF16) for i in range(2)]
                    aoC = [fw.sb(ph, "aoD%d" % i, [128, 8 * G], BF16) for i in range(2)]
                    ylC = [fw.sb(ph, "ylD%d" % i, [128, 8 * G], BF16) for i in range(2)]
                    xC = [fw.sb(ph, "xD%d" % i, [128, 8 * G], F32) for i in range(2)]
                    mg = fw.sb(ph, "mg", [128, 8 * G], BF16)
                    sg = [fw.sb(ph, "sg%d" % i, [128, G], F32) for i in range(2)]
                    m1 = [fw.sb(ph, "m1%d" % i, [128, G], F32) for i in range(2)]
                    for g in range(NGo):
                        b = g % 2
                        sl = lambda A: A[:, :, g * G:(g + 1) * G].rearrange("c p t -> p c t")
                        v3 = lambda t_: t_[:].rearrange("p (c t) -> p c t", c=8)
                        fw.dma(v3(hC[b]), sl(HT), reads=["HT_g%d" % g], writes=["hD%d" % b])
                        fw.dma(v3(aoC[b]), sl(AO), reads=["AO"], writes=["aoD%d" % b])
                        fw.dma(v3(ylC[b]), sl(YL), reads=["YL"], writes=["ylD%d" % b])
                        fw.dma(v3(xC[b]), sl(XT), reads=["XT_g%d" % g], writes=["xD%d" % b])
                        for c in range(8):
                            o4 = (c % 2) * 4
                            proj(o4 + 0, woa, c * 128, 128, aoC[b], "aoD%d" % b, "woa")
                            proj(o4 + 1, wg_, c * 128, 128, hC[b], "hD%d" % b, "wgt")
                            proj(o4 + 2, wol, c * 128, 128, ylC[b], "ylD%d" % b, "wol")
                            proj(o4 + 3, wg_, 1024 + c * 128, 128, hC[b], "hD%d" % b, "wgt")
                            fw.op("act", lambda e: e.activation(out=sg[0][:], in_=pb[o4 + 1][:], func=AF.Sigmoid), reads=[PB[o4 + 1]], writes=["sg0"])
                            fw.op("act", lambda e: e.activation(out=sg[1][:], in_=pb[o4 + 3][:], func=AF.Sigmoid), reads=[PB[o4 + 3]], writes=["sg1"])
                            fw.op("dve", lambda e: e.tensor_tensor(out=m1[0][:], in0=pb[o4 + 0][:], in1=sg[0][:], op=ALU.mult),
                                  reads=[PB[o4 + 0], "sg0"], writes=["m10"])
                            fw.op("dve", lambda e: e.tensor_tensor(out=m1[1][:], in0=pb[o4 + 2][:], in1=sg[1][:], op=ALU.mult),
                                  reads=[PB[o4 + 2], "sg1"], writes=["m11"])
                            fw.op("dve", lambda e, c=c: e.tensor_tensor(out=mg[:, c * G:(c + 1) * G], in0=m1[0][:], in1=m1[1][:], op=ALU.add),
                                  reads=["m10", "m11"], writes=["mg"])
                        g1o = l * 48 + 16
                        for c in range(8):
                            bank = 4 + (c % 2)
                            proj(bank, wo, c * 128, 128, mg, "mg", "wo")
                            fw.op("dve", lambda e, c=c, bank=bank: e.scalar_tensor_tensor(
                                out=xC[b][:, c * G:(c + 1) * G], in0=pb[bank][:], scalar=modT[:, g1o + c:g1o + c + 1],
                                in1=xC[b][:, c * G:(c + 1) * G], op0=ALU.mult, op1=ALU.add),
                                reads=[PB[bank], "modT", "xD%d" % b], writes=["xD%d" % b])
                        fw.dma(sl(X1T), v3(xC[b]), reads=["xD%d" % b], writes=["X1T_g%d" % g])
                fw.barrier()

                ck("C2%d" % l)
                if l == 0:
                    with ExitStack() as ph:
                        wg = fw.sb(ph, "fwg", [128, 8, DFF], BF16)
                        wu = fw.sb(ph, "fwu", [128, 8, DFF], BF16)
                        wd = fw.sb(ph, "fwd", [128, NF0, D], BF16)
                        load_w(wg, f_wg[0], "fwg")
                        load_w(wu, f_wu[0], "fwu")
                        load_w(wd, f_wd[0], "fwd", kchunks=NF0)
                        xD = [fw.sb(ph, "xE%d" % i, [128, 8 * G], F32) for i in range(2)]
                        hD = fw.sb(ph, "hE", [128, 8 * G], BF16)
                        tmpD = fw.sb(ph, "tmpE", [128, 2 * G], F32)
                        rsD = fw.sb(ph, "rsE", [128, G], F32)
                        act = fw.sb(ph, "actE", [128, NF0 * G], BF16)
                        sil = [fw.sb(ph, "sil%d" % i, [128, G], F32) for i in range(2)]
                        for g in range(NGo):
                            b = g % 2
                            sl = lambda A: A[:, :, g * G:(g + 1) * G].rearrange("c p t -> p c t")
                            v3 = lambda t_: t_[:].rearrange("p (c t) -> p c t", c=8)
                            if g == 0:
                                fw.dma(v3(xD[b]), sl(X1T), reads=["X1T_g%d" % g], writes=["xE%d" % b])
                                norm_mod(xD[b], "xE%d" % b, l, 1, hD, "hE", tmpD, "tmpE", hD, "hE", rsD, "rsE", 0)
                            for f in range(NF0):
                                bg, bu = 1 + (f % 2) * 2, 2 + (f % 2) * 2
                                proj(bg, wg, f * 128, 128, hD, "hE", "fwg")
                                proj(bu, wu, f * 128, 128, hD, "hE", "fwu")
                                fw.op("act", lambda e, f=f, bg=bg: e.activation(out=sil[f % 2][:], in_=pb[bg][:], func=AF.Silu),
                                      reads=[PB[bg]], writes=["sil%d" % (f % 2)])
                                fw.op("dve", lambda e, f=f, bu=bu: e.tensor_tensor(out=act[:, f * G:(f + 1) * G], in0=pb[bu][:],
                                                                                   in1=sil[f % 2][:], op=ALU.mult),
                                      reads=[PB[bu], "sil%d" % (f % 2)], writes=["actE%d" % f])
                            g2o = l * 48 + 40
                            if g + 1 < NGo:
                                b1 = (g + 1) % 2
                                fw.dma(v3(xD[b1]), X1T[:, :, (g + 1) * G:(g + 2) * G].rearrange("c p t -> p c t"),
                                       reads=["X1T_g%d" % (g + 1)], writes=["xE%d" % b1])
                                norm_mod(xD[b1], "xE%d" % b1, l, 1, hD, "hE", tmpD, "tmpE", hD, "hE", rsD, "rsE", 0, part=1)
                            for c in range(8):
                                if c == 3 and g + 1 < NGo:
                                    norm_mod(xD[b1], "xE%d" % b1, l, 1, hD, "hE", tmpD, "tmpE", hD, "hE", rsD, "rsE", 0, part=2)
                                bank = 5 + (c % 2)
                                for f in range(NF0):
                                    fw.op("pe", lambda e, f=f, c=c, bank=bank: e.matmul(
                                        pb[bank][:], lhsT=wd[:, f, c * 128:(c + 1) * 128], rhs=act[:, f * G:(f + 1) * G],
                                        start=(f == 0), stop=(f == NF0 - 1)),
                                        reads=["fwd", "actE%d" % f], writes=[PB[bank]], inc=(f == NF0 - 1))
                                fw.op("dve", lambda e, c=c, bank=bank: e.scalar_tensor_tensor(
                                    out=xD[b][:, c * G:(c + 1) * G], in0=pb[bank][:], scalar=modT[:, g2o + c:g2o + c + 1],
                                    in1=xD[b][:, c * G:(c + 1) * G], op0=ALU.mult, op1=ALU.add),
                                    reads=[PB[bank], "modT", "xE%d" % b], writes=["xE%d" % b])
                            fw.dma(sl(XT), v3(xD[b]), reads=["xE%d" % b], writes=["XT_g%d" % g])
                    fw.barrier()
                    ck("D0")
                else:
                    with ExitStack() as ph:
                        NTk = NTo // 128
                        acc = fw.sb(ph, "acc", [128, 8, NTo], F32)
                        hM = fw.sb(ph, "hM", [128, 8, NTo], BF16)
                        comb = fw.sb(ph, "comb", [128, NTk, 8], F32)
                        rw = fw.sb(ph, "rw", [128, 8, 8], F32)
                        rwh = fw.sb(ph, "rwh", [128, 8, 8], BF16)
                        rwl = fw.sb(ph, "rwl", [128, 8, 8], BF16)
                        for k in range(8):
                            fw.dma(rw[:, k, :], r_w[0, k * 128:(k + 1) * 128, :], writes=["rw"])
                        fw.op("dve", lambda e: e.tensor_copy(out=rwh[:], in_=rw[:]), reads=["rw"], writes=["rwh"])
                        fw.op("dve", lambda e: e.tensor_tensor(out=rw[:], in0=rw[:], in1=rwh[:], op=ALU.subtract), reads=["rw", "rwh"], writes=["rw"])
                        fw.op("dve", lambda e: e.tensor_copy(out=rwl[:], in_=rw[:]), reads=["rw"], writes=["rwl"])
                        with ExitStack() as ph2:
                            xD = fw.sb(ph2, "xM", [128, 8 * G], F32)
                            hD = fw.sb(ph2, "hMg", [128, 8 * G], BF16)
                            hF = fw.sb(ph2, "hMf", [128, 8 * G], F32)
                            hFh = fw.sb(ph2, "hMfh", [128, 8 * G], BF16)
                            hFl = fw.sb(ph2, "hMfl", [128, 8 * G], BF16)
                            tmpD = fw.sb(ph2, "tmpM", [128, 2 * G], F32)
                            rsD = fw.sb(ph2, "rsM", [128, G], F32)
                            lg = fw.sb(ph2, "lg", [128, 8], F32)
                            m8 = fw.sb(ph2, "m8", [128, 8], F32)
                            ex = fw.sb(ph2, "ex", [128, 8], F32)
                            msk = fw.sb(ph2, "msk", [128, 8], F32)
                            nm1 = fw.sb(ph2, "nm1", [128, 1], F32)
                            ssum = fw.sb(ph2, "ssum", [128, 1], F32)
                            for g in range(NGo):
                                sl = lambda A: A[:, :, g * G:(g + 1) * G].rearrange("c p t -> p c t")
                                v3 = lambda t_: t_[:].rearrange("p (c t) -> p c t", c=8)
                                fw.dma(v3(xD), sl(X1T), reads=["X1T_g%d" % g], writes=["xM"])
                                norm_mod(xD, "xM", l, 1, hD, "hMg", tmpD, "tmpM", hD, "hMg", rsD, "rsM", 0, hf=hF, hfkey="hMf")
                                fw.op("act", lambda e, g=g: e.activation(out=acc[:, :, g * G:(g + 1) * G], in_=v3(xD), func=AF.Copy),
                                      reads=["xM"], writes=["acc_g%d" % g])
                                fw.op("dve", lambda e, g=g: e.tensor_copy(out=hM[:, :, g * G:(g + 1) * G], in_=v3(hD)),
                                      reads=["hMg"], writes=["hM"])
                                fw.op("act", lambda e: e.activation(out=hFh[:], in_=hF[:], func=AF.Copy), reads=["hMf"], writes=["hMfh"])
                                fw.op("dve", lambda e: e.tensor_tensor(out=hF[:], in0=hF[:], in1=hFh[:], op=ALU.subtract),
                                      reads=["hMf", "hMfh"], writes=["hMf"])
                                fw.op("act", lambda e: e.activation(out=hFl[:], in_=hF[:], func=AF.Copy), reads=["hMf"], writes=["hMfl"])
                                for j in range(4):
                                    tk = g * 4 + j
                                    n_ = 0
                                    for c in range(8):
                                        for (ha, hk_, wa, wk_) in ((hFh, "hMfh", rwh, "rwh"), (hFh, "hMfh", rwl, "rwl"), (hFl, "hMfl", rwh, "rwh")):
                                            fw.op("pe", lambda e, c=c, j=j, ha=ha, wa=wa, n_=n_: e.matmul(
                                                pb[1][:, 0:8], lhsT=ha[:, c * G + j * 128:c * G + (j + 1) * 128], rhs=wa[:, c, :],
                                                start=(n_ == 0), stop=(n_ == 23)), reads=[hk_, wk_], writes=[PB[1]], inc=(n_ == 23))
                                            n_ += 1
                                    fw.op("dve", lambda e: e.tensor_tensor(out=lg[:], in0=pb[1][:, 0:8], in1=vecs[:, V_RB:V_RB + 8], op=ALU.add),
                                          reads=[PB[1], "vecs"], writes=["lg"])
                                    fw.op("dve", lambda e: e.max(out=m8[:], in_=lg[:]), reads=["lg"], writes=["m8"])
                                    fw.op("dve", lambda e: e.tensor_scalar(out=msk[:], in0=lg[:], scalar1=m8[:, 1:2], scalar2=None, op0=ALU.is_ge),
                                          reads=["lg", "m8"], writes=["msk"])
                                    fw.op("dve", lambda e: e.tensor_scalar_mul(out=nm1[:], in0=m8[:, 0:1], scalar1=-1.0),
                                          reads=["m8"], writes=["nm1"])
                                    fw.op("act", lambda e: e.activation(out=ex[:], in_=lg[:], func=AF.Exp, bias=nm1[:]),
                                          reads=["lg", "nm1"], writes=["ex"])
                                    fw.op("dve", lambda e: e.tensor_tensor(out=ex[:], in0=ex[:], in1=msk[:], op=ALU.mult),
                                          reads=["ex", "msk"], writes=["ex"])
                                    fw.op("dve", lambda e: e.reduce_sum(out=ssum[:], in_=ex[:], axis=mybir.AxisListType.X),
                                          reads=["ex"], writes=["ssum"])
                                    fw.op("dve", lambda e: e.reciprocal(out=ssum[:], in_=ssum[:]), reads=["ssum"], writes=["ssum"])
                                    fw.op("dve", lambda e, tk=tk: e.tensor_scalar(out=comb[:, tk, :], in0=ex[:], scalar1=ssum[:, 0:1], scalar2=None,
                                                                                   op0=ALU.mult), reads=["ex", "ssum"], writes=["comb"])
                        fw.barrier()
                        NCH = 7
                        FC = EFF // NCH
                        wgc = [fw.sb(ph, "mwg%d" % i, [128, 8, FC], BF16) for i in range(2)]
                        wuc = [fw.sb(ph, "mwu%d" % i, [128, 8, FC], BF16) for i in range(2)]
                        wdc = [fw.sb(ph, "mwd%d" % i, [128, 4, D], BF16) for i in range(2)]
                        cbc = [fw.sb(ph, "cbc%d" % i, [128, NTo], F32) for i in range(2)]
                        dg = [fw.sb(ph, "dg%d" % i, [128, 128], F32) for i in range(2)]
                        dgh = [fw.sb(ph, "dgh%d" % i, [128, 128], BF16) for i in range(2)]
                        dgl = [fw.sb(ph, "dgl%d" % i, [128, 128], BF16) for i in range(2)]
                        actM = [fw.sb(ph, "actM%d" % i, [128, 4 * G], BF16) for i in range(2)]
                        sil = [fw.sb(ph, "silM%d" % i, [128, G], F32) for i in range(2)]
                        tm = [fw.sb(ph, "tmM%d" % i, [128, G], F32) for i in range(2)]
                        g2o = l * 48 + 40
                        it = 0
                        for ex_ in range(NE):
                            cb = cbc[ex_ % 2]
                            cbk = "cbc%d" % (ex_ % 2)
                            for tk in range(NTk):
                                d_ = dg[tk % 2]
                                fw.op("dve", lambda e, tk=tk, d_=d_, ex_=ex_: e.tensor_scalar(
                                    out=d_[:], in0=ident[:], scalar1=comb[:, tk, ex_:ex_ + 1], scalar2=None, op0=ALU.mult),
                                    reads=["ident", "comb"], writes=["dg%d" % (tk % 2)])
                                bank = 6 + (tk % 2)
                                dh_, dl_ = dgh[tk % 2], dgl[tk % 2]
                                fw.op("dve", lambda e, d_=d_, dh_=dh_: e.tensor_copy(out=dh_[:], in_=d_[:]), reads=["dg%d" % (tk % 2)], writes=["dgh%d" % (tk % 2)])
                                fw.op("dve", lambda e, d_=d_, dh_=dh_: e.tensor_tensor(out=d_[:], in0=d_[:], in1=dh_[:], op=ALU.subtract),
                                      reads=["dg%d" % (tk % 2), "dgh%d" % (tk % 2)], writes=["dg%d" % (tk % 2)])
                                fw.op("dve", lambda e, d_=d_, dl_=dl_: e.tensor_copy(out=dl_[:], in_=d_[:]), reads=["dg%d" % (tk % 2)], writes=["dgl%d" % (tk % 2)])
                                fw.op("pe", lambda e, dh_=dh_, bank=bank: e.matmul(pb[bank][:, 0:128], lhsT=ones_bf[:], rhs=dh_[:], start=True, stop=False),
                                      reads=["ones_bf", "dgh%d" % (tk % 2)], writes=[PB[bank]], inc=False)
                                fw.op("pe", lambda e, dl_=dl_, bank=bank: e.matmul(pb[bank][:, 0:128], lhsT=ones_bf[:], rhs=dl_[:], start=False, stop=True),
                                      reads=["ones_bf", "dgl%d" % (tk % 2)], writes=[PB[bank]])
                                fw.op("act", lambda e, tk=tk, cb=cb, bank=bank: e.activation(out=cb[:, tk * 128:(tk + 1) * 128], in_=pb[bank][:, 0:128],
                                                                                             func=AF.Copy), reads=[PB[bank]], writes=[cbk])
                            for ch in range(NCH):
                                wb = it % 2
                                it += 1
                                kg_, ku_, kd_ = "mwg%d" % wb, "mwu%d" % wb, "mwd%d" % wb
                                load_w(wgc[wb], m_wg[0, ex_, :, ch * FC:(ch + 1) * FC], kg_)
                                load_w(wuc[wb], m_wu[0, ex_, :, ch * FC:(ch + 1) * FC], ku_)
                                load_w(wdc[wb], m_wd[0, ex_, ch * FC:(ch + 1) * FC, :], kd_, kchunks=4)
                                for g in range(NGo):
                                    am = actM[g % 2]
                                    amk = "actM%d" % (g % 2)
                                    for f in range(4):
                                        bg, bu = (f % 2) * 2, 1 + (f % 2) * 2
                                        for c in range(8):
                                            fw.op("pe", lambda e, c=c, f=f, g=g, wb=wb, bg=bg: e.matmul(
                                                pb[bg][:], lhsT=wgc[wb][:, c, f * 128:(f + 1) * 128], rhs=hM[:, c, g * G:(g + 1) * G],
                                                start=(c == 0), stop=(c == 7)), reads=[kg_, "hM"], writes=[PB[bg]], inc=(c == 7))
                                        for c in range(8):
                                            fw.op("pe", lambda e, c=c, f=f, g=g, wb=wb, bu=bu: e.matmul(
                                                pb[bu][:], lhsT=wuc[wb][:, c, f * 128:(f + 1) * 128], rhs=hM[:, c, g * G:(g + 1) * G],
                                                start=(c == 0), stop=(c == 7)), reads=[ku_, "hM"], writes=[PB[bu]], inc=(c == 7))
                                        fw.op("act", lambda e, f=f, bg=bg: e.activation(out=sil[f % 2][:], in_=pb[bg][:], func=AF.Silu),
                                              reads=[PB[bg]], writes=["silM%d" % (f % 2)])
                                        fw.op("dve", lambda e, f=f, bu=bu, am=am: e.tensor_tensor(out=am[:, f * G:(f + 1) * G], in0=pb[bu][:],
                                                                                                 in1=sil[f % 2][:], op=ALU.mult),
                                              reads=[PB[bu], "silM%d" % (f % 2)], writes=[amk + "_%d" % f])
                                    for c in range(8):
                                        bank = 4 + (c % 4)
                                        for f in range(4):
                                            fw.op("pe", lambda e, c=c, f=f, wb=wb, bank=bank, am=am: e.matmul(
                                                pb[bank][:], lhsT=wdc[wb][:, f, c * 128:(c + 1) * 128], rhs=am[:, f * G:(f + 1) * G],
                                                start=(f == 0), stop=(f == 3)), reads=[kd_, amk + "_%d" % f], writes=[PB[bank]], inc=(f == 3))
                                        fw.op("dve", lambda e, c=c, bank=bank, g=g, cb=cb: e.tensor_tensor(
                                            out=tm[c % 2][:], in0=pb[bank][:], in1=cb[:, g * G:(g + 1) * G], op=ALU.mult),
                                            reads=[PB[bank], cbk], writes=["tmM%d" % (c % 2)])
                                        fw.op("dve", lambda e, c=c, g=g: e.scalar_tensor_tensor(
                                            out=acc[:, c, g * G:(g + 1) * G], in0=tm[c % 2][:], scalar=modT[:, g2o + c:g2o + c + 1],
                                            in1=acc[:, c, g * G:(g + 1) * G], op0=ALU.mult, op1=ALU.add),
                                            reads=["tmM%d" % (c % 2), "modT", "acc_g%d" % g], writes=["acc_g%d" % g])
                        for g in range(NGo):
                            fw.dma(XT[:, :, g * G:(g + 1) * G].rearrange("c p t -> p c t"), acc[:, :, g * G:(g + 1) * G],
                                   reads=["acc_g%d" % g], writes=["XT_g%d" % g])
                    fw.barrier()

            NGf = out_rows // G
            with ExitStack() as ph:
                xF = [fw.sb(ph, "xF%d" % i, [128, 8 * G], F32) for i in range(2)]
                sqF = fw.sb(ph, "sqF", [128, 8 * G], BF16)
                rsF = fw.sb(ph, "rsF", [128, G], F32)
                yF = fw.sb(ph, "yF", [128, 8 * G], F32)
                oF = [fw.sb(ph, "oF%d" % i, [128, D], F32) for i in range(2)]
                oi = 0
                for g in range(NGf):
                    b = g % 2
                    xk = "xF%d" % b
                    fw.dma(xF[b][:].rearrange("p (c t) -> p c t", c=8), XT[:, :, g * G:(g + 1) * G].rearrange("c p t -> p c t"),
                           reads=["XT_g%d" % g], writes=[xk])
                    fw.op("act", lambda e, b=b: e.activation(out=sqF[:], in_=xF[b][:], func=AF.Square), reads=[xk], writes=["sqF"])
                    for c in range(8):
                        fw.op("pe", lambda e, c=c: e.matmul(pb[0][:], lhsT=ones_bf[:], rhs=sqF[:, c * G:(c + 1) * G], start=(c == 0), stop=(c == 7)),
                              reads=["ones_bf", "sqF"], writes=[PB[0]], inc=(c == 7))
                    fw.op("act", lambda e: e.activation(out=rsF[:], in_=pb[0][:], func=AF.Ln, scale=1.0 / D, bias=epsc[:]),
                          reads=[PB[0], "epsc"], writes=["rsF"])
                    fw.op("act", lambda e: e.activation(out=rsF[:], in_=rsF[:], func=AF.Exp, scale=-0.5), reads=["rsF"], writes=["rsF"])
                    for c in range(8):
                        fw.op("dve", lambda e, c=c, b=b: e.scalar_tensor_tensor(
                            out=yF[:, c * G:(c + 1) * G], in0=xF[b][:, c * G:(c + 1) * G], scalar=vecs[:, V_FG + c:V_FG + c + 1],
                            in1=rsF[:], op0=ALU.mult, op1=ALU.mult), reads=[xk, "vecs", "rsF"], writes=["yF%d" % c])
                    for j in range(4):
                        ob = oi % 2
                        oi += 1
                        for half in range(2):
                            bank = 1 + half + 2 * ob
                            for cc in range(4):
                                c = half * 4 + cc
                                fw.op("pe", lambda e, c=c, cc=cc, j=j, bank=bank: e.transpose(
                                    pb[bank][:, cc * 128:(cc + 1) * 128], yF[:, c * G + j * 128:c * G + (j + 1) * 128], ident[:]),
                                    reads=["yF%d" % c, "ident"], writes=[PB[bank]], inc=(cc == 3))
                            if half == 0:
                                fw.op("act", lambda e, ob=ob, bank=bank: e.activation(out=oF[ob][:, 0:512], in_=pb[bank][:], func=AF.Copy),
                                      reads=[PB[bank]], writes=["oF%d" % ob])
                            else:
                                fw.op("dve", lambda e, ob=ob, bank=bank: e.tensor_copy(out=oF[ob][:, 512:1024], in_=pb[bank][:]),
                                      reads=[PB[bank]], writes=["oF%d" % ob])
                        r0 = g * G + j * 128
                        fw.dma(out_ap[r0:r0 + 128, :], oF[ob][:], reads=["oF%d" % ob], writes=["out_%d" % (r0 // 128)])
                fw.wait_all("sp", ["out_%d" % i for i in range(out_rows // 128)])
        except _Stop:
            fw.barrier()
            for k_ in open_kv:
                k_.__exit__(None, None, None)
        fw.finish()
    return nc


def _pmaj(v, n):
    return np.ascontiguousarray(np.asarray(v, np.float32).reshape(n, 128).T)


def _rope_tables():
    pos = np.arange(T)
    row = (pos // 64).astype(np.float32)
    col = (pos % 64).astype(np.float32)
    n_freq = 32
    inv_freq = np.exp(-math.log(10000.0) * (2.0 * np.arange(n_freq, dtype=np.float32) / 64.0)).astype(np.float32)
    cosT = np.zeros((128, T), np.float32)
    sinT = np.zeros((128, T), np.float32)
    for d in range(128):
        axis, half, f = d // 64, (d % 64) // 32, d % 32
        ang = (row if axis == 0 else col) * inv_freq[f]
        cosT[d] = np.cos(ang)
        sinT[d] = np.sin(ang) * (-1.0 if half == 0 else 1.0)
    return cosT, sinT


_NC_CACHE = {}


def _get_nc(n_layers, debug=False):
    key = (n_layers, debug)
    if key not in _NC_CACHE:
        _NC_CACHE[key] = build_nc(n_layers, debug)
    return _NC_CACHE[key]


def make_in_maps(inputs, n_layers=2):
    f = lambda k: np.asarray(inputs[k], np.float32)
    x, c = f("x"), f("c")
    cosT, sinT = _rope_tables()
    partner = np.array([d + 32 if (d % 64) < 32 else d - 32 for d in range(128)])
    in_maps = []
    shared = {}
    for k in ["w_mod", "w_in", "w_o_attn", "w_o_lru", "w_out", "ffn_w_gate", "ffn_w_up", "ffn_w_down"]:
        shared[k] = np.ascontiguousarray(f(k))
    if n_layers > 1:
        for k in ["router_w", "moe_w_gate", "moe_w_up", "moe_w_down"]:
            shared[k] = np.ascontiguousarray(f(k))
    lwa, lwx = f("lru_w_a"), f("lru_w_x")
    for core in range(8):
        bi, odd = core // 2, core % 2
        vecs = np.zeros((128, NV), np.float32)
        for l in range(2):
            vecs[:, V_BMOD + l * 48:V_BMOD + (l + 1) * 48] = _pmaj(f("b_mod")[l], 48)
            qg, kg = f("q_norm_gain")[l], f("k_norm_gain")[l]
            vecs[:, V_QG + l * 4 + 0] = qg
            vecs[:, V_QG + l * 4 + 1] = qg[partner]
            vecs[:, V_QG + l * 4 + 2] = kg
            vecs[:, V_QG + l * 4 + 3] = kg[partner]
            cw = f("conv_w")[l]
            taps = np.zeros((5, 1024), np.float32)
            if not odd:
                taps[0:4] = cw
            else:
                taps[1:5] = cw[::-1]
            for j in range(5):
                vecs[:, V_CONVW + l * 40 + j * 8:V_CONVW + l * 40 + (j + 1) * 8] = _pmaj(taps[j], 8)
            vecs[:, V_CONVB + l * 8:V_CONVB + (l + 1) * 8] = _pmaj(f("conv_b")[l], 8)
            for d in range(2):
                sd = d if not odd else 1 - d
                o = V_LRU + l * 48 + d * 24
                vecs[:, o:o + 8] = _pmaj(f("lru_b_a")[l, sd], 8)
                vecs[:, o + 8:o + 16] = _pmaj(f("lru_b_x")[l, sd], 8)
                vecs[:, o + 16:o + 24] = _pmaj(f("lru_lambda")[l, sd], 8)
        vecs[:, V_FG:V_FG + 8] = _pmaj(f("final_gain"), 8)
        vecs[:, V_RB:V_RB + 8] = np.broadcast_to(f("router_b")[0][None, :], (128, 8))
        vecs[:, V_C:V_C + 8] = _pmaj(c[bi], 8)
        xs = x[bi]
        m = dict(shared)
        if odd:
            m["x"] = np.ascontiguousarray(xs[::-1])
            m["cosT"] = np.ascontiguousarray(cosT[:, ::-1])
            m["sinT"] = np.ascontiguousarray(sinT[:, ::-1])
            m["lru_wa"] = np.ascontiguousarray(lwa[:, ::-1])
            m["lru_wx"] = np.ascontiguousarray(lwx[:, ::-1])
        else:
            m["x"] = np.ascontiguousarray(xs)
            m["cosT"] = cosT
            m["sinT"] = sinT
            m["lru_wa"] = np.ascontiguousarray(lwa)
            m["lru_wx"] = np.ascontiguousarray(lwx)
        m["vecs"] = vecs
        in_maps.append(m)
    return in_maps


def kernel(**inputs):
    nc = _get_nc(2)
    in_maps = make_in_maps(inputs, 2)
    res = run_bass_kernel_spmd(nc, in_maps, core_ids=list(range(8)))
    out = np.zeros((4, T, D), np.float32)
    for core in range(8):
        bi, odd = core // 2, core % 2
        o = np.asarray(res.results[core]["out"], np.float32)
        if odd:
            out[bi, T // 2:] = o[::-1]
        else:
            out[bi, :T // 2] = o
    return out
```

```python
import math
import os
_PARTS = os.environ.get('PA_PARTS', 'nkvrg')
import numpy as np
import concourse.bass as bass
import concourse.mybir as mybir
from concourse.bass_utils import run_bass_kernel_spmd
from contextlib import ExitStack

F32 = mybir.dt.float32
BF16 = mybir.dt.bfloat16
ALU = mybir.AluOpType
AF = mybir.ActivationFunctionType

ENGS = ("pe", "act", "dve", "pool", "sp")

T = 4096
D = 1024
G = 512
NGRP = T // G
DFF = 2816
NF0 = DFF // 128
EFF = 3584
NE = 8
IN_W = 5632
EPS = 1e-6
OQ, OK_, OV, OXR, OXG, OGA, OGL = 0, 1024, 1280, 1536, 2560, 3584, 4608

V_BMOD = 0
V_QG = 96
V_CONVW = 104
V_CONVB = 184
V_LRU = 200
V_FG = 296
V_RB = 304
V_C = 312
NV = 320


class _Rec:
    def __getattr__(self, name):
        def f(*a, **k):
            self.call = (name, a, k)
            return self
        return f


class FW:
    def __init__(self, nc, stack, n_dma_sems=48):
        self.nc = nc
        self.stack = stack
        self.sem = {k: stack.enter_context(nc.semaphore("s_" + k)) for k in ENGS}
        self.cnt = {k: 0 for k in ENGS}
        self.pending = {k: [] for k in ENGS}
        self.seen = {k: {} for k in ENGS}
        self.prog = {k: [] for k in ENGS}
        self.dsems = [stack.enter_context(nc.semaphore("d%d" % i)) for i in range(n_dma_sems)]
        self.dval = [0] * n_dma_sems
        self.n_hw = n_dma_sems - 20
        self.dnext_hw = 0
        self.dnext_sw = 0
        self.semobj = {}
        for k in ENGS:
            self.semobj["s_" + k] = self.sem[k]
        for i, s in enumerate(self.dsems):
            self.semobj["d%d" % i] = s
        self.lastw = {}
        self.readers = {}
        self.ninstr = 0

    def sb(self, st, name, shape, dt):
        self.nsb = getattr(self, "nsb", 0) + 1
        return st.enter_context(self.nc.sbuf_tensor("sb%d_%s" % (self.nsb, name), list(shape), dt))

    def _need(self, eng, reads, writes):
        need = {}

        def add(sv):
            if sv is None:
                return
            s, v = sv
            if eng == "pe" and s == "s_pe":
                return
            if need.get(s, 0) < v:
                need[s] = v
        for k in reads:
            add(self.lastw.get(k))
        for k in writes:
            add(self.lastw.get(k))
            for r in self.readers.get(k, ()):
                add(r)
        out = []
        seen = self.seen[eng]
        for s, v in need.items():
            if seen.get(s, 0) >= v:
                continue
            seen[s] = v
            out.append((s, v))
        return out

    def _emit_waits(self, eng, waits):
        for s, v in waits:
            so = self.semobj[s]
            self.prog[eng].append(lambda e, so=so, v=v: e.wait_ge(so, v))

    def op(self, eng, fn, reads=(), writes=(), inc=True):
        pr = [k for k in reads if k.startswith("pb")]
        if pr:
            reads = [k for k in reads if not k.startswith("pb")]
            writes = list(writes) + pr
        rec = _Rec()
        fn(rec)
        mname, margs, mkw = rec.call
        fn = lambda e, mname=mname, margs=margs, mkw=mkw: getattr(e, mname)(*margs, **mkw)
        waits = self._need(eng, reads, writes)
        self._emit_waits(eng, waits)
        self.pending[eng].append((tuple(reads), tuple(writes)))
        self.ninstr += 1
        if inc:
            self.cnt[eng] += 1
            v = self.cnt[eng]
            s = "s_" + eng
            so = self.sem[eng]
            self.prog[eng].append(lambda e, fn=fn, so=so: fn(e).then_inc(so, 1))
            for rd, wr in self.pending[eng]:
                for k in rd:
                    self.readers.setdefault(k, []).append((s, v))
                for k in wr:
                    self.lastw[k] = (s, v)
                    self.readers[k] = []
            self.pending[eng] = []
        else:
            self.prog[eng].append(lambda e, fn=fn: fn(e))

    def dma(self, out, in_, reads=(), writes=(), q="sp", **kw):
        if q == "pool":
            i = self.n_hw + self.dnext_sw
            self.dnext_sw = (self.dnext_sw + 1) % (len(self.dsems) - self.n_hw)
        else:
            i = self.dnext_hw
            self.dnext_hw = (self.dnext_hw + 1) % self.n_hw
        s = "d%d" % i
        waits = self._need(q, reads, writes)
        prev = self.dval[i]
        if prev > 0 and self.seen[q].get(s, 0) < prev:
            self.seen[q][s] = prev
            waits.append((s, prev))
        self._emit_waits(q, waits)
        self.dval[i] += 16
        v = self.dval[i]
        so = self.dsems[i]
        self.prog[q].append(
            lambda e, out=out, in_=in_, so=so, kw=kw: e.dma_start(out=out, in_=in_, **kw).then_inc(so, 16))
        self.ninstr += 1
        for k in reads:
            self.readers.setdefault(k, []).append((s, v))
        for k in writes:
            self.lastw[k] = (s, v)
            self.readers[k] = []

    def wait_all(self, eng, keys):
        self._emit_waits(eng, self._need(eng, keys, ()))

    def barrier(self):
        allv = [("s_" + k, self.cnt[k]) for k in ENGS if self.cnt[k] > 0]
        allv += [("d%d" % i, v) for i, v in enumerate(self.dval) if v > 0]
        for eng in ENGS:
            w = []
            for s, v in allv:
                if self.seen[eng].get(s, 0) < v:
                    self.seen[eng][s] = v
                    w.append((s, v))
            self._emit_waits(eng, w)

    def finish(self):
        nc = self.nc
        prog = self.prog
        with nc.Block() as block:
            @block.tensor
            def _(e):
                for f in prog["pe"]:
                    f(e)

            @block.scalar
            def _(e):
                for f in prog["act"]:
                    f(e)

            @block.vector
            def _(e):
                for f in prog["dve"]:
                    f(e)

            @block.gpsimd
            def _(e):
                for f in prog["pool"]:
                    f(e)

            @block.sync
            def _(e):
                for f in prog["sp"]:
                    f(e)


class _Stop(Exception):
    pass


def build_nc(n_layers=2, debug=False, stop_after=None):
    nc = bass.Bass("TRN2", target_bir_lowering=False)

    def ck(name):
        if stop_after == name:
            raise _Stop()

    def din(name, shape, dt=F32):
        return nc.dram_tensor(name, list(shape), dt, kind="ExternalInput").ap()

    x_in = din("x", [T, D])
    vecs_in = din("vecs", [128, NV])
    cos_in = din("cosT", [128, T])
    sin_in = din("sinT", [128, T])
    w_mod = din("w_mod", [2, D, 6 * D])
    w_in = din("w_in", [2, D, IN_W])
    lru_wa = din("lru_wa", [2, 2, 8, 128, 128])
    lru_wx = din("lru_wx", [2, 2, 8, 128, 128])
    w_oa = din("w_o_attn", [2, D, D])
    w_ol = din("w_o_lru", [2, D, D])
    w_out = din("w_out", [2, D, D])
    f_wg = din("ffn_w_gate", [1, D, DFF])
    f_wu = din("ffn_w_up", [1, D, DFF])
    f_wd = din("ffn_w_down", [1, DFF, D])
    if n_layers > 1:
        r_w = din("router_w", [1, D, NE])
        m_wg = din("moe_w_gate", [1, NE, D, EFF])
        m_wu = din("moe_w_up", [1, NE, D, EFF])
        m_wd = din("moe_w_down", [1, NE, EFF, D])
    out_rows = T if n_layers == 1 else T // 2
    out_ap = nc.dram_tensor("out", [out_rows, D], F32, kind="ExternalOutput").ap()

    def dscr(name, shape, dt):
        return nc.dram_tensor(name, list(shape), dt).ap()

    XT = dscr("XT", [8, 128, T], F32)
    X1T = dscr("X1T", [8, 128, T], F32)
    HT = dscr("HT", [8, 128, T], BF16)
    XR = dscr("XR", [8, 128, T], BF16)
    XG = dscr("XG", [8, 128, T], BF16)
    YL = dscr("YL", [8, 128, T], BF16)
    AO = dscr("AO", [8, 128, T], BF16)

    with ExitStack() as st:
        fw = FW(nc, st)
        pb = [st.enter_context(nc.psum_tensor("pb%d" % i, [128, 512], F32)) for i in range(8)]
        PB = ["pb%d" % i for i in range(8)]
        ident = fw.sb(st, "ident", [128, 128], F32)
        ones_bf = fw.sb(st, "ones_bf", [128, 128], BF16)
        ones_f = fw.sb(st, "ones_f", [128, 128], F32)
        vecs = fw.sb(st, "vecs", [128, NV], F32)
        modT = fw.sb(st, "modT", [128, 2 * 48], F32)
        sc1p = fw.sb(st, "sc1p", [128, 2 * 48], F32)
        cneg = fw.sb(st, "cneg", [128, 32], F32)
        epsc = fw.sb(st, "epsc", [128, 1], F32)
        cact = fw.sb(st, "cact", [128, 8], F32)

        fw.op("dve", lambda e: e.memset(ident[:], 0.0), writes=["ident"])
        fw.op("pool", lambda e: e.affine_select(out=ident[:], in_=ident[:], compare_op=ALU.not_equal, fill=1.0,
                                                base=0, pattern=[[-1, 128]], channel_multiplier=1),
              reads=["ident"], writes=["ident"])
        fw.op("dve", lambda e: e.memset(ones_bf[:], 1.0), writes=["ones_bf"])
        fw.op("dve", lambda e: e.memset(ones_f[:], 1.0), writes=["ones_f"])
        fw.op("dve", lambda e: e.memset(epsc[:], EPS), writes=["epsc"])
        fw.dma(vecs[:], vecs_in[:, :], writes=["vecs"])

        open_kv = []
        try:
            fw.op("act", lambda e: e.activation(out=cact[:], in_=vecs[:, V_C:V_C + 8], func=AF.Silu),
                  reads=["vecs"], writes=["cact"])
            with ExitStack() as ph:
                xin = [fw.sb(ph, "xin%d" % i, [128, 4, D], F32) for i in range(2)]
                xts = [fw.sb(ph, "xts%d" % i, [128, 8, G], F32) for i in range(2)]
                for g in range(NGRP):
                    b = g % 2
                    fw.dma(xin[b][:], x_in[g * G:(g + 1) * G, :].rearrange("(j p) d -> p j d", p=128), writes=["xin%d" % b])
                    for c in range(8):
                        bank = 1 + (c % 4)
                        for j in range(4):
                            fw.op("pe", lambda e: e.transpose(
                                pb[bank][:, j * 128:(j + 1) * 128], xin[b][:, j, c * 128:(c + 1) * 128], ident[:]),
                                reads=["xin%d" % b, "ident"], writes=[PB[bank]], inc=(j == 3))
                        if c % 2 == 0:
                            fw.op("act", lambda e: e.activation(out=xts[b][:, c, :], in_=pb[bank][:], func=AF.Copy),
                                  reads=[PB[bank]], writes=["xts%d" % b])
                        else:
                            fw.op("dve", lambda e: e.tensor_copy(out=xts[b][:, c, :], in_=pb[bank][:]),
                                  reads=[PB[bank]], writes=["xts%d" % b])
                    fw.dma(XT[:, :, g * G:(g + 1) * G].rearrange("c p t -> p c t"), xts[b][:],
                           reads=["xts%d" % b], writes=["XT_g%d" % g])
                wmb = [fw.sb(ph, "wmb%d" % i, [128, 8, 768], BF16) for i in range(2)]
                cab = fw.sb(ph, "cab", [128, 8, 2], BF16)
                for dd in range(2):
                    fw.op("dve", lambda e, dd=dd: e.tensor_copy(out=cab[:, :, dd], in_=cact[:]), reads=["cact"], writes=["cab"])
                ci = 0
                for l in range(n_layers):
                    for cc in range(8):
                        b = ci % 2
                        ci += 1
                        for k in range(8):
                            fw.dma(wmb[b][:, k, :], w_mod[l, k * 128:(k + 1) * 128, cc * 768:(cc + 1) * 768],
                                   writes=["wmb%d" % b], q="pool", max_dma_last_dim=8192)
                        for jj in range(6):
                            j = cc * 6 + jj
                            for k in range(8):
                                fw.op("pe", lambda e, b=b, jj=jj, k=k, j=j, l=l: e.matmul(
                                    pb[0][:, 2 * (l * 48 + j):2 * (l * 48 + j) + 2], lhsT=wmb[b][:, k, jj * 128:(jj + 1) * 128],
                                    rhs=cab[:, k, :], start=(k == 0), stop=(k == 7)),
                                    reads=["wmb%d" % b, "cab"], writes=[PB[0]], inc=(k == 7))
                nl = n_layers * 48
                fw.op("dve", lambda e: e.tensor_tensor(out=modT[:, 0:nl], in0=pb[0][:, 0:2 * nl].rearrange("p (n two) -> p n two", two=2)[:, :, 0],
                                                       in1=vecs[:, V_BMOD:V_BMOD + nl], op=ALU.add), reads=[PB[0], "vecs"], writes=["modT"])
                fw.op("dve", lambda e: e.tensor_scalar_add(out=sc1p[:, 0:nl], in0=modT[:, 0:nl], scalar1=1.0),
                      reads=["modT"], writes=["sc1p"])
                tmpc = fw.sb(ph, "tmpc", [128, 32], F32)
                for l in range(2):
                    for d in range(2):
                        src = vecs[:, V_LRU + l * 48 + d * 24 + 16:V_LRU + l * 48 + d * 24 + 24]
                        dst = tmpc[:, (l * 2 + d) * 8:(l * 2 + d) * 8 + 8]
                        fw.op("act", lambda e, src=src, dst=dst: e.activation(out=dst, in_=src, func=AF.Exp, scale=-1.0),
                              reads=["vecs"], writes=["tmpc"])
                fw.op("act", lambda e: e.activation(out=tmpc[:], in_=tmpc[:], func=AF.Ln, bias=1.0),
                      reads=["tmpc"], writes=["tmpc"])
                fw.op("dve", lambda e: e.tensor_scalar_mul(out=cneg[:], in0=tmpc[:], scalar1=-8.0),
                      reads=["tmpc"], writes=["cneg"])

            fw.barrier()
            ck("0")

            def load_w(dst3, src2, key, kchunks=8):
                for k in range(kchunks):
                    fw.dma(dst3[:, k, :], src2[k * 128:(k + 1) * 128, :], writes=[key], q="pool", max_dma_last_dim=8192)

            def norm_mod(xt, xkey, l, which, hT, hkey, tmp, tmpkey, sq, sqkey, rs, rskey, bank, hf=None, hfkey=None, part=0):
                if part in (0, 1):
                    fw.op("act", lambda e: e.activation(out=sq[:], in_=xt[:], func=AF.Square), reads=[xkey], writes=[sqkey])
                if part == 1:
                    return
                for c in range(8):
                    fw.op("pe", lambda e, c=c: e.matmul(pb[bank][:], lhsT=ones_bf[:], rhs=sq[:, c * G:(c + 1) * G],
                                                        start=(c == 0), stop=(c == 7)),
                          reads=["ones_bf", sqkey], writes=[PB[bank]], inc=(c == 7))
                fw.op("act", lambda e: e.activation(out=rs[:], in_=pb[bank][:], func=AF.Ln, scale=1.0 / D, bias=epsc[:]),
                      reads=[PB[bank], "epsc"], writes=[rskey])
                fw.op("act", lambda e: e.activation(out=rs[:], in_=rs[:], func=AF.Exp, scale=-0.5),
                      reads=[rskey], writes=[rskey])
                sh0 = l * 48 + which * 24
                for c in range(8):
                    tv = tmp[:, (c % 2) * G:(c % 2 + 1) * G]
                    tk_ = tmpkey + str(c % 2)
                    fw.op("dve", lambda e, c=c: e.tensor_tensor(out=tv, in0=xt[:, c * G:(c + 1) * G],
                                                                in1=rs[:], op=ALU.mult),
                          reads=[xkey, rskey], writes=[tk_])
                    fw.op("act", lambda e, c=c: e.activation(out=hT[:, c * G:(c + 1) * G], in_=tv,
                                                             func=AF.Identity, scale=sc1p[:, sh0 + 8 + c:sh0 + 9 + c],
                                                             bias=modT[:, sh0 + c:sh0 + c + 1]),
                          reads=[tk_, "sc1p", "modT"], writes=[hkey])
                    if hf is not None:
                        fw.op("dve", lambda e, c=c: e.tensor_scalar(out=hf[:, c * G:(c + 1) * G], in0=tv,
                                                                    scalar1=sc1p[:, sh0 + 8 + c:sh0 + 9 + c],
                                                                    scalar2=modT[:, sh0 + c:sh0 + c + 1],
                                                                    op0=ALU.mult, op1=ALU.add),
                              reads=[tk_, "sc1p", "modT"], writes=[hfkey])

            def proj(bank, w3, col0, ncol, hT, hkey, wkey, tok0=0, ntok=G, kch=8, last_inc=True):
                for c in range(kch):
                    fw.op("pe", lambda e, c=c: e.matmul(pb[bank][0:ncol, 0:ntok], lhsT=w3[:, c, col0:col0 + ncol],
                                                        rhs=hT[:, c * G + tok0:c * G + tok0 + ntok],
                                                        start=(c == 0), stop=(c == kch - 1)),
                          reads=[wkey, hkey], writes=[PB[bank]], inc=(c == kch - 1))

            for l in range(n_layers):
                NGo = NGRP if l == 0 else NGRP // 2
                NTo = NGo * G
                vb = lambda off, n=1: vecs[:, off:off + n]
                kvst = ExitStack()
                kvst.__enter__()
                open_kv.append(kvst)
                KT = fw.sb(kvst, "KT%d" % l, [128, 2, T], BF16)
                Vt = fw.sb(kvst, "V%d" % l, [128, T // 128, 256], BF16)
                with ExitStack() as ph:
                    wA = fw.sb(ph, "wA", [128, 8, 2816], BF16)
                    if 'W' not in os.environ.get('PA_SKIP', ''):
                        load_w(wA[:, :, 0:2560], w_in[l, :, OK_:OK_ + 2560], "wA")
                    xA = [fw.sb(ph, "xA%d" % i, [128, 8 * G], F32) for i in range(2)]
                    hA = [fw.sb(ph, "hA%d" % i, [128, 8 * G], BF16) for i in range(2)]
                    tmpA = fw.sb(ph, "tmpA", [128, 2 * G], F32)
                    rsA = fw.sb(ph, "rsA", [128, G], F32)
                    def rot_copy(w, dst0, src0, nheads, key):
                        srcv = w[:, :, src0:src0 + nheads * 128].rearrange("p k (h a b f) -> p k h a b f", h=nheads, a=2, b=2)
                        dstv = w[:, :, dst0:dst0 + nheads * 128].rearrange("p k (h a b f) -> p k h a b f", h=nheads, a=2, b=2)
                        for k in range(8):
                            for bb in range(2):
                                for hh in range(nheads):
                                    fw.op("dve", lambda e, k=k, bb=bb, hh=hh: e.tensor_copy(
                                        out=dstv[:, k, hh, :, bb, :], in_=srcv[:, k, hh, :, 1 - bb, :]),
                                        reads=[key], writes=[key + "r"])
                    if 'R' not in os.environ.get('PA_SKIP', ''):
                        rot_copy(wA, 2560, 0, 2, "wA")
                    ctab = fw.sb(ph, "ctabA", [128, T], F32)
                    stab = fw.sb(ph, "stabA", [128, T], F32)
                    if 'T' not in os.environ.get('PA_SKIP', ''):
                        fw.dma(ctab[:], cos_in[:, :], writes=["ctab"])
                        fw.dma(stab[:], sin_in[:, :], writes=["stab"])
                    sqk = fw.sb(ph, "sqk", [128, G], BF16)
                    rk = fw.sb(ph, "rk", [128, G], F32)
                    t1 = fw.sb(ph, "t1", [128, G], F32)
                    t2 = fw.sb(ph, "t2", [128, G], F32)
                    xrs = [fw.sb(ph, "xrs0", [128, 8, G], BF16)] * 2
                    xgs = [fw.sb(ph, "xgs0", [128, 8, G], BF16)] * 2

                    def qk_norm_rope(bA, bB, bC, g_ap, gp_ap, tok0, out_ap, outkey):
                        qn = int(os.environ.get('QK_N', '8'))
                        opi = [0]
                        def op_(*a, **k):
                            opi[0] += 1
                            if opi[0] <= qn:
                                fw.op(*a, **k)
                        op_("act", lambda e: e.activation(out=sqk[:], in_=pb[bA][:], func=AF.Square),
                              reads=[PB[bA]], writes=["sqk"])
                        op_("pe", lambda e: e.matmul(pb[bC][:], lhsT=ones_bf[:], rhs=sqk[:], start=True, stop=True),
                              reads=["sqk", "ones_bf"], writes=[PB[bC]])
                        op_("act", lambda e: e.activation(out=rk[:], in_=pb[bC][:], func=AF.Ln, scale=1.0 / 128, bias=epsc[:]),
                              reads=[PB[bC], "epsc"], writes=["rk"])
                        op_("act", lambda e: e.activation(out=rk[:], in_=rk[:], func=AF.Exp, scale=-0.5),
                              reads=["rk"], writes=["rk"])
                        op_("dve", lambda e: e.scalar_tensor_tensor(out=t1[:], in0=pb[bA][:], scalar=g_ap,
                                                                      in1=(rk[:] if os.environ.get('QK_ALT') else ctab[:, tok0:tok0 + G]), op0=ALU.mult, op1=ALU.mult),
                              reads=[PB[bA], "vecs", "ctab", "rk"], writes=["t1"])
                        op_("dve", lambda e: e.scalar_tensor_tensor(out=t2[:], in0=pb[bB][:], scalar=gp_ap,
                                                                      in1=stab[:, tok0:tok0 + G], op0=ALU.mult, op1=ALU.mult),
                              reads=[PB[bB], "vecs", "stab"], writes=["t2"])
                        op_("dve", lambda e: e.tensor_tensor(out=t1[:], in0=t1[:], in1=t2[:], op=ALU.add),
                              reads=["t1", "t2"], writes=["t1"])
                        op_("dve", lambda e: e.tensor_tensor(out=out_ap, in0=t1[:], in1=rk[:], op=ALU.mult),
                              reads=["t1", "rk"], writes=[outkey])

                    def load_norm_A(g, part):
                        b = g % 2
                        xk, hk = "xA%d" % b, "hA%d" % b
                        if part in (0, 1):
                            fw.dma(xA[b][:].rearrange("p (c t) -> p c t", c=8),
                                   XT[:, :, g * G:(g + 1) * G].rearrange("c p t -> p c t"),
                                   reads=["XT_g%d" % g], writes=[xk])
                        norm_mod(xA[b], xk, l, 0, hA[b], hk, tmpA, "tmpA", hA[b], hk, rsA, "rsA", 0, part=part)

                    load_norm_A(0, 0)
                    for g in range(NGRP):
                        b = g % 2
                        xk, hk = "xA%d" % b, "hA%d" % b
                        fw.dma(HT[:, :, g * G:(g + 1) * G].rearrange("c p t -> p c t"),
                               hA[b][:].rearrange("p (c t) -> p c t", c=8), reads=[hk], writes=["HT_g%d" % g])
                        for kv in range(2 if 'k' in _PARTS else 0):
                            proj(1, wA, kv * 128, 128, hA[b], hk, "wA")
                            proj(2, wA, 2560 + kv * 128, 128, hA[b], hk, "wAr")
                            qk_norm_rope(1, 2, 3, vb(V_QG + l * 4 + 2), vb(V_QG + l * 4 + 3), g * G,
                                         KT[:, kv, g * G:(g + 1) * G], "KT")
                        if g + 1 < NGRP:
                            load_norm_A(g + 1, 1)
                        for j in range(4 if 'v' in _PARTS else 0):
                            bank = 4 + (j % 2)
                            for c in range(8):
                                fw.op("pe", lambda e, c=c, j=j, bank=bank: e.matmul(
                                    pb[bank][:, 0:256], lhsT=hA[b][:, c * G + j * 128:c * G + (j + 1) * 128],
                                    rhs=wA[:, c, 256:512], start=(c == 0), stop=(c == 7)),
                                    reads=[hk, "wA"], writes=[PB[bank]], inc=(c == 7))
                            fw.op("act", lambda e, j=j, bank=bank: e.activation(out=Vt[:, g * 4 + j, :], in_=pb[bank][:, 0:256],
                                                                                func=AF.Copy),
                                  reads=[PB[bank]], writes=["V"])
                        if g + 1 < NGRP:
                            load_norm_A(g + 1, 2)
                        for blk in range(8 if 'r' in _PARTS else 0):
                            bank = 4 + (blk % 4)
                            proj(bank, wA, 512 + blk * 128, 128, hA[b], hk, "wA")
                            if blk % 2 == 0:
                                fw.op("dve", lambda e, blk=blk, bank=bank: e.tensor_copy(out=xrs[b][:, blk, :], in_=pb[bank][:]),
                                      reads=[PB[bank]], writes=["xrs0"])
                            else:
                                fw.op("act", lambda e, blk=blk, bank=bank: e.activation(out=xrs[b][:, blk, :], in_=pb[bank][:],
                                                                                        func=AF.Copy),
                                      reads=[PB[bank]], writes=["xrs0"])
                        if 'r' in _PARTS:
                            fw.dma(XR[:, :, g * G:(g + 1) * G].rearrange("c p t -> p c t"), xrs[b][:],
                                   reads=["xrs0"], writes=["XR"])
                        if g < NGo and 'g' in _PARTS:
                            for blk in range(8):
                                bank = 4 + (blk % 4)
                                proj(bank, wA, 1536 + blk * 128, 128, hA[b], hk, "wA")
                                fw.op("act", lambda e, blk=blk, bank=bank: e.activation(out=xgs[b][:, blk, :], in_=pb[bank][:],
                                                                                        func=AF.Gelu_apprx_tanh),
                                      reads=[PB[bank]], writes=["xgs0"])
                            fw.dma(XG[:, :, g * G:(g + 1) * G].rearrange("c p t -> p c t"), xgs[b][:],
                                   reads=["xgs0"], writes=["XG"])
                fw.barrier()

                ck("A%d" % l)
                with ExitStack() as ph:
                    wlr = fw.sb(ph, "wlr", [128, 32, 128], BF16)
                    for d in range(2):
                        fw.dma(wlr[:, d * 16:d * 16 + 8, :], lru_wa[l, d].rearrange("n i j -> i n j"), writes=["wlr"],
                               q="pool", max_dma_last_dim=8192)
                        fw.dma(wlr[:, d * 16 + 8:d * 16 + 16, :], lru_wx[l, d].rearrange("n i j -> i n j"), writes=["wlr"],
                               q="pool", max_dma_last_dim=8192)
                    CE = os.environ.get("CONV_ENG", "pool")
                    xrp = [fw.sb(ph, "xrp%d" % i, [128, T + 4], BF16) for i in range(2)]
                    xcb = [fw.sb(ph, "xcb%d" % i, [128, T], BF16) for i in range(2)]
                    hb = [fw.sb(ph, "hb%d" % i, [128, T], BF16) for i in range(2)]
                    rb_ = [fw.sb(ph, "rb%d" % i, [128, T], F32) for i in range(2)]
                    ibs = [fw.sb(ph, "ib%d" % i, [128, T], F32) for i in range(2)]
                    abs_ = [fw.sb(ph, "ab%d" % i, [128, T], F32) for i in range(2)]
                    xgb = fw.sb(ph, "xgb", [128, NTo], BF16)
                    for i in range(2):
                        fw.op("dve", lambda e, i=i: e.memset(xrp[i][:, 0:2], 0.0), writes=["xrp%d" % i])
                        fw.op("dve", lambda e, i=i: e.memset(xrp[i][:, T + 2:T + 4], 0.0), writes=["xrp%d" % i])
                    cw = V_CONVW + l * 40

                    dgc = fw.sb(ph, "dgc", [128, 40, 128], BF16)
                    for blk_ in range(8):
                        for j in range(5):
                            fw.op("dve", lambda e: e.tensor_scalar(
                                out=dgc[:, blk_ * 5 + j, :], in0=ident[:], scalar1=vecs[:, cw + j * 8 + blk_:cw + j * 8 + blk_ + 1],
                                scalar2=None, op0=ALU.mult), reads=["ident", "vecs"], writes=["dgc"])

                    def conv_stage(blk):
                        pb_ = blk % 2
                        xk_, ck_, dk_ = "xrp%d" % pb_, "xcb%d" % pb_, "dgc"
                        fw.dma(xrp[pb_][:, 2:T + 2], XR[blk, :, :], reads=["XR"], writes=[xk_])
                        for g8 in range(NGRP):
                            bank = 6 + (g8 % 2)
                            for j in range(5):
                                fw.op("pe", lambda e, j=j: e.matmul(
                                    pb[bank][:], lhsT=dgc[:, blk * 5 + j, :], rhs=xrp[pb_][:, j + g8 * G:j + (g8 + 1) * G],
                                    start=(j == 0), stop=(j == 4)), reads=[dk_, xk_], writes=[PB[bank]], inc=(j == 4))
                            fw.op("dve", lambda e: e.tensor_scalar(
                                out=xcb[pb_][:, g8 * G:(g8 + 1) * G], in0=pb[bank][:],
                                scalar1=vecs[:, V_CONVB + l * 8 + blk:V_CONVB + l * 8 + blk + 1], scalar2=None, op0=ALU.add),
                                reads=[PB[bank], "vecs"], writes=[ck_])

                    conv_stage(0)
                    for blk in range(8):
                        pb_ = blk % 2
                        ck_ = "xcb%d" % pb_
                        fw.dma(xgb[:], XG[blk, :, 0:NTo], reads=["XG"], writes=["xgb"])
                        for d in range(2):
                            lo = V_LRU + l * 48 + d * 24
                            r, ib, ab = rb_[d], ibs[d], abs_[d]
                            rkey, ikey, akey = "rb%d" % d, "ib%d" % d, "ab%d" % d
                            Td = NTo if d == 0 else T
                            for g8 in range(Td // G):
                                ba, bi = (g8 % 3) * 2, (g8 % 3) * 2 + 1
                                fw.op("pe", lambda e: e.matmul(
                                    pb[ba][:], lhsT=wlr[:, d * 16 + blk, :], rhs=xcb[pb_][:, g8 * G:(g8 + 1) * G], start=True, stop=True),
                                    reads=["wlr", ck_], writes=[PB[ba]])
                                fw.op("pe", lambda e: e.matmul(
                                    pb[bi][:], lhsT=wlr[:, d * 16 + 8 + blk, :], rhs=xcb[pb_][:, g8 * G:(g8 + 1) * G], start=True, stop=True),
                                    reads=["wlr", ck_], writes=[PB[bi]])
                                fw.op("act", lambda e: e.activation(
                                    out=r[:, g8 * G:(g8 + 1) * G], in_=pb[ba][:], func=AF.Sigmoid,
                                    bias=vecs[:, lo + blk:lo + blk + 1]), reads=[PB[ba], "vecs"], writes=[rkey])
                                fw.op("act", lambda e: e.activation(
                                    out=ib[:, g8 * G:(g8 + 1) * G], in_=pb[bi][:], func=AF.Sigmoid,
                                    bias=vecs[:, lo + 8 + blk:lo + 8 + blk + 1]), reads=[PB[bi], "vecs"], writes=[ikey])
                            if d == 0 and blk + 1 < 8:
                                conv_stage(blk + 1)
                            fw.op("dve", lambda e: e.tensor_tensor(out=ib[:, 0:Td], in0=ib[:, 0:Td], in1=xcb[pb_][:, 0:Td], op=ALU.mult),
                                  reads=[ikey, ck_], writes=[ikey])
                            cn = cneg[:, (l * 2 + d) * 8 + blk:(l * 2 + d) * 8 + blk + 1]
                            fw.op("act", lambda e: e.activation(out=ab[:, 0:Td], in_=r[:, 0:Td], func=AF.Exp, scale=cn),
                                  reads=[rkey, "cneg"], writes=[akey])
                            fw.op("act", lambda e: e.activation(out=r[:, 0:Td], in_=ab[:, 0:Td], func=AF.Square),
                                  reads=[akey], writes=[rkey])
                            fw.op("act", lambda e: e.activation(out=r[:, 0:Td], in_=r[:, 0:Td], func=AF.Sqrt, scale=-1.0, bias=1.0),
                                  reads=[rkey], writes=[rkey])
                            fw.op("dve", lambda e: e.tensor_tensor(out=ib[:, 0:Td], in0=ib[:, 0:Td], in1=r[:, 0:Td], op=ALU.mult),
                                  reads=[ikey, rkey], writes=[ikey])
                            if d == 0:
                                fw.op("dve", lambda e: e.tensor_tensor_scan(out=hb[0][:, 0:Td], data0=ab[:, 0:Td], data1=ib[:, 0:Td], initial=0.0,
                                                                            op0=ALU.mult, op1=ALU.add),
                                      reads=[akey, ikey], writes=["hb0"])
                            else:
                                fw.op("dve", lambda e: e.tensor_tensor_scan(out=hb[1][:, ::-1], data0=ab[:, ::-1], data1=ib[:, ::-1],
                                                                            initial=0.0, op0=ALU.mult, op1=ALU.add),
                                      reads=[akey, ikey], writes=["hb1"])
                        fw.op("pool", lambda e: e.tensor_tensor(out=hb[0][:, 0:NTo], in0=hb[0][:, 0:NTo], in1=hb[1][:, 0:NTo],
                                                                op=ALU.add), reads=["hb0", "hb1"], writes=["hb0"])
                        fw.op("pool", lambda e: e.tensor_tensor(out=xgb[:], in0=hb[0][:, 0:NTo], in1=xgb[:], op=ALU.mult),
                              reads=["hb0", "xgb"], writes=["xgb"])
                        fw.dma(YL[blk, :, 0:NTo], xgb[:], reads=["xgb"], writes=["YL"])
                fw.barrier()

                ck("B%d" % l)
                with ExitStack() as ph:
                    wq = fw.sb(ph, "wq", [128, 8, 2048], BF16)
                    load_w(wq[:, :, 0:1024], w_in[l, :, OQ:OQ + 1024], "wq")
                    ctab = fw.sb(ph, "ctabC", [128, NTo], F32)
                    stab = fw.sb(ph, "stabC", [128, NTo], F32)
                    fw.dma(ctab[:], cos_in[:, 0:NTo], writes=["ctab"])
                    fw.dma(stab[:], sin_in[:, 0:NTo], writes=["stab"])
                    srcv = wq[:, :, 0:1024].rearrange("p k (h a b f) -> p k h a b f", h=8, a=2, b=2)
                    dstv = wq[:, :, 1024:2048].rearrange("p k (h a b f) -> p k h a b f", h=8, a=2, b=2)
                    for k in range(8):
                        for bb in range(2):
                            for a in range(2):
                                fw.op("dve", lambda e, k=k, bb=bb, a=a: e.tensor_copy(
                                    out=dstv[:, k, :, a, bb, :], in_=srcv[:, k, :, a, 1 - bb, :]),
                                    reads=["wq"], writes=["wqr"])
                    hC = [fw.sb(ph, "hC%d" % i, [128, 8 * G], BF16) for i in range(2)]
                    qT = fw.sb(ph, "qT", [128, 8, G], BF16)
                    sqk = fw.sb(ph, "sqkC", [128, G], BF16)
                    rk = fw.sb(ph, "rkC", [128, G], F32)
                    t1 = fw.sb(ph, "t1C", [128, G], F32)
                    t2 = fw.sb(ph, "t2C", [128, G], F32)
                    PTb = [fw.sb(ph, "PT%d" % i, [128, G], BF16) for i in range(3)]
                    rec = fw.sb(ph, "rec", [128, G], F32)
                    aos = [fw.sb(ph, "aos%d" % i, [128, 8, G], BF16) for i in range(2)]
                    sc = 1.0 / math.sqrt(128.0)
                    for g in range(min(NGo, int(os.environ.get('C1_NG', '99')))):
                        b = g % 2
                        hk = "hC%d" % b
                        fw.dma(hC[b][:].rearrange("p (c t) -> p c t", c=8),
                               HT[:, :, g * G:(g + 1) * G].rearrange("c p t -> p c t"), reads=["HT_g%d" % g], writes=[hk])
                        for h in range(8):
                            proj(0, wq, h * 128, 128, hC[b], hk, "wq")
                            proj(1, wq, 1024 + h * 128, 128, hC[b], hk, "wqr")
                            fw.op("act", lambda e: e.activation(out=sqk[:], in_=pb[0][:], func=AF.Square),
                                  reads=[PB[0]], writes=["sqk"])
                            fw.op("pe", lambda e: e.matmul(pb[2][:], lhsT=ones_bf[:], rhs=sqk[:], start=True, stop=True),
                                  reads=["sqk", "ones_bf"], writes=[PB[2]])
                            fw.op("act", lambda e: e.activation(out=rk[:], in_=pb[2][:], func=AF.Ln, scale=1.0 / 128, bias=epsc[:]),
                                  reads=[PB[2], "epsc"], writes=["rk"])
                            fw.op("act", lambda e: e.activation(out=rk[:], in_=rk[:], func=AF.Exp, scale=-0.5),
                                  reads=["rk"], writes=["rk"])
                            fw.op("dve", lambda e: e.scalar_tensor_tensor(out=t1[:], in0=pb[0][:], scalar=vb(V_QG + l * 4 + 0),
                                                                          in1=ctab[:, g * G:(g + 1) * G], op0=ALU.mult, op1=ALU.mult),
                                  reads=[PB[0], "vecs", "ctab"], writes=["t1"])
                            fw.op("dve", lambda e: e.scalar_tensor_tensor(out=t2[:], in0=pb[1][:], scalar=vb(V_QG + l * 4 + 1),
                                                                          in1=stab[:, g * G:(g + 1) * G], op0=ALU.mult, op1=ALU.mult),
                                  reads=[PB[1], "vecs", "stab"], writes=["t2"])
                            fw.op("dve", lambda e: e.tensor_tensor(out=t1[:], in0=t1[:], in1=t2[:], op=ALU.add),
                                  reads=["t1", "t2"], writes=["t1"])
                            fw.op("dve", lambda e, h=h: e.tensor_tensor(out=qT[:, h, :], in0=t1[:], in1=rk[:], op=ALU.mult),
                                  reads=["t1", "rk"], writes=["qT%d" % h])
                        for h in range(8):
                            kv = h // 4
                            bo, bs = 4 + (h % 2) * 2, 5 + (h % 2) * 2
                            NKT = T // 128

                            def emit_st(kt):
                                bst = kt % 3
                                fw.op("pe", lambda e: e.matmul(
                                    pb[bst][:], lhsT=KT[:, kv, kt * 128:(kt + 1) * 128], rhs=qT[:, h, :], start=True, stop=True),
                                    reads=["KT", "qT%d" % h], writes=[PB[bst]])
                                fw.op("act", lambda e: e.activation(out=PTb[kt % 3][:], in_=pb[bst][:], func=AF.Exp, scale=sc),
                                      reads=[PB[bst]], writes=["PT%d" % (kt % 3)])

                            emit_st(0)
                            emit_st(1)
                            for kt in range(NKT):
                                if kt + 2 < NKT:
                                    emit_st(kt + 2)
                                ptk = "PT%d" % (kt % 3)
                                last = kt == NKT - 1
                                fw.op("pe", lambda e: e.matmul(
                                    pb[bo][:], lhsT=Vt[:, kt, kv * 128:(kv + 1) * 128], rhs=PTb[kt % 3][:], start=(kt == 0), stop=last),
                                    reads=["V", ptk], writes=[PB[bo]], inc=False)
                                fw.op("pe", lambda e: e.matmul(
                                    pb[bs][:], lhsT=ones_bf[:], rhs=PTb[kt % 3][:], start=(kt == 0), stop=last),
                                    reads=["ones_bf", ptk], writes=[PB[bs]], inc=True)
                            fw.op("dve", lambda e, bs=bs: e.reciprocal(out=rec[:], in_=pb[bs][:]), reads=[PB[bs]], writes=["rec"])
                            fw.op("dve", lambda e, bo=bo, h=h: e.tensor_tensor(out=aos[b][:, h, :], in0=pb[bo][:], in1=rec[:], op=ALU.mult),
                                  reads=[PB[bo], "rec"], writes=["aos%d" % b])
                        fw.dma(AO[:, :, g * G:(g + 1) * G].rearrange("c p t -> p c t"), aos[b][:],
                               reads=["aos%d" % b], writes=["AO"])
                fw.barrier()
                kvst.__exit__(None, None, None)
                open_kv.remove(kvst)

                ck("C1%d" % l)
                with ExitStack() as ph:
                    wg_ = fw.sb(ph, "wgt", [128, 8, 2048], BF16)
                    load_w(wg_, w_in[l, :, OGA:OGA + 2048], "wgt")
                    woa = fw.sb(ph, "woa", [128, 8, D], BF16)
                    wol = fw.sb(ph, "wol", [128, 8, D], BF16)
                    wo = fw.sb(ph, "wo", [128, 8, D], BF16)
                    load_w(woa, w_oa[l], "woa")
                    load_w(wol, w_ol[l], "wol")
                    load_w(wo, w_out[l], "wo")
                    hC = [fw.sb(ph, "hD%d" % i, [128, 8 * G], BF16) for i in range(2)]
                    aoC = [fw.sb(ph, "aoD%d" % i, [128, 8 * G], BF16) for i in range(2)]
                    ylC = [fw.sb(ph, "ylD%d" % i, [128, 8 * G], BF16) for i in range(2)]
                    xC = [fw.sb(ph, "xD%d" % i, [128, 8 * G], F32) for i in range(2)]
                    mg = fw.sb(ph, "mg", [128, 8 * G], BF16)
                    sg = [fw.sb(ph, "sg%d" % i, [128, G], F32) for i in range(2)]
                    m1 = [fw.sb(ph, "m1%d" % i, [128, G], F32) for i in range(2)]
                    for g in range(NGo):
                        b = g % 2
                        sl = lambda A: A[:, :, g * G:(g + 1) * G].rearrange("c p t -> p c t")
                        v3 = lambda t_: t_[:].rearrange("p (c t) -> p c t", c=8)
                        fw.dma(v3(hC[b]), sl(HT), reads=["HT_g%d" % g], writes=["hD%d" % b])
                        fw.dma(v3(aoC[b]), sl(AO), reads=["AO"], writes=["aoD%d" % b])
                        fw.dma(v3(ylC[b]), sl(YL), reads=["YL"], writes=["ylD%d" % b])
                        fw.dma(v3(xC[b]), sl(XT), reads=["XT_g%d" % g], writes=["xD%d" % b])
                        for c in range(8):
                            o4 = (c % 2) * 4
                            proj(o4 + 0, woa, c * 128, 128, aoC[b], "aoD%d" % b, "woa")
                            proj(o4 + 1, wg_, c * 128, 128, hC[b], "hD%d" % b, "wgt")
                            proj(o4 + 2, wol, c * 128, 128, ylC[b], "ylD%d" % b, "wol")
                            proj(o4 + 3, wg_, 1024 + c * 128, 128, hC[b], "hD%d" % b, "wgt")
                            fw.op("act", lambda e: e.activation(out=sg[0][:], in_=pb[o4 + 1][:], func=AF.Sigmoid), reads=[PB[o4 + 1]], writes=["sg0"])
                            fw.op("act", lambda e: e.activation(out=sg[1][:], in_=pb[o4 + 3][:], func=AF.Sigmoid), reads=[PB[o4 + 3]], writes=["sg1"])
                            fw.op("dve", lambda e: e.tensor_tensor(out=m1[0][:], in0=pb[o4 + 0][:], in1=sg[0][:], op=ALU.mult),
                                  reads=[PB[o4 + 0], "sg0"], writes=["m10"])
                            fw.op("dve", lambda e: e.tensor_tensor(out=m1[1][:], in0=pb[o4 + 2][:], in1=sg[1][:], op=ALU.mult),
                                  reads=[PB[o4 + 2], "sg1"], writes=["m11"])
                            fw.op("dve", lambda e, c=c: e.tensor_tensor(out=mg[:, c * G:(c + 1) * G], in0=m1[0][:], in1=m1[1][:], op=ALU.add),
                                  reads=["m10", "m11"], writes=["mg"])
                        g1o = l * 48 + 16
                        for c in range(8):
                            bank = 4 + (c % 2)
                            proj(bank, wo, c * 128, 128, mg, "mg", "wo")
                            fw.op("dve", lambda e, c=c, bank=bank: e.scalar_tensor_tensor(
                                out=xC[b][:, c * G:(c + 1) * G], in0=pb[bank][:], scalar=modT[:, g1o + c:g1o + c + 1],
                                in1=xC[b][:, c * G:(c + 1) * G], op0=ALU.mult, op1=ALU.add),
                                reads=[PB[bank], "modT", "xD%d" % b], writes=["xD%d" % b])
                        fw.dma(sl(X1T), v3(xC[b]), reads=["xD%d" % b], writes=["X1T_g%d" % g])
                fw.barrier()

                ck("C2%d" % l)
                if l == 0:
                    with ExitStack() as ph:
                        wg = fw.sb(ph, "fwg", [128, 8, DFF], BF16)
                        wu = fw.sb(ph, "fwu", [128, 8, DFF], BF16)
                        wd = fw.sb(ph, "fwd", [128, NF0, D], BF16)
                        load_w(wg, f_wg[0], "fwg")
                        load_w(wu, f_wu[0], "fwu")
                        load_w(wd, f_wd[0], "fwd", kchunks=NF0)
                        xD = [fw.sb(ph, "xE%d" % i, [128, 8 * G], F32) for i in range(2)]
                        hD = fw.sb(ph, "hE", [128, 8 * G], BF16)
                        tmpD = fw.sb(ph, "tmpE", [128, 2 * G], F32)
                        rsD = fw.sb(ph, "rsE", [128, G], F32)
                        act = fw.sb(ph, "actE", [128, NF0 * G], BF16)
                        sil = [fw.sb(ph, "sil%d" % i, [128, G], F32) for i in range(2)]
                        for g in range(NGo):
                            b = g % 2
                            sl = lambda A: A[:, :, g * G:(g + 1) * G].rearrange("c p t -> p c t")
                            v3 = lambda t_: t_[:].rearrange("p (c t) -> p c t", c=8)
                            if g == 0:
                                fw.dma(v3(xD[b]), sl(X1T), reads=["X1T_g%d" % g], writes=["xE%d" % b])
                                norm_mod(xD[b], "xE%d" % b, l, 1, hD, "hE", tmpD, "tmpE", hD, "hE", rsD, "rsE", 0)
                            for f in range(NF0):
                                bg, bu = 1 + (f % 2) * 2, 2 + (f % 2) * 2
                                proj(bg, wg, f * 128, 128, hD, "hE", "fwg")
                                proj(bu, wu, f * 128, 128, hD, "hE", "fwu")
                                fw.op("act", lambda e, f=f, bg=bg: e.activation(out=sil[f % 2][:], in_=pb[bg][:], func=AF.Silu),
                                      reads=[PB[bg]], writes=["sil%d" % (f % 2)])
                                fw.op("dve", lambda e, f=f, bu=bu: e.tensor_tensor(out=act[:, f * G:(f + 1) * G], in0=pb[bu][:],
                                                                                   in1=sil[f % 2][:], op=ALU.mult),
                                      reads=[PB[bu], "sil%d" % (f % 2)], writes=["actE%d" % f])
                            g2o = l * 48 + 40
                            if g + 1 < NGo:
                                b1 = (g + 1) % 2
                                fw.dma(v3(xD[b1]), X1T[:, :, (g + 1) * G:(g + 2) * G].rearrange("c p t -> p c t"),
                                       reads=["X1T_g%d" % (g + 1)], writes=["xE%d" % b1])
                                norm_mod(xD[b1], "xE%d" % b1, l, 1, hD, "hE", tmpD, "tmpE", hD, "hE", rsD, "rsE", 0, part=1)
                            for c in range(8):
                                if c == 3 and g + 1 < NGo:
                                    norm_mod(xD[b1], "xE%d" % b1, l, 1, hD, "hE", tmpD, "tmpE", hD, "hE", rsD, "rsE", 0, part=2)
                                bank = 5 + (c % 2)
                                for f in range(NF0):
                                    fw.op("pe", lambda e, f=f, c=c, bank=bank: e.matmul(
                                        pb[bank][:], lhsT=wd[:, f, c * 128:(c + 1) * 128], rhs=act[:, f * G:(f + 1) * G],
                                        start=(f == 0), stop=(f == NF0 - 1)),
                                        reads=["fwd", "actE%d" % f], writes=[PB[bank]], inc=(f == NF0 - 1))
                                fw.op("dve", lambda e, c=c, bank=bank: e.scalar_tensor_tensor(
                                    out=xD[b][:, c * G:(c + 1) * G], in0=pb[bank][:], scalar=modT[:, g2o + c:g2o + c + 1],
                                    in1=xD[b][:, c * G:(c + 1) * G], op0=ALU.mult, op1=ALU.add),
                                    reads=[PB[bank], "modT", "xE%d" % b], writes=["xE%d" % b])
                            fw.dma(sl(XT), v3(xD[b]), reads=["xE%d" % b], writes=["XT_g%d" % g])
                    fw.barrier()
                    ck("D0")
                else:
                    with ExitStack() as ph:
                        NTk = NTo // 128
                        acc = fw.sb(ph, "acc", [128, 8, NTo], F32)
                        hM = fw.sb(ph, "hM", [128, 8, NTo], BF16)
                        comb = fw.sb(ph, "comb", [128, NTk, 8], F32)
                        rw = fw.sb(ph, "rw", [128, 8, 8], F32)
                        rwh = fw.sb(ph, "rwh", [128, 8, 8], BF16)
                        rwl = fw.sb(ph, "rwl", [128, 8, 8], BF16)
                        for k in range(8):
                            fw.dma(rw[:, k, :], r_w[0, k * 128:(k + 1) * 128, :], writes=["rw"])
                        fw.op("dve", lambda e: e.tensor_copy(out=rwh[:], in_=rw[:]), reads=["rw"], writes=["rwh"])
                        fw.op("dve", lambda e: e.tensor_tensor(out=rw[:], in0=rw[:], in1=rwh[:], op=ALU.subtract), reads=["rw", "rwh"], writes=["rw"])
                        fw.op("dve", lambda e: e.tensor_copy(out=rwl[:], in_=rw[:]), reads=["rw"], writes=["rwl"])
                        with ExitStack() as ph2:
                            xD = fw.sb(ph2, "xM", [128, 8 * G], F32)
                            hD = fw.sb(ph2, "hMg", [128, 8 * G], BF16)
                            hF = fw.sb(ph2, "hMf", [128, 8 * G], F32)
                            hFh = fw.sb(ph2, "hMfh", [128, 8 * G], BF16)
                            hFl = fw.sb(ph2, "hMfl", [128, 8 * G], BF16)
                            tmpD = fw.sb(ph2, "tmpM", [128, 2 * G], F32)
                            rsD = fw.sb(ph2, "rsM", [128, G], F32)
                            lg = fw.sb(ph2, "lg", [128, 8], F32)
                            m8 = fw.sb(ph2, "m8", [128, 8], F32)
                            ex = fw.sb(ph2, "ex", [128, 8], F32)
                            msk = fw.sb(ph2, "msk", [128, 8], F32)
                            nm1 = fw.sb(ph2, "nm1", [128, 1], F32)
                            ssum = fw.sb(ph2, "ssum", [128, 1], F32)
                            for g in range(NGo):
                                sl = lambda A: A[:, :, g * G:(g + 1) * G].rearrange("c p t -> p c t")
                                v3 = lambda t_: t_[:].rearrange("p (c t) -> p c t", c=8)
                                fw.dma(v3(xD), sl(X1T), reads=["X1T_g%d" % g], writes=["xM"])
                                norm_mod(xD, "xM", l, 1, hD, "hMg", tmpD, "tmpM", hD, "hMg", rsD, "rsM", 0, hf=hF, hfkey="hMf")
                                fw.op("act", lambda e, g=g: e.activation(out=acc[:, :, g * G:(g + 1) * G], in_=v3(xD), func=AF.Copy),
                                      reads=["xM"], writes=["acc_g%d" % g])
                                fw.op("dve", lambda e, g=g: e.tensor_copy(out=hM[:, :, g * G:(g + 1) * G], in_=v3(hD)),
                                      reads=["hMg"], writes=["hM"])
                                fw.op("act", lambda e: e.activation(out=hFh[:], in_=hF[:], func=AF.Copy), reads=["hMf"], writes=["hMfh"])
                                fw.op("dve", lambda e: e.tensor_tensor(out=hF[:], in0=hF[:], in1=hFh[:], op=ALU.subtract),
                                      reads=["hMf", "hMfh"], writes=["hMf"])
                                fw.op("act", lambda e: e.activation(out=hFl[:], in_=hF[:], func=AF.Copy), reads=["hMf"], writes=["hMfl"])
                                for j in range(4):
                                    tk = g * 4 + j
                                    n_ = 0
                                    for c in range(8):
                                        for (ha, hk_, wa, wk_) in ((hFh, "hMfh", rwh, "rwh"), (hFh, "hMfh", rwl, "rwl"), (hFl, "hMfl", rwh, "rwh")):
                                            fw.op("pe", lambda e, c=c, j=j, ha=ha, wa=wa, n_=n_: e.matmul(
                                                pb[1][:, 0:8], lhsT=ha[:, c * G + j * 128:c * G + (j + 1) * 128], rhs=wa[:, c, :],
                                                start=(n_ == 0), stop=(n_ == 23)), reads=[hk_, wk_], writes=[PB[1]], inc=(n_ == 23))
                                            n_ += 1
                                    fw.op("dve", lambda e: e.tensor_tensor(out=lg[:], in0=pb[1][:, 0:8], in1=vecs[:, V_RB:V_RB + 8], op=ALU.add),
                                          reads=[PB[1], "vecs"], writes=["lg"])
                                    fw.op("dve", lambda e: e.max(out=m8[:], in_=lg[:]), reads=["lg"], writes=["m8"])
                                    fw.op("dve", lambda e: e.tensor_scalar(out=msk[:], in0=lg[:], scalar1=m8[:, 1:2], scalar2=None, op0=ALU.is_ge),
                                          reads=["lg", "m8"], writes=["msk"])
                                    fw.op("dve", lambda e: e.tensor_scalar_mul(out=nm1[:], in0=m8[:, 0:1], scalar1=-1.0),
                                          reads=["m8"], writes=["nm1"])
                                    fw.op("act", lambda e: e.activation(out=ex[:], in_=lg[:], func=AF.Exp, bias=nm1[:]),
                                          reads=["lg", "nm1"], writes=["ex"])
                                    fw.op("dve", lambda e: e.tensor_tensor(out=ex[:], in0=ex[:], in1=msk[:], op=ALU.mult),
                                          reads=["ex", "msk"], writes=["ex"])
                                    fw.op("dve", lambda e: e.reduce_sum(out=ssum[:], in_=ex[:], axis=mybir.AxisListType.X),
                                          reads=["ex"], writes=["ssum"])
                                    fw.op("dve", lambda e: e.reciprocal(out=ssum[:], in_=ssum[:]), reads=["ssum"], writes=["ssum"])
                                    fw.op("dve", lambda e, tk=tk: e.tensor_scalar(out=comb[:, tk, :], in0=ex[:], scalar1=ssum[:, 0:1], scalar2=None,
                                                                                   op0=ALU.mult), reads=["ex", "ssum"], writes=["comb"])
                        fw.barrier()
                        NCH = 7
                        FC = EFF // NCH
                        wgc = [fw.sb(ph, "mwg%d" % i, [128, 8, FC], BF16) for i in range(2)]
                        wuc = [fw.sb(ph, "mwu%d" % i, [128, 8, FC], BF16) for i in range(2)]
                        wdc = [fw.sb(ph, "mwd%d" % i, [128, 4, D], BF16) for i in range(2)]
                        cbc = [fw.sb(ph, "cbc%d" % i, [128, NTo], F32) for i in range(2)]
                        dg = [fw.sb(ph, "dg%d" % i, [128, 128], F32) for i in range(2)]
                        dgh = [fw.sb(ph, "dgh%d" % i, [128, 128], BF16) for i in range(2)]
                        dgl = [fw.sb(ph, "dgl%d" % i, [128, 128], BF16) for i in range(2)]
                        actM = [fw.sb(ph, "actM%d" % i, [128, 4 * G], BF16) for i in range(2)]
                        sil = [fw.sb(ph, "silM%d" % i, [128, G], F32) for i in range(2)]
                        tm = [fw.sb(ph, "tmM%d" % i, [128, G], F32) for i in range(2)]
                        g2o = l * 48 + 40
                        it = 0
                        def build_cbc(ex_):
                            cb = cbc[ex_ % 2]
                            cbk = "cbc%d" % (ex_ % 2)
                            for tk in range(NTk):
                                d_ = dg[tk % 2]
                                fw.op("dve", lambda e: e.tensor_scalar(
                                    out=d_[:], in0=ident[:], scalar1=comb[:, tk, ex_:ex_ + 1], scalar2=None, op0=ALU.mult),
                                    reads=["ident", "comb"], writes=["dg%d" % (tk % 2)])
                                bank = 6 + (tk % 2)
                                dh_, dl_ = dgh[tk % 2], dgl[tk % 2]
                                fw.op("dve", lambda e: e.tensor_copy(out=dh_[:], in_=d_[:]), reads=["dg%d" % (tk % 2)], writes=["dgh%d" % (tk % 2)])
                                fw.op("dve", lambda e: e.tensor_tensor(out=d_[:], in0=d_[:], in1=dh_[:], op=ALU.subtract),
                                      reads=["dg%d" % (tk % 2), "dgh%d" % (tk % 2)], writes=["dg%d" % (tk % 2)])
                                fw.op("dve", lambda e: e.tensor_copy(out=dl_[:], in_=d_[:]), reads=["dg%d" % (tk % 2)], writes=["dgl%d" % (tk % 2)])
                                fw.op("pe", lambda e: e.matmul(pb[bank][:, 0:128], lhsT=ones_bf[:], rhs=dh_[:], start=True, stop=False),
                                      reads=["ones_bf", "dgh%d" % (tk % 2)], writes=[PB[bank]], inc=False)
                                fw.op("pe", lambda e: e.matmul(pb[bank][:, 0:128], lhsT=ones_bf[:], rhs=dl_[:], start=False, stop=True),
                                      reads=["ones_bf", "dgl%d" % (tk % 2)], writes=[PB[bank]])
                                fw.op("act", lambda e: e.activation(out=cb[:, tk * 128:(tk + 1) * 128], in_=pb[bank][:, 0:128],
                                                                    func=AF.Copy), reads=[PB[bank]], writes=[cbk])

                        build_cbc(0)
                        for ex_ in range(NE):
                            cb = cbc[ex_ % 2]
                            cbk = "cbc%d" % (ex_ % 2)
                            for ch in range(NCH):
                                if ch == 2 and ex_ + 1 < NE:
                                    build_cbc(ex_ + 1)
                                wb = it % 2
                                it += 1
                                kg_, ku_, kd_ = "mwg%d" % wb, "mwu%d" % wb, "mwd%d" % wb
                                load_w(wgc[wb], m_wg[0, ex_, :, ch * FC:(ch + 1) * FC], kg_)
                                load_w(wuc[wb], m_wu[0, ex_, :, ch * FC:(ch + 1) * FC], ku_)
                                load_w(wdc[wb], m_wd[0, ex_, ch * FC:(ch + 1) * FC, :], kd_, kchunks=4)
                                for g in range(NGo):
                                    am = actM[g % 2]
                                    amk = "actM%d" % (g % 2)
                                    for f in range(4):
                                        bg, bu = (f % 2) * 2, 1 + (f % 2) * 2
                                        for c in range(8):
                                            fw.op("pe", lambda e, c=c, f=f, g=g, wb=wb, bg=bg: e.matmul(
                                                pb[bg][:], lhsT=wgc[wb][:, c, f * 128:(f + 1) * 128], rhs=hM[:, c, g * G:(g + 1) * G],
                                                start=(c == 0), stop=(c == 7)), reads=[kg_, "hM"], writes=[PB[bg]], inc=(c == 7))
                                        for c in range(8):
                                            fw.op("pe", lambda e, c=c, f=f, g=g, wb=wb, bu=bu: e.matmul(
                                                pb[bu][:], lhsT=wuc[wb][:, c, f * 128:(f + 1) * 128], rhs=hM[:, c, g * G:(g + 1) * G],
                                                start=(c == 0), stop=(c == 7)), reads=[ku_, "hM"], writes=[PB[bu]], inc=(c == 7))
                                        fw.op("act", lambda e, f=f, bg=bg: e.activation(out=sil[f % 2][:], in_=pb[bg][:], func=AF.Silu),
                                              reads=[PB[bg]], writes=["silM%d" % (f % 2)])
                                        fw.op("dve", lambda e, f=f, bu=bu, am=am: e.tensor_tensor(out=am[:, f * G:(f + 1) * G], in0=pb[bu][:],
                                                                                                 in1=sil[f % 2][:], op=ALU.mult),
                                              reads=[PB[bu], "silM%d" % (f % 2)], writes=[amk + "_%d" % f])
                                    for c in range(8):
                                        bank = 4 + (c % 4)
                                        for f in range(4):
                                            fw.op("pe", lambda e, c=c, f=f, wb=wb, bank=bank, am=am: e.matmul(
                                                pb[bank][:], lhsT=wdc[wb][:, f, c * 128:(c + 1) * 128], rhs=am[:, f * G:(f + 1) * G],
                                                start=(f == 0), stop=(f == 3)), reads=[kd_, amk + "_%d" % f], writes=[PB[bank]], inc=(f == 3))
                                        fw.op("dve", lambda e, c=c, bank=bank, g=g, cb=cb: e.tensor_tensor(
                                            out=tm[c % 2][:], in0=pb[bank][:], in1=cb[:, g * G:(g + 1) * G], op=ALU.mult),
                                            reads=[PB[bank], cbk], writes=["tmM%d" % (c % 2)])
                                        fw.op("dve", lambda e, c=c, g=g: e.scalar_tensor_tensor(
                                            out=acc[:, c, g * G:(g + 1) * G], in0=tm[c % 2][:], scalar=modT[:, g2o + c:g2o + c + 1],
                                            in1=acc[:, c, g * G:(g + 1) * G], op0=ALU.mult, op1=ALU.add),
                                            reads=["tmM%d" % (c % 2), "modT", "acc_g%d" % g], writes=["acc_g%d" % g])
                        for g in range(NGo):
                            fw.dma(XT[:, :, g * G:(g + 1) * G].rearrange("c p t -> p c t"), acc[:, :, g * G:(g + 1) * G],
                                   reads=["acc_g%d" % g], writes=["XT_g%d" % g])
                    fw.barrier()

            NGf = out_rows // G
            with ExitStack() as ph:
                xF = [fw.sb(ph, "xF%d" % i, [128, 8 * G], F32) for i in range(2)]
                sqF = fw.sb(ph, "sqF", [128, 8 * G], BF16)
                rsF = fw.sb(ph, "rsF", [128, G], F32)
                yF = fw.sb(ph, "yF", [128, 8 * G], F32)
                oF = [fw.sb(ph, "oF%d" % i, [128, D], F32) for i in range(2)]
                oi = 0
                for g in range(NGf):
                    b = g % 2
                    xk = "xF%d" % b
                    fw.dma(xF[b][:].rearrange("p (c t) -> p c t", c=8), XT[:, :, g * G:(g + 1) * G].rearrange("c p t -> p c t"),
                           reads=["XT_g%d" % g], writes=[xk])
                    fw.op("act", lambda e, b=b: e.activation(out=sqF[:], in_=xF[b][:], func=AF.Square), reads=[xk], writes=["sqF"])
                    for c in range(8):
                        fw.op("pe", lambda e, c=c: e.matmul(pb[0][:], lhsT=ones_bf[:], rhs=sqF[:, c * G:(c + 1) * G], start=(c == 0), stop=(c == 7)),
                              reads=["ones_bf", "sqF"], writes=[PB[0]], inc=(c == 7))
                    fw.op("act", lambda e: e.activation(out=rsF[:], in_=pb[0][:], func=AF.Ln, scale=1.0 / D, bias=epsc[:]),
                          reads=[PB[0], "epsc"], writes=["rsF"])
                    fw.op("act", lambda e: e.activation(out=rsF[:], in_=rsF[:], func=AF.Exp, scale=-0.5), reads=["rsF"], writes=["rsF"])
                    for c in range(8):
                        fw.op("dve", lambda e, c=c, b=b: e.scalar_tensor_tensor(
                            out=yF[:, c * G:(c + 1) * G], in0=xF[b][:, c * G:(c + 1) * G], scalar=vecs[:, V_FG + c:V_FG + c + 1],
                            in1=rsF[:], op0=ALU.mult, op1=ALU.mult), reads=[xk, "vecs", "rsF"], writes=["yF%d" % c])
                    for j in range(4):
                        ob = oi % 2
                        oi += 1
                        for half in range(2):
                            bank = 1 + half + 2 * ob
                            for cc in range(4):
                                c = half * 4 + cc
                                fw.op("pe", lambda e, c=c, cc=cc, j=j, bank=bank: e.transpose(
                                    pb[bank][:, cc * 128:(cc + 1) * 128], yF[:, c * G + j * 128:c * G + (j + 1) * 128], ident[:]),
                                    reads=["yF%d" % c, "ident"], writes=[PB[bank]], inc=(cc == 3))
                            if half == 0:
                                fw.op("act", lambda e, ob=ob, bank=bank: e.activation(out=oF[ob][:, 0:512], in_=pb[bank][:], func=AF.Copy),
                                      reads=[PB[bank]], writes=["oF%d" % ob])
                            else:
                                fw.op("dve", lambda e, ob=ob, bank=bank: e.tensor_copy(out=oF[ob][:, 512:1024], in_=pb[bank][:]),
                                      reads=[PB[bank]], writes=["oF%d" % ob])
                        r0 = g * G + j * 128
                        fw.dma(out_ap[r0:r0 + 128, :], oF[ob][:], reads=["oF%d" % ob], writes=["out_%d" % (r0 // 128)])
                fw.wait_all("sp", ["out_%d" % i for i in range(out_rows // 128)])
        except _Stop:
            fw.barrier()
            for k_ in open_kv:
                k_.__exit__(None, None, None)
        fw.finish()
    return nc


def _pmaj(v, n):
    return np.ascontiguousarray(np.asarray(v, np.float32).reshape(n, 128).T)


def _rope_tables():
    pos = np.arange(T)
    row = (pos // 64).astype(np.float32)
    col = (pos % 64).astype(np.float32)
    n_freq = 32
    inv_freq = np.exp(-math.log(10000.0) * (2.0 * np.arange(n_freq, dtype=np.float32) / 64.0)).astype(np.float32)
    cosT = np.zeros((128, T), np.float32)
    sinT = np.zeros((128, T), np.float32)
    for d in range(128):
        axis, half, f = d // 64, (d % 64) // 32, d % 32
        ang = (row if axis == 0 else col) * inv_freq[f]
        cosT[d] = np.cos(ang)
        sinT[d] = np.sin(ang) * (-1.0 if half == 0 else 1.0)
    return cosT, sinT


_NC_CACHE = {}


def _get_nc(n_layers, debug=False):
    key = (n_layers, debug)
    if key not in _NC_CACHE:
        _NC_CACHE[key] = build_nc(n_layers, debug)
    return _NC_CACHE[key]


def make_in_maps(inputs, n_layers=2):
    f = lambda k: np.asarray(inputs[k], np.float32)
    x, c = f("x"), f("c")
    cosT, sinT = _rope_tables()
    partner = np.array([d + 32 if (d % 64) < 32 else d - 32 for d in range(128)])
    in_maps = []
    shared = {}
    for k in ["w_mod", "w_in", "w_o_attn", "w_o_lru", "w_out", "ffn_w_gate", "ffn_w_up", "ffn_w_down"]:
        shared[k] = np.ascontiguousarray(f(k))
    if n_layers > 1:
        for k in ["router_w", "moe_w_gate", "moe_w_up", "moe_w_down"]:
            shared[k] = np.ascontiguousarray(f(k))
    lwa, lwx = f("lru_w_a"), f("lru_w_x")
    for core in range(8):
        bi, odd = core // 2, core % 2
        vecs = np.zeros((128, NV), np.float32)
        for l in range(2):
            vecs[:, V_BMOD + l * 48:V_BMOD + (l + 1) * 48] = _pmaj(f("b_mod")[l], 48)
            qg, kg = f("q_norm_gain")[l], f("k_norm_gain")[l]
            vecs[:, V_QG + l * 4 + 0] = qg
            vecs[:, V_QG + l * 4 + 1] = qg[partner]
            vecs[:, V_QG + l * 4 + 2] = kg
            vecs[:, V_QG + l * 4 + 3] = kg[partner]
            cw = f("conv_w")[l]
            taps = np.zeros((5, 1024), np.float32)
            if not odd:
                taps[0:4] = cw
            else:
                taps[1:5] = cw[::-1]
            for j in range(5):
                vecs[:, V_CONVW + l * 40 + j * 8:V_CONVW + l * 40 + (j + 1) * 8] = _pmaj(taps[j], 8)
            vecs[:, V_CONVB + l * 8:V_CONVB + (l + 1) * 8] = _pmaj(f("conv_b")[l], 8)
            for d in range(2):
                sd = d if not odd else 1 - d
                o = V_LRU + l * 48 + d * 24
                vecs[:, o:o + 8] = _pmaj(f("lru_b_a")[l, sd], 8)
                vecs[:, o + 8:o + 16] = _pmaj(f("lru_b_x")[l, sd], 8)
                vecs[:, o + 16:o + 24] = _pmaj(f("lru_lambda")[l, sd], 8)
        vecs[:, V_FG:V_FG + 8] = _pmaj(f("final_gain"), 8)
        vecs[:, V_RB:V_RB + 8] = np.broadcast_to(f("router_b")[0][None, :], (128, 8))
        vecs[:, V_C:V_C + 8] = _pmaj(c[bi], 8)
        xs = x[bi]
        m = dict(shared)
        if odd:
            m["x"] = np.ascontiguousarray(xs[::-1])
            m["cosT"] = np.ascontiguousarray(cosT[:, ::-1])
            m["sinT"] = np.ascontiguousarray(sinT[:, ::-1])
            m["lru_wa"] = np.ascontiguousarray(lwa[:, ::-1])
            m["lru_wx"] = np.ascontiguousarray(lwx[:, ::-1])
        else:
            m["x"] = np.ascontiguousarray(xs)
            m["cosT"] = cosT
            m["sinT"] = sinT
            m["lru_wa"] = np.ascontiguousarray(lwa)
            m["lru_wx"] = np.ascontiguousarray(lwx)
        m["vecs"] = vecs
        in_maps.append(m)
    return in_maps


def kernel(**inputs):
    nc = _get_nc(2)
    in_maps = make_in_maps(inputs, 2)
    res = run_bass_kernel_spmd(nc, in_maps, core_ids=list(range(8)))
    out = np.zeros((4, T, D), np.float32)
    for core in range(8):
        bi, odd = core // 2, core % 2
        o = np.asarray(res.results[core]["out"], np.float32)
        if odd:
            out[bi, T // 2:] = o[::-1]
        else:
            out[bi, :T // 2] = o
    return out
```

```python
import math
import os
_PARTS = os.environ.get('PA_PARTS', 'nkvrg')
import numpy as np
import concourse.bass as bass
import concourse.mybir as mybir
from concourse.bass_utils import run_bass_kernel_spmd
from contextlib import ExitStack

F32 = mybir.dt.float32
BF16 = mybir.dt.bfloat16
ALU = mybir.AluOpType
AF = mybir.ActivationFunctionType

ENGS = ("pe", "act", "dve", "pool", "sp")

T = 4096
D = 1024
G = 512
NGRP = T // G
DFF = 2816
NF0 = DFF // 128
EFF = 3584
NE = 8
IN_W = 5632
EPS = 1e-6
OQ, OK_, OV, OXR, OXG, OGA, OGL = 0, 1024, 1280, 1536, 2560, 3584, 4608

V_BMOD = 0
V_QG = 96
V_CONVW = 104
V_CONVB = 184
V_LRU = 200
V_FG = 296
V_RB = 304
V_C = 312
NV = 320


class _Rec:
    def __getattr__(self, name):
        def f(*a, **k):
            self.call = (name, a, k)
            return self
        return f


class FW:
    def __init__(self, nc, stack, n_dma_sems=48):
        self.nc = nc
        self.stack = stack
        self.sem = {k: stack.enter_context(nc.semaphore("s_" + k)) for k in ENGS}
        self.cnt = {k: 0 for k in ENGS}
        self.pending = {k: [] for k in ENGS}
        self.seen = {k: {} for k in ENGS}
        self.prog = {k: [] for k in ENGS}
        self.dsems = [stack.enter_context(nc.semaphore("d%d" % i)) for i in range(n_dma_sems)]
        self.dval = [0] * n_dma_sems
        self.n_hw = n_dma_sems - 20
        self.dnext_hw = 0
        self.dnext_sw = 0
        self.semobj = {}
        for k in ENGS:
            self.semobj["s_" + k] = self.sem[k]
        for i, s in enumerate(self.dsems):
            self.semobj["d%d" % i] = s
        self.lastw = {}
        self.readers = {}
        self.ninstr = 0

    def sb(self, st, name, shape, dt):
        self.nsb = getattr(self, "nsb", 0) + 1
        return st.enter_context(self.nc.sbuf_tensor("sb%d_%s" % (self.nsb, name), list(shape), dt))

    def _need(self, eng, reads, writes):
        need = {}

        def add(sv):
            if sv is None:
                return
            s, v = sv
            if eng == "pe" and s == "s_pe":
                return
            if need.get(s, 0) < v:
                need[s] = v
        for k in reads:
            add(self.lastw.get(k))
        for k in writes:
            add(self.lastw.get(k))
            for r in self.readers.get(k, ()):
                add(r)
        out = []
        seen = self.seen[eng]
        for s, v in need.items():
            if seen.get(s, 0) >= v:
                continue
            seen[s] = v
            out.append((s, v))
        return out

    def _emit_waits(self, eng, waits):
        for s, v in waits:
            so = self.semobj[s]
            self.prog[eng].append(lambda e, so=so, v=v: e.wait_ge(so, v))

    def op(self, eng, fn, reads=(), writes=(), inc=True):
        pr = [k for k in reads if k.startswith("pb")]
        if pr:
            reads = [k for k in reads if not k.startswith("pb")]
            writes = list(writes) + pr
        rec = _Rec()
        fn(rec)
        mname, margs, mkw = rec.call
        fn = lambda e, mname=mname, margs=margs, mkw=mkw: getattr(e, mname)(*margs, **mkw)
        waits = self._need(eng, reads, writes)
        self._emit_waits(eng, waits)
        self.pending[eng].append((tuple(reads), tuple(writes)))
        self.ninstr += 1
        if inc:
            self.cnt[eng] += 1
            v = self.cnt[eng]
            s = "s_" + eng
            so = self.sem[eng]
            self.prog[eng].append(lambda e, fn=fn, so=so: fn(e).then_inc(so, 1))
            for rd, wr in self.pending[eng]:
                for k in rd:
                    self.readers.setdefault(k, []).append((s, v))
                for k in wr:
                    self.lastw[k] = (s, v)
                    self.readers[k] = []
            self.pending[eng] = []
        else:
            self.prog[eng].append(lambda e, fn=fn: fn(e))

    def dma(self, out, in_, reads=(), writes=(), q="sp", **kw):
        if q == "pool":
            i = self.n_hw + self.dnext_sw
            self.dnext_sw = (self.dnext_sw + 1) % (len(self.dsems) - self.n_hw)
        else:
            i = self.dnext_hw
            self.dnext_hw = (self.dnext_hw + 1) % self.n_hw
        s = "d%d" % i
        waits = self._need(q, reads, writes)
        prev = self.dval[i]
        if prev > 0 and self.seen[q].get(s, 0) < prev:
            self.seen[q][s] = prev
            waits.append((s, prev))
        self._emit_waits(q, waits)
        self.dval[i] += 16
        v = self.dval[i]
        so = self.dsems[i]
        self.prog[q].append(
            lambda e, out=out, in_=in_, so=so, kw=kw: e.dma_start(out=out, in_=in_, **kw).then_inc(so, 16))
        self.ninstr += 1
        for k in reads:
            self.readers.setdefault(k, []).append((s, v))
        for k in writes:
            self.lastw[k] = (s, v)
            self.readers[k] = []

    def wait_all(self, eng, keys):
        self._emit_waits(eng, self._need(eng, keys, ()))

    def barrier(self):
        allv = [("s_" + k, self.cnt[k]) for k in ENGS if self.cnt[k] > 0]
        allv += [("d%d" % i, v) for i, v in enumerate(self.dval) if v > 0]
        for eng in ENGS:
            w = []
            for s, v in allv:
                if self.seen[eng].get(s, 0) < v:
                    self.seen[eng][s] = v
                    w.append((s, v))
            self._emit_waits(eng, w)

    def finish(self):
        nc = self.nc
        prog = self.prog
        with nc.Block() as block:
            @block.tensor
            def _(e):
                for f in prog["pe"]:
                    f(e)

            @block.scalar
            def _(e):
                for f in prog["act"]:
                    f(e)

            @block.vector
            def _(e):
                for f in prog["dve"]:
                    f(e)

            @block.gpsimd
            def _(e):
                for f in prog["pool"]:
                    f(e)

            @block.sync
            def _(e):
                for f in prog["sp"]:
                    f(e)


class _Stop(Exception):
    pass


def build_nc(n_layers=2, debug=False, stop_after=None):
    nc = bass.Bass("TRN2", target_bir_lowering=False)

    def ck(name):
        if stop_after == name:
            raise _Stop()

    def din(name, shape, dt=F32):
        return nc.dram_tensor(name, list(shape), dt, kind="ExternalInput").ap()

    x_in = din("x", [T, D])
    vecs_in = din("vecs", [128, NV])
    cos_in = din("cosT", [128, T])
    sin_in = din("sinT", [128, T])
    w_mod = din("w_mod", [2, D, 6 * D])
    w_in = din("w_in", [2, D, IN_W])
    lru_wa = din("lru_wa", [2, 2, 8, 128, 128])
    lru_wx = din("lru_wx", [2, 2, 8, 128, 128])
    w_oa = din("w_o_attn", [2, D, D])
    w_ol = din("w_o_lru", [2, D, D])
    w_out = din("w_out", [2, D, D])
    f_wg = din("ffn_w_gate", [1, D, DFF])
    f_wu = din("ffn_w_up", [1, D, DFF])
    f_wd = din("ffn_w_down", [1, DFF, D])
    if n_layers > 1:
        r_w = din("router_w", [1, D, NE])
        m_wg = din("moe_w_gate", [1, NE, D, EFF])
        m_wu = din("moe_w_up", [1, NE, D, EFF])
        m_wd = din("moe_w_down", [1, NE, EFF, D])
    out_rows = T if n_layers == 1 else T // 2
    out_ap = nc.dram_tensor("out", [out_rows, D], F32, kind="ExternalOutput").ap()

    def dscr(name, shape, dt):
        return nc.dram_tensor(name, list(shape), dt).ap()

    XT = dscr("XT", [8, 128, T], F32)
    X1T = dscr("X1T", [8, 128, T], F32)
    HT = dscr("HT", [8, 128, T], BF16)
    XR = dscr("XR", [8, 128, T], BF16)
    XG = dscr("XG", [8, 128, T], BF16)
    YL = dscr("YL", [8, 128, T], BF16)
    AO = dscr("AO", [8, 128, T], BF16)

    with ExitStack() as st:
        fw = FW(nc, st)
        pb = [st.enter_context(nc.psum_tensor("pb%d" % i, [128, 512], F32)) for i in range(8)]
        PB = ["pb%d" % i for i in range(8)]
        ident = fw.sb(st, "ident", [128, 128], F32)
        ones_bf = fw.sb(st, "ones_bf", [128, 128], BF16)
        ones_f = fw.sb(st, "ones_f", [128, 128], F32)
        vecs = fw.sb(st, "vecs", [128, NV], F32)
        modT = fw.sb(st, "modT", [128, 2 * 48], F32)
        sc1p = fw.sb(st, "sc1p", [128, 2 * 48], F32)
        cneg = fw.sb(st, "cneg", [128, 32], F32)
        epsc = fw.sb(st, "epsc", [128, 1], F32)
        cact = fw.sb(st, "cact", [128, 8], F32)

        fw.op("dve", lambda e: e.memset(ident[:], 0.0), writes=["ident"])
        fw.op("pool", lambda e: e.affine_select(out=ident[:], in_=ident[:], compare_op=ALU.not_equal, fill=1.0,
                                                base=0, pattern=[[-1, 128]], channel_multiplier=1),
              reads=["ident"], writes=["ident"])
        fw.op("dve", lambda e: e.memset(ones_bf[:], 1.0), writes=["ones_bf"])
        fw.op("dve", lambda e: e.memset(ones_f[:], 1.0), writes=["ones_f"])
        fw.op("dve", lambda e: e.memset(epsc[:], EPS), writes=["epsc"])
        fw.dma(vecs[:], vecs_in[:, :], writes=["vecs"])

        open_kv = []
        try:
            fw.op("act", lambda e: e.activation(out=cact[:], in_=vecs[:, V_C:V_C + 8], func=AF.Silu),
                  reads=["vecs"], writes=["cact"])
            with ExitStack() as ph:
                xin = [fw.sb(ph, "xin%d" % i, [128, 4, D], F32) for i in range(2)]
                xts = [fw.sb(ph, "xts%d" % i, [128, 8, G], F32) for i in range(2)]
                for g in range(NGRP):
                    b = g % 2
                    fw.dma(xin[b][:], x_in[g * G:(g + 1) * G, :].rearrange("(j p) d -> p j d", p=128), writes=["xin%d" % b])
                    for c in range(8):
                        bank = 1 + (c % 4)
                        for j in range(4):
                            fw.op("pe", lambda e: e.transpose(
                                pb[bank][:, j * 128:(j + 1) * 128], xin[b][:, j, c * 128:(c + 1) * 128], ident[:]),
                                reads=["xin%d" % b, "ident"], writes=[PB[bank]], inc=(j == 3))
                        if c % 2 == 0:
                            fw.op("act", lambda e: e.activation(out=xts[b][:, c, :], in_=pb[bank][:], func=AF.Copy),
                                  reads=[PB[bank]], writes=["xts%d" % b])
                        else:
                            fw.op("dve", lambda e: e.tensor_copy(out=xts[b][:, c, :], in_=pb[bank][:]),
                                  reads=[PB[bank]], writes=["xts%d" % b])
                    fw.dma(XT[:, :, g * G:(g + 1) * G].rearrange("c p t -> p c t"), xts[b][:],
                           reads=["xts%d" % b], writes=["XT_g%d" % g])
                wmb = [fw.sb(ph, "wmb%d" % i, [128, 8, 768], BF16) for i in range(2)]
                cab = fw.sb(ph, "cab", [128, 8, 2], BF16)
                for dd in range(2):
                    fw.op("dve", lambda e, dd=dd: e.tensor_copy(out=cab[:, :, dd], in_=cact[:]), reads=["cact"], writes=["cab"])
                ci = 0
                for l in range(n_layers):
                    for cc in range(8):
                        b = ci % 2
                        ci += 1
                        for k in range(8):
                            fw.dma(wmb[b][:, k, :], w_mod[l, k * 128:(k + 1) * 128, cc * 768:(cc + 1) * 768],
                                   writes=["wmb%d" % b], q="pool", max_dma_last_dim=8192)
                        for jj in range(6):
                            j = cc * 6 + jj
                            for k in range(8):
                                fw.op("pe", lambda e, b=b, jj=jj, k=k, j=j, l=l: e.matmul(
                                    pb[0][:, 2 * (l * 48 + j):2 * (l * 48 + j) + 2], lhsT=wmb[b][:, k, jj * 128:(jj + 1) * 128],
                                    rhs=cab[:, k, :], start=(k == 0), stop=(k == 7)),
                                    reads=["wmb%d" % b, "cab"], writes=[PB[0]], inc=(k == 7))
                nl = n_layers * 48
                fw.op("dve", lambda e: e.tensor_tensor(out=modT[:, 0:nl], in0=pb[0][:, 0:2 * nl].rearrange("p (n two) -> p n two", two=2)[:, :, 0],
                                                       in1=vecs[:, V_BMOD:V_BMOD + nl], op=ALU.add), reads=[PB[0], "vecs"], writes=["modT"])
                fw.op("dve", lambda e: e.tensor_scalar_add(out=sc1p[:, 0:nl], in0=modT[:, 0:nl], scalar1=1.0),
                      reads=["modT"], writes=["sc1p"])
                tmpc = fw.sb(ph, "tmpc", [128, 32], F32)
                for l in range(2):
                    for d in range(2):
                        src = vecs[:, V_LRU + l * 48 + d * 24 + 16:V_LRU + l * 48 + d * 24 + 24]
                        dst = tmpc[:, (l * 2 + d) * 8:(l * 2 + d) * 8 + 8]
                        fw.op("act", lambda e, src=src, dst=dst: e.activation(out=dst, in_=src, func=AF.Exp, scale=-1.0),
                              reads=["vecs"], writes=["tmpc"])
                fw.op("act", lambda e: e.activation(out=tmpc[:], in_=tmpc[:], func=AF.Ln, bias=1.0),
                      reads=["tmpc"], writes=["tmpc"])
                fw.op("dve", lambda e: e.tensor_scalar_mul(out=cneg[:], in0=tmpc[:], scalar1=-8.0),
                      reads=["tmpc"], writes=["cneg"])

            fw.barrier()
            ck("0")

            def load_w(dst3, src2, key, kchunks=8):
                for k in range(kchunks):
                    fw.dma(dst3[:, k, :], src2[k * 128:(k + 1) * 128, :], writes=[key], q="pool", max_dma_last_dim=8192)

            def norm_mod(xt, xkey, l, which, hT, hkey, tmp, tmpkey, sq, sqkey, rs, rskey, bank, hf=None, hfkey=None, part=0):
                if part in (0, 1):
                    fw.op("act", lambda e: e.activation(out=sq[:], in_=xt[:], func=AF.Square), reads=[xkey], writes=[sqkey])
                if part == 1:
                    return
                for c in range(8):
                    fw.op("pe", lambda e, c=c: e.matmul(pb[bank][:], lhsT=ones_bf[:], rhs=sq[:, c * G:(c + 1) * G],
                                                        start=(c == 0), stop=(c == 7)),
                          reads=["ones_bf", sqkey], writes=[PB[bank]], inc=(c == 7))
                fw.op("act", lambda e: e.activation(out=rs[:], in_=pb[bank][:], func=AF.Ln, scale=1.0 / D, bias=epsc[:]),
                      reads=[PB[bank], "epsc"], writes=[rskey])
                fw.op("act", lambda e: e.activation(out=rs[:], in_=rs[:], func=AF.Exp, scale=-0.5),
                      reads=[rskey], writes=[rskey])
                sh0 = l * 48 + which * 24
                for c in range(8):
                    tv = tmp[:, (c % 2) * G:(c % 2 + 1) * G]
                    tk_ = tmpkey + str(c % 2)
                    fw.op("dve", lambda e, c=c: e.tensor_tensor(out=tv, in0=xt[:, c * G:(c + 1) * G],
                                                                in1=rs[:], op=ALU.mult),
                          reads=[xkey, rskey], writes=[tk_])
                    fw.op("act", lambda e, c=c: e.activation(out=hT[:, c * G:(c + 1) * G], in_=tv,
                                                             func=AF.Identity, scale=sc1p[:, sh0 + 8 + c:sh0 + 9 + c],
                                                             bias=modT[:, sh0 + c:sh0 + c + 1]),
                          reads=[tk_, "sc1p", "modT"], writes=[hkey])
                    if hf is not None:
                        fw.op("dve", lambda e, c=c: e.tensor_scalar(out=hf[:, c * G:(c + 1) * G], in0=tv,
                                                                    scalar1=sc1p[:, sh0 + 8 + c:sh0 + 9 + c],
                                                                    scalar2=modT[:, sh0 + c:sh0 + c + 1],
                                                                    op0=ALU.mult, op1=ALU.add),
                              reads=[tk_, "sc1p", "modT"], writes=[hfkey])

            def proj(bank, w3, col0, ncol, hT, hkey, wkey, tok0=0, ntok=G, kch=8, last_inc=True):
                for c in range(kch):
                    fw.op("pe", lambda e, c=c: e.matmul(pb[bank][0:ncol, 0:ntok], lhsT=w3[:, c, col0:col0 + ncol],
                                                        rhs=hT[:, c * G + tok0:c * G + tok0 + ntok],
                                                        start=(c == 0), stop=(c == kch - 1)),
                          reads=[wkey, hkey], writes=[PB[bank]], inc=(c == kch - 1))

            for l in range(n_layers):
                NGo = NGRP if l == 0 else NGRP // 2
                NTo = NGo * G
                vb = lambda off, n=1: vecs[:, off:off + n]
                kvst = ExitStack()
                kvst.__enter__()
                open_kv.append(kvst)
                KT = fw.sb(kvst, "KT%d" % l, [128, 2, T], BF16)
                Vt = fw.sb(kvst, "V%d" % l, [128, T // 128, 256], BF16)
                with ExitStack() as ph:
                    wA = fw.sb(ph, "wA", [128, 8, 2816], BF16)
                    if 'W' not in os.environ.get('PA_SKIP', ''):
                        load_w(wA[:, :, 0:2560], w_in[l, :, OK_:OK_ + 2560], "wA")
                    xA = [fw.sb(ph, "xA%d" % i, [128, 8 * G], F32) for i in range(2)]
                    hA = [fw.sb(ph, "hA%d" % i, [128, 8 * G], BF16) for i in range(2)]
                    tmpA = fw.sb(ph, "tmpA", [128, 2 * G], F32)
                    rsA = fw.sb(ph, "rsA", [128, G], F32)
                    def rot_copy(w, dst0, src0, nheads, key):
                        srcv = w[:, :, src0:src0 + nheads * 128].rearrange("p k (h a b f) -> p k h a b f", h=nheads, a=2, b=2)
                        dstv = w[:, :, dst0:dst0 + nheads * 128].rearrange("p k (h a b f) -> p k h a b f", h=nheads, a=2, b=2)
                        for k in range(8):
                            for bb in range(2):
                                for hh in range(nheads):
                                    fw.op("dve", lambda e, k=k, bb=bb, hh=hh: e.tensor_copy(
                                        out=dstv[:, k, hh, :, bb, :], in_=srcv[:, k, hh, :, 1 - bb, :]),
                                        reads=[key], writes=[key + "r"])
                    if 'R' not in os.environ.get('PA_SKIP', ''):
                        rot_copy(wA, 2560, 0, 2, "wA")
                    ctab = fw.sb(ph, "ctabA", [128, T], F32)
                    stab = fw.sb(ph, "stabA", [128, T], F32)
                    if 'T' not in os.environ.get('PA_SKIP', ''):
                        fw.dma(ctab[:], cos_in[:, :], writes=["ctab"])
                        fw.dma(stab[:], sin_in[:, :], writes=["stab"])
                    sqk = fw.sb(ph, "sqk", [128, G], BF16)
                    rk = fw.sb(ph, "rk", [128, G], F32)
                    t1 = fw.sb(ph, "t1", [128, G], F32)
                    t2 = fw.sb(ph, "t2", [128, G], F32)
                    xrs = [fw.sb(ph, "xrs0", [128, 8, G], BF16)] * 2
                    xgs = [fw.sb(ph, "xgs0", [128, 8, G], BF16)] * 2

                    def qk_norm_rope(bA, bB, bC, g_ap, gp_ap, tok0, out_ap, outkey):
                        qn = int(os.environ.get('QK_N', '8'))
                        opi = [0]
                        def op_(*a, **k):
                            opi[0] += 1
                            if opi[0] <= qn:
                                fw.op(*a, **k)
                        op_("act", lambda e: e.activation(out=sqk[:], in_=pb[bA][:], func=AF.Square),
                              reads=[PB[bA]], writes=["sqk"])
                        op_("pe", lambda e: e.matmul(pb[bC][:], lhsT=ones_bf[:], rhs=sqk[:], start=True, stop=True),
                              reads=["sqk", "ones_bf"], writes=[PB[bC]])
                        op_("act", lambda e: e.activation(out=rk[:], in_=pb[bC][:], func=AF.Ln, scale=1.0 / 128, bias=epsc[:]),
                              reads=[PB[bC], "epsc"], writes=["rk"])
                        op_("act", lambda e: e.activation(out=rk[:], in_=rk[:], func=AF.Exp, scale=-0.5),
                              reads=["rk"], writes=["rk"])
                        op_("dve", lambda e: e.scalar_tensor_tensor(out=t1[:], in0=pb[bA][:], scalar=g_ap,
                                                                      in1=(rk[:] if os.environ.get('QK_ALT') else ctab[:, tok0:tok0 + G]), op0=ALU.mult, op1=ALU.mult),
                              reads=[PB[bA], "vecs", "ctab", "rk"], writes=["t1"])
                        op_("dve", lambda e: e.scalar_tensor_tensor(out=t2[:], in0=pb[bB][:], scalar=gp_ap,
                                                                      in1=stab[:, tok0:tok0 + G], op0=ALU.mult, op1=ALU.mult),
                              reads=[PB[bB], "vecs", "stab"], writes=["t2"])
                        op_("dve", lambda e: e.tensor_tensor(out=t1[:], in0=t1[:], in1=t2[:], op=ALU.add),
                              reads=["t1", "t2"], writes=["t1"])
                        op_("dve", lambda e: e.tensor_tensor(out=out_ap, in0=t1[:], in1=rk[:], op=ALU.mult),
                              reads=["t1", "rk"], writes=[outkey])

                    def load_norm_A(g, part):
                        b = g % 2
                        xk, hk = "xA%d" % b, "hA%d" % b
                        if part in (0, 1):
                            fw.dma(xA[b][:].rearrange("p (c t) -> p c t", c=8),
                                   XT[:, :, g * G:(g + 1) * G].rearrange("c p t -> p c t"),
                                   reads=["XT_g%d" % g], writes=[xk])
                        norm_mod(xA[b], xk, l, 0, hA[b], hk, tmpA, "tmpA", hA[b], hk, rsA, "rsA", 0, part=part)

                    load_norm_A(0, 0)
                    for g in range(NGRP):
                        b = g % 2
                        xk, hk = "xA%d" % b, "hA%d" % b
                        fw.dma(HT[:, :, g * G:(g + 1) * G].rearrange("c p t -> p c t"),
                               hA[b][:].rearrange("p (c t) -> p c t", c=8), reads=[hk], writes=["HT_g%d" % g])
                        for kv in range(2 if 'k' in _PARTS else 0):
                            proj(1, wA, kv * 128, 128, hA[b], hk, "wA")
                            proj(2, wA, 2560 + kv * 128, 128, hA[b], hk, "wAr")
                            qk_norm_rope(1, 2, 3, vb(V_QG + l * 4 + 2), vb(V_QG + l * 4 + 3), g * G,
                                         KT[:, kv, g * G:(g + 1) * G], "KT")
                        if g + 1 < NGRP:
                            load_norm_A(g + 1, 1)
                        for j in range(4 if 'v' in _PARTS else 0):
                            bank = 4 + (j % 2)
                            for c in range(8):
                                fw.op("pe", lambda e, c=c, j=j, bank=bank: e.matmul(
                                    pb[bank][:, 0:256], lhsT=hA[b][:, c * G + j * 128:c * G + (j + 1) * 128],
                                    rhs=wA[:, c, 256:512], start=(c == 0), stop=(c == 7)),
                                    reads=[hk, "wA"], writes=[PB[bank]], inc=(c == 7))
                            fw.op("act", lambda e, j=j, bank=bank: e.activation(out=Vt[:, g * 4 + j, :], in_=pb[bank][:, 0:256],
                                                                                func=AF.Copy),
                                  reads=[PB[bank]], writes=["V"])
                        if g + 1 < NGRP:
                            load_norm_A(g + 1, 2)
                        for blk in range(8 if 'r' in _PARTS else 0):
                            bank = 4 + (blk % 4)
                            proj(bank, wA, 512 + blk * 128, 128, hA[b], hk, "wA")
                            if blk % 2 == 0:
                                fw.op("dve", lambda e, blk=blk, bank=bank: e.tensor_copy(out=xrs[b][:, blk, :], in_=pb[bank][:]),
                                      reads=[PB[bank]], writes=["xrs0"])
                            else:
                                fw.op("act", lambda e, blk=blk, bank=bank: e.activation(out=xrs[b][:, blk, :], in_=pb[bank][:],
                                                                                        func=AF.Copy),
                                      reads=[PB[bank]], writes=["xrs0"])
                        if 'r' in _PARTS:
                            fw.dma(XR[:, :, g * G:(g + 1) * G].rearrange("c p t -> p c t"), xrs[b][:],
                                   reads=["xrs0"], writes=["XR"])
                        if g < NGo and 'g' in _PARTS:
                            for blk in range(8):
                                bank = 4 + (blk % 4)
                                proj(bank, wA, 1536 + blk * 128, 128, hA[b], hk, "wA")
                                fw.op("act", lambda e, blk=blk, bank=bank: e.activation(out=xgs[b][:, blk, :], in_=pb[bank][:],
                                                                                        func=AF.Gelu_apprx_tanh),
                                      reads=[PB[bank]], writes=["xgs0"])
                            fw.dma(XG[:, :, g * G:(g + 1) * G].rearrange("c p t -> p c t"), xgs[b][:],
                                   reads=["xgs0"], writes=["XG"])
                fw.barrier()

                ck("A%d" % l)
                with ExitStack() as ph:
                    wlr = fw.sb(ph, "wlr", [128, 32, 128], BF16)
                    for d in range(2):
                        fw.dma(wlr[:, d * 16:d * 16 + 8, :], lru_wa[l, d].rearrange("n i j -> i n j"), writes=["wlr"],
                               q="pool", max_dma_last_dim=8192)
                        fw.dma(wlr[:, d * 16 + 8:d * 16 + 16, :], lru_wx[l, d].rearrange("n i j -> i n j"), writes=["wlr"],
                               q="pool", max_dma_last_dim=8192)
                    CE = os.environ.get("CONV_ENG", "pool")
                    xrp = [fw.sb(ph, "xrp%d" % i, [128, T + 4], BF16) for i in range(2)]
                    xcb = [fw.sb(ph, "xcb%d" % i, [128, T], BF16) for i in range(2)]
                    hb = [fw.sb(ph, "hb%d" % i, [128, T], BF16) for i in range(2)]
                    rb_ = [fw.sb(ph, "rb%d" % i, [128, T], F32) for i in range(2)]
                    ibs = [fw.sb(ph, "ib%d" % i, [128, T], F32) for i in range(2)]
                    abs_ = [fw.sb(ph, "ab%d" % i, [128, T], F32) for i in range(2)]
                    xgb = fw.sb(ph, "xgb", [128, NTo], BF16)
                    for i in range(2):
                        fw.op("dve", lambda e, i=i: e.memset(xrp[i][:, 0:2], 0.0), writes=["xrp%d" % i])
                        fw.op("dve", lambda e, i=i: e.memset(xrp[i][:, T + 2:T + 4], 0.0), writes=["xrp%d" % i])
                    cw = V_CONVW + l * 40

                    dgc = fw.sb(ph, "dgc", [128, 40, 128], BF16)
                    for blk_ in range(8):
                        for j in range(5):
                            fw.op("dve", lambda e: e.tensor_scalar(
                                out=dgc[:, blk_ * 5 + j, :], in0=ident[:], scalar1=vecs[:, cw + j * 8 + blk_:cw + j * 8 + blk_ + 1],
                                scalar2=None, op0=ALU.mult), reads=["ident", "vecs"], writes=["dgc"])

                    def conv_stage(blk):
                        pb_ = blk % 2
                        xk_, ck_, dk_ = "xrp%d" % pb_, "xcb%d" % pb_, "dgc"
                        fw.dma(xrp[pb_][:, 2:T + 2], XR[blk, :, :], reads=["XR"], writes=[xk_])
                        for g8 in range(NGRP):
                            bank = 6 + (g8 % 2)
                            for j in range(5):
                                fw.op("pe", lambda e, j=j: e.matmul(
                                    pb[bank][:], lhsT=dgc[:, blk * 5 + j, :], rhs=xrp[pb_][:, j + g8 * G:j + (g8 + 1) * G],
                                    start=(j == 0), stop=(j == 4)), reads=[dk_, xk_], writes=[PB[bank]], inc=(j == 4))
                            fw.op("dve", lambda e: e.tensor_scalar(
                                out=xcb[pb_][:, g8 * G:(g8 + 1) * G], in0=pb[bank][:],
                                scalar1=vecs[:, V_CONVB + l * 8 + blk:V_CONVB + l * 8 + blk + 1], scalar2=None, op0=ALU.add),
                                reads=[PB[bank], "vecs"], writes=[ck_])

                    conv_stage(0)
                    for blk in range(8):
                        pb_ = blk % 2
                        ck_ = "xcb%d" % pb_
                        fw.dma(xgb[:], XG[blk, :, 0:NTo], reads=["XG"], writes=["xgb"])
                        for d in range(2):
                            lo = V_LRU + l * 48 + d * 24
                            r, ib, ab = rb_[d], ibs[d], abs_[d]
                            rkey, ikey, akey = "rb%d" % d, "ib%d" % d, "ab%d" % d
                            Td = NTo if d == 0 else T
                            for g8 in range(Td // G):
                                ba, bi = (g8 % 3) * 2, (g8 % 3) * 2 + 1
                                fw.op("pe", lambda e: e.matmul(
                                    pb[ba][:], lhsT=wlr[:, d * 16 + blk, :], rhs=xcb[pb_][:, g8 * G:(g8 + 1) * G], start=True, stop=True),
                                    reads=["wlr", ck_], writes=[PB[ba]])
                                fw.op("pe", lambda e: e.matmul(
                                    pb[bi][:], lhsT=wlr[:, d * 16 + 8 + blk, :], rhs=xcb[pb_][:, g8 * G:(g8 + 1) * G], start=True, stop=True),
                                    reads=["wlr", ck_], writes=[PB[bi]])
                                fw.op("act", lambda e: e.activation(
                                    out=r[:, g8 * G:(g8 + 1) * G], in_=pb[ba][:], func=AF.Sigmoid,
                                    bias=vecs[:, lo + blk:lo + blk + 1]), reads=[PB[ba], "vecs"], writes=[rkey])
                                fw.op("act", lambda e: e.activation(
                                    out=ib[:, g8 * G:(g8 + 1) * G], in_=pb[bi][:], func=AF.Sigmoid,
                                    bias=vecs[:, lo + 8 + blk:lo + 8 + blk + 1]), reads=[PB[bi], "vecs"], writes=[ikey])
                            if d == 0 and blk + 1 < 8:
                                conv_stage(blk + 1)
                            fw.op("dve", lambda e: e.tensor_tensor(out=ib[:, 0:Td], in0=ib[:, 0:Td], in1=xcb[pb_][:, 0:Td], op=ALU.mult),
                                  reads=[ikey, ck_], writes=[ikey])
                            cn = cneg[:, (l * 2 + d) * 8 + blk:(l * 2 + d) * 8 + blk + 1]
                            fw.op("act", lambda e: e.activation(out=ab[:, 0:Td], in_=r[:, 0:Td], func=AF.Exp, scale=cn),
                                  reads=[rkey, "cneg"], writes=[akey])
                            fw.op("act", lambda e: e.activation(out=r[:, 0:Td], in_=ab[:, 0:Td], func=AF.Square),
                                  reads=[akey], writes=[rkey])
                            fw.op("act", lambda e: e.activation(out=r[:, 0:Td], in_=r[:, 0:Td], func=AF.Sqrt, scale=-1.0, bias=1.0),
                                  reads=[rkey], writes=[rkey])
                            fw.op("dve", lambda e: e.tensor_tensor(out=ib[:, 0:Td], in0=ib[:, 0:Td], in1=r[:, 0:Td], op=ALU.mult),
                                  reads=[ikey, rkey], writes=[ikey])
                            if d == 0:
                                fw.op("dve", lambda e: e.tensor_tensor_scan(out=hb[0][:, 0:Td], data0=ab[:, 0:Td], data1=ib[:, 0:Td], initial=0.0,
                                                                            op0=ALU.mult, op1=ALU.add),
                                      reads=[akey, ikey], writes=["hb0"])
                            else:
                                fw.op("dve", lambda e: e.tensor_tensor_scan(out=hb[1][:, ::-1], data0=ab[:, ::-1], data1=ib[:, ::-1],
                                                                            initial=0.0, op0=ALU.mult, op1=ALU.add),
                                      reads=[akey, ikey], writes=["hb1"])
                        fw.op("pool", lambda e: e.tensor_tensor(out=hb[0][:, 0:NTo], in0=hb[0][:, 0:NTo], in1=hb[1][:, 0:NTo],
                                                                op=ALU.add), reads=["hb0", "hb1"], writes=["hb0"])
                        fw.op("pool", lambda e: e.tensor_tensor(out=xgb[:], in0=hb[0][:, 0:NTo], in1=xgb[:], op=ALU.mult),
                              reads=["hb0", "xgb"], writes=["xgb"])
                        fw.dma(YL[blk, :, 0:NTo], xgb[:], reads=["xgb"], writes=["YL"])
                fw.barrier()

                ck("B%d" % l)
                with ExitStack() as ph:
                    wq = fw.sb(ph, "wq", [128, 8, 2048], BF16)
                    load_w(wq[:, :, 0:1024], w_in[l, :, OQ:OQ + 1024], "wq")
                    ctab = fw.sb(ph, "ctabC", [128, NTo], F32)
                    stab = fw.sb(ph, "stabC", [128, NTo], F32)
                    fw.dma(ctab[:], cos_in[:, 0:NTo], writes=["ctab"])
                    fw.dma(stab[:], sin_in[:, 0:NTo], writes=["stab"])
                    srcv = wq[:, :, 0:1024].rearrange("p k (h a b f) -> p k h a b f", h=8, a=2, b=2)
                    dstv = wq[:, :, 1024:2048].rearrange("p k (h a b f) -> p k h a b f", h=8, a=2, b=2)
                    for k in range(8):
                        for bb in range(2):
                            for a in range(2):
                                fw.op("dve", lambda e, k=k, bb=bb, a=a: e.tensor_copy(
                                    out=dstv[:, k, :, a, bb, :], in_=srcv[:, k, :, a, 1 - bb, :]),
                                    reads=["wq"], writes=["wqr"])
                    hC = [fw.sb(ph, "hC%d" % i, [128, 8 * G], BF16) for i in range(2)]
                    qT = fw.sb(ph, "qT", [128, 8, G], BF16)
                    sqk = fw.sb(ph, "sqkC", [128, G], BF16)
                    rk = fw.sb(ph, "rkC", [128, G], F32)
                    t1 = fw.sb(ph, "t1C", [128, G], F32)
                    t2 = fw.sb(ph, "t2C", [128, G], F32)
                    PTb = [fw.sb(ph, "PT%d" % i, [128, G], BF16) for i in range(3)]
                    rec = fw.sb(ph, "rec", [128, G], F32)
                    aos = [fw.sb(ph, "aos%d" % i, [128, 8, G], BF16) for i in range(2)]
                    sc = 1.0 / math.sqrt(128.0)
                    for g in range(min(NGo, int(os.environ.get('C1_NG', '99')))):
                        b = g % 2
                        hk = "hC%d" % b
                        fw.dma(hC[b][:].rearrange("p (c t) -> p c t", c=8),
                               HT[:, :, g * G:(g + 1) * G].rearrange("c p t -> p c t"), reads=["HT_g%d" % g], writes=[hk])
                        for h in range(8):
                            proj(0, wq, h * 128, 128, hC[b], hk, "wq")
                            proj(1, wq, 1024 + h * 128, 128, hC[b], hk, "wqr")
                            fw.op("act", lambda e: e.activation(out=sqk[:], in_=pb[0][:], func=AF.Square),
                                  reads=[PB[0]], writes=["sqk"])
                            fw.op("pe", lambda e: e.matmul(pb[2][:], lhsT=ones_bf[:], rhs=sqk[:], start=True, stop=True),
                                  reads=["sqk", "ones_bf"], writes=[PB[2]])
                            fw.op("act", lambda e: e.activation(out=rk[:], in_=pb[2][:], func=AF.Ln, scale=1.0 / 128, bias=epsc[:]),
                                  reads=[PB[2], "epsc"], writes=["rk"])
                            fw.op("act", lambda e: e.activation(out=rk[:], in_=rk[:], func=AF.Exp, scale=-0.5),
                                  reads=["rk"], writes=["rk"])
                            fw.op("dve", lambda e: e.scalar_tensor_tensor(out=t1[:], in0=pb[0][:], scalar=vb(V_QG + l * 4 + 0),
                                                                          in1=ctab[:, g * G:(g + 1) * G], op0=ALU.mult, op1=ALU.mult),
                                  reads=[PB[0], "vecs", "ctab"], writes=["t1"])
                            fw.op("dve", lambda e: e.scalar_tensor_tensor(out=t2[:], in0=pb[1][:], scalar=vb(V_QG + l * 4 + 1),
                                                                          in1=stab[:, g * G:(g + 1) * G], op0=ALU.mult, op1=ALU.mult),
                                  reads=[PB[1], "vecs", "stab"], writes=["t2"])
                            fw.op("dve", lambda e: e.tensor_tensor(out=t1[:], in0=t1[:], in1=t2[:], op=ALU.add),
                                  reads=["t1", "t2"], writes=["t1"])
                            fw.op("dve", lambda e, h=h: e.tensor_tensor(out=qT[:, h, :], in0=t1[:], in1=rk[:], op=ALU.mult),
                                  reads=["t1", "rk"], writes=["qT%d" % h])
                        for h in range(8):
                            kv = h // 4
                            bo, bs = 4 + (h % 2) * 2, 5 + (h % 2) * 2
                            NKT = T // 128

                            def emit_st(kt):
                                bst = kt % 3
                                fw.op("pe", lambda e: e.matmul(
                                    pb[bst][:], lhsT=KT[:, kv, kt * 128:(kt + 1) * 128], rhs=qT[:, h, :], start=True, stop=True),
                                    reads=["KT", "qT%d" % h], writes=[PB[bst]])
                                fw.op("act", lambda e: e.activation(out=PTb[kt % 3][:], in_=pb[bst][:], func=AF.Exp, scale=sc),
                                      reads=[PB[bst]], writes=["PT%d" % (kt % 3)])

                            emit_st(0)
                            emit_st(1)
                            for kt in range(NKT):
                                if kt + 2 < NKT:
                                    emit_st(kt + 2)
                                ptk = "PT%d" % (kt % 3)
                                last = kt == NKT - 1
                                fw.op("pe", lambda e: e.matmul(
                                    pb[bo][:], lhsT=Vt[:, kt, kv * 128:(kv + 1) * 128], rhs=PTb[kt % 3][:], start=(kt == 0), stop=last),
                                    reads=["V", ptk], writes=[PB[bo]], inc=False)
                                fw.op("pe", lambda e: e.matmul(
                                    pb[bs][:], lhsT=ones_bf[:], rhs=PTb[kt % 3][:], start=(kt == 0), stop=last),
                                    reads=["ones_bf", ptk], writes=[PB[bs]], inc=True)
                            fw.op("dve", lambda e, bs=bs: e.reciprocal(out=rec[:], in_=pb[bs][:]), reads=[PB[bs]], writes=["rec"])
                            fw.op("dve", lambda e, bo=bo, h=h: e.tensor_tensor(out=aos[b][:, h, :], in0=pb[bo][:], in1=rec[:], op=ALU.mult),
                                  reads=[PB[bo], "rec"], writes=["aos%d" % b])
                        fw.dma(AO[:, :, g * G:(g + 1) * G].rearrange("c p t -> p c t"), aos[b][:],
                               reads=["aos%d" % b], writes=["AO"])
                fw.barrier()
                kvst.__exit__(None, None, None)
                open_kv.remove(kvst)

                ck("C1%d" % l)
                with ExitStack() as ph:
                    wg_ = fw.sb(ph, "wgt", [128, 8, 2048], BF16)
                    load_w(wg_, w_in[l, :, OGA:OGA + 2048], "wgt")
                    woa = fw.sb(ph, "woa", [128, 8, D], BF16)
                    wol = fw.sb(ph, "wol", [128, 8, D], BF16)
                    wo = fw.sb(ph, "wo", [128, 8, D], BF16)
                    load_w(woa, w_oa[l], "woa")
                    load_w(wol, w_ol[l], "wol")
                    load_w(wo, w_out[l], "wo")
                    hC = [fw.sb(ph, "hD%d" % i, [128, 8 * G], BF16) for i in range(2)]
                    aoC = [fw.sb(ph, "aoD%d" % i, [128, 8 * G], BF16) for i in range(2)]
                    ylC = [fw.sb(ph, "ylD%d" % i, [128, 8 * G], BF16) for i in range(2)]
                    xC = [fw.sb(ph, "xD%d" % i, [128, 8 * G], F32) for i in range(2)]
                    mg = fw.sb(ph, "mg", [128, 8 * G], BF16)
                    sg = [fw.sb(ph, "sg%d" % i, [128, G], F32) for i in range(2)]
                    m1 = [fw.sb(ph, "m1%d" % i, [128, G], F32) for i in range(2)]
                    for g in range(NGo):
                        b = g % 2
                        sl = lambda A: A[:, :, g * G:(g + 1) * G].rearrange("c p t -> p c t")
                        v3 = lambda t_: t_[:].rearrange("p (c t) -> p c t", c=8)
                        fw.dma(v3(hC[b]), sl(HT), reads=["HT_g%d" % g], writes=["hD%d" % b])
                        fw.dma(v3(aoC[b]), sl(AO), reads=["AO"], writes=["aoD%d" % b])
                        fw.dma(v3(ylC[b]), sl(YL), reads=["YL"], writes=["ylD%d" % b])
                        fw.dma(v3(xC[b]), sl(XT), reads=["XT_g%d" % g], writes=["xD%d" % b])
                        for c in range(8):
                            o4 = (c % 2) * 4
                            proj(o4 + 0, woa, c * 128, 128, aoC[b], "aoD%d" % b, "woa")
                            proj(o4 + 1, wg_, c * 128, 128, hC[b], "hD%d" % b, "wgt")
                            proj(o4 + 2, wol, c * 128, 128, ylC[b], "ylD%d" % b, "wol")
                            proj(o4 + 3, wg_, 1024 + c * 128, 128, hC[b], "hD%d" % b, "wgt")
                            fw.op("act", lambda e: e.activation(out=sg[0][:], in_=pb[o4 + 1][:], func=AF.Sigmoid), reads=[PB[o4 + 1]], writes=["sg0"])
                            fw.op("act", lambda e: e.activation(out=sg[1][:], in_=pb[o4 + 3][:], func=AF.Sigmoid), reads=[PB[o4 + 3]], writes=["sg1"])
                            fw.op("dve", lambda e: e.tensor_tensor(out=m1[0][:], in0=pb[o4 + 0][:], in1=sg[0][:], op=ALU.mult),
                                  reads=[PB[o4 + 0], "sg0"], writes=["m10"])
                            fw.op("dve", lambda e: e.tensor_tensor(out=m1[1][:], in0=pb[o4 + 2][:], in1=sg[1][:], op=ALU.mult),
                                  reads=[PB[o4 + 2], "sg1"], writes=["m11"])
                            fw.op("dve", lambda e, c=c: e.tensor_tensor(out=mg[:, c * G:(c + 1) * G], in0=m1[0][:], in1=m1[1][:], op=ALU.add),
                                  reads=["m10", "m11"], writes=["mg"])
                        g1o = l * 48 + 16
                        for c in range(8):
                            bank = 4 + (c % 2)
                            proj(bank, wo, c * 128, 128, mg, "mg", "wo")
                            fw.op("dve", lambda e, c=c, bank=bank: e.scalar_tensor_tensor(
                                out=xC[b][:, c * G:(c + 1) * G], in0=pb[bank][:], scalar=modT[:, g1o + c:g1o + c + 1],
                                in1=xC[b][:, c * G:(c + 1) * G], op0=ALU.mult, op1=ALU.add),
                                reads=[PB[bank], "modT", "xD%d" % b], writes=["xD%d" % b])
                        fw.dma(sl(X1T), v3(xC[b]), reads=["xD%d" % b], writes=["X1T_g%d" % g])
                fw.barrier()

                ck("C2%d" % l)
                if l == 0:
                    with ExitStack() as ph:
                        wg = fw.sb(ph, "fwg", [128, 8, DFF], BF16)
                        wu = fw.sb(ph, "fwu", [128, 8, DFF], BF16)
                        wd = fw.sb(ph, "fwd", [128, NF0, D], BF16)
                        load_w(wg, f_wg[0], "fwg")
                        load_w(wu, f_wu[0], "fwu")
                        load_w(wd, f_wd[0], "fwd", kchunks=NF0)
                        xD = [fw.sb(ph, "xE%d" % i, [128, 8 * G], F32) for i in range(2)]
                        hD = fw.sb(ph, "hE", [128, 8 * G], BF16)
                        tmpD = fw.sb(ph, "tmpE", [128, 2 * G], F32)
                        rsD = fw.sb(ph, "rsE", [128, G], F32)
                        act = fw.sb(ph, "actE", [128, NF0 * G], BF16)
                        sil = [fw.sb(ph, "sil%d" % i, [128, G], F32) for i in range(2)]
                        for g in range(NGo):
                            b = g % 2
                            sl = lambda A: A[:, :, g * G:(g + 1) * G].rearrange("c p t -> p c t")
                            v3 = lambda t_: t_[:].rearrange("p (c t) -> p c t", c=8)
                            if g == 0:
                                fw.dma(v3(xD[b]), sl(X1T), reads=["X1T_g%d" % g], writes=["xE%d" % b])
                                norm_mod(xD[b], "xE%d" % b, l, 1, hD, "hE", tmpD, "tmpE", hD, "hE", rsD, "rsE", 0)
                            for f in range(NF0):
                                bg, bu = 1 + (f % 2) * 2, 2 + (f % 2) * 2
                                proj(bg, wg, f * 128, 128, hD, "hE", "fwg")
                                proj(bu, wu, f * 128, 128, hD, "hE", "fwu")
                                fw.op("act", lambda e, f=f, bg=bg: e.activation(out=sil[f % 2][:], in_=pb[bg][:], func=AF.Silu),
                                      reads=[PB[bg]], writes=["sil%d" % (f % 2)])
                                fw.op("dve", lambda e, f=f, bu=bu: e.tensor_tensor(out=act[:, f * G:(f + 1) * G], in0=pb[bu][:],
                                                                                   in1=sil[f % 2][:], op=ALU.mult),
                                      reads=[PB[bu], "sil%d" % (f % 2)], writes=["actE%d" % f])
                            g2o = l * 48 + 40
                            if g + 1 < NGo:
                                b1 = (g + 1) % 2
                                fw.dma(v3(xD[b1]), X1T[:, :, (g + 1) * G:(g + 2) * G].rearrange("c p t -> p c t"),
                                       reads=["X1T_g%d" % (g + 1)], writes=["xE%d" % b1])
                                norm_mod(xD[b1], "xE%d" % b1, l, 1, hD, "hE", tmpD, "tmpE", hD, "hE", rsD, "rsE", 0, part=1)
                            for c in range(8):
                                if c == 3 and g + 1 < NGo:
                                    norm_mod(xD[b1], "xE%d" % b1, l, 1, hD, "hE", tmpD, "tmpE", hD, "hE", rsD, "rsE", 0, part=2)
                                bank = 5 + (c % 2)
                                for f in range(NF0):
                                    fw.op("pe", lambda e, f=f, c=c, bank=bank: e.matmul(
                                        pb[bank][:], lhsT=wd[:, f, c * 128:(c + 1) * 128], rhs=act[:, f * G:(f + 1) * G],
                                        start=(f == 0), stop=(f == NF0 - 1)),
                                        reads=["fwd", "actE%d" % f], writes=[PB[bank]], inc=(f == NF0 - 1))
                                fw.op("dve", lambda e, c=c, bank=bank: e.scalar_tensor_tensor(
                                    out=xD[b][:, c * G:(c + 1) * G], in0=pb[bank][:], scalar=modT[:, g2o + c:g2o + c + 1],
                                    in1=xD[b][:, c * G:(c + 1) * G], op0=ALU.mult, op1=ALU.add),
                                    reads=[PB[bank], "modT", "xE%d" % b], writes=["xE%d" % b])
                            fw.dma(sl(XT), v3(xD[b]), reads=["xE%d" % b], writes=["XT_g%d" % g])
                    fw.barrier()
                    ck("D0")
                else:
                    with ExitStack() as ph:
                        NTk = NTo // 128
                        acc = fw.sb(ph, "acc", [128, 8, NTo], F32)
                        hM = fw.sb(ph, "hM", [128, 8, NTo], BF16)
                        comb = fw.sb(ph, "comb", [128, NTk, 8], F32)
                        rw = fw.sb(ph, "rw", [128, 8, 8], F32)
                        rwh = fw.sb(ph, "rwh", [128, 8, 8], BF16)
                        rwl = fw.sb(ph, "rwl", [128, 8, 8], BF16)
                        for k in range(8):
                            fw.dma(rw[:, k, :], r_w[0, k * 128:(k + 1) * 128, :], writes=["rw"])
                        fw.op("dve", lambda e: e.tensor_copy(out=rwh[:], in_=rw[:]), reads=["rw"], writes=["rwh"])
                        fw.op("dve", lambda e: e.tensor_tensor(out=rw[:], in0=rw[:], in1=rwh[:], op=ALU.subtract), reads=["rw", "rwh"], writes=["rw"])
                        fw.op("dve", lambda e: e.tensor_copy(out=rwl[:], in_=rw[:]), reads=["rw"], writes=["rwl"])
                        with ExitStack() as ph2:
                            xD = fw.sb(ph2, "xM", [128, 8 * G], F32)
                            hD = fw.sb(ph2, "hMg", [128, 8 * G], BF16)
                            hF = fw.sb(ph2, "hMf", [128, 8 * G], F32)
                            hFh = fw.sb(ph2, "hMfh", [128, 8 * G], BF16)
                            hFl = fw.sb(ph2, "hMfl", [128, 8 * G], BF16)
                            tmpD = fw.sb(ph2, "tmpM", [128, 2 * G], F32)
                            rsD = fw.sb(ph2, "rsM", [128, G], F32)
                            lg = fw.sb(ph2, "lg", [128, 8], F32)
                            m8 = fw.sb(ph2, "m8", [128, 8], F32)
                            ex = fw.sb(ph2, "ex", [128, 8], F32)
                            msk = fw.sb(ph2, "msk", [128, 8], F32)
                            nm1 = fw.sb(ph2, "nm1", [128, 1], F32)
                            ssum = fw.sb(ph2, "ssum", [128, 1], F32)
                            for g in range(NGo):
                                sl = lambda A: A[:, :, g * G:(g + 1) * G].rearrange("c p t -> p c t")
                                v3 = lambda t_: t_[:].rearrange("p (c t) -> p c t", c=8)
                                fw.dma(v3(xD), sl(X1T), reads=["X1T_g%d" % g], writes=["xM"])
                                norm_mod(xD, "xM", l, 1, hD, "hMg", tmpD, "tmpM", hD, "hMg", rsD, "rsM", 0, hf=hF, hfkey="hMf")
                                fw.op("act", lambda e, g=g: e.activation(out=acc[:, :, g * G:(g + 1) * G], in_=v3(xD), func=AF.Copy),
                                      reads=["xM"], writes=["acc_g%d" % g])
                                fw.op("dve", lambda e, g=g: e.tensor_copy(out=hM[:, :, g * G:(g + 1) * G], in_=v3(hD)),
                                      reads=["hMg"], writes=["hM"])
                                fw.op("act", lambda e: e.activation(out=hFh[:], in_=hF[:], func=AF.Copy), reads=["hMf"], writes=["hMfh"])
                                fw.op("dve", lambda e: e.tensor_tensor(out=hF[:], in0=hF[:], in1=hFh[:], op=ALU.subtract),
                                      reads=["hMf", "hMfh"], writes=["hMf"])
                                fw.op("act", lambda e: e.activation(out=hFl[:], in_=hF[:], func=AF.Copy), reads=["hMf"], writes=["hMfl"])
                                for j in range(4):
                                    tk = g * 4 + j
                                    n_ = 0
                                    for c in range(8):
                                        for (ha, hk_, wa, wk_) in ((hFh, "hMfh", rwh, "rwh"), (hFh, "hMfh", rwl, "rwl"), (hFl, "hMfl", rwh, "rwh")):
                                            fw.op("pe", lambda e, c=c, j=j, ha=ha, wa=wa, n_=n_: e.matmul(
                                                pb[1][:, 0:8], lhsT=ha[:, c * G + j * 128:c * G + (j + 1) * 128], rhs=wa[:, c, :],
                                                start=(n_ == 0), stop=(n_ == 23)), reads=[hk_, wk_], writes=[PB[1]], inc=(n_ == 23))
                                            n_ += 1
                                    fw.op("dve", lambda e: e.tensor_tensor(out=lg[:], in0=pb[1][:, 0:8], in1=vecs[:, V_RB:V_RB + 8], op=ALU.add),
                                          reads=[PB[1], "vecs"], writes=["lg"])
                                    fw.op("dve", lambda e: e.max(out=m8[:], in_=lg[:]), reads=["lg"], writes=["m8"])
                                    fw.op("dve", lambda e: e.tensor_scalar(out=msk[:], in0=lg[:], scalar1=m8[:, 1:2], scalar2=None, op0=ALU.is_ge),
                                          reads=["lg", "m8"], writes=["msk"])
                                    fw.op("dve", lambda e: e.tensor_scalar_mul(out=nm1[:], in0=m8[:, 0:1], scalar1=-1.0),
                                          reads=["m8"], writes=["nm1"])
                                    fw.op("act", lambda e: e.activation(out=ex[:], in_=lg[:], func=AF.Exp, bias=nm1[:]),
                                          reads=["lg", "nm1"], writes=["ex"])
                                    fw.op("dve", lambda e: e.tensor_tensor(out=ex[:], in0=ex[:], in1=msk[:], op=ALU.mult),
                                          reads=["ex", "msk"], writes=["ex"])
                                    fw.op("dve", lambda e: e.reduce_sum(out=ssum[:], in_=ex[:], axis=mybir.AxisListType.X),
                                          reads=["ex"], writes=["ssum"])
                                    fw.op("dve", lambda e: e.reciprocal(out=ssum[:], in_=ssum[:]), reads=["ssum"], writes=["ssum"])
                                    fw.op("dve", lambda e, tk=tk: e.tensor_scalar(out=comb[:, tk, :], in0=ex[:], scalar1=ssum[:, 0:1], scalar2=None,
                                                                                   op0=ALU.mult), reads=["ex", "ssum"], writes=["comb"])
                        fw.barrier()
                        NCH = 7
                        FC = EFF // NCH
                        wgc = [fw.sb(ph, "mwg%d" % i, [128, 8, FC], BF16) for i in range(2)]
                        wuc = [fw.sb(ph, "mwu%d" % i, [128, 8, FC], BF16) for i in range(2)]
                        wdc = [fw.sb(ph, "mwd%d" % i, [128, 4, D], BF16) for i in range(2)]
                        cbc = [fw.sb(ph, "cbc%d" % i, [128, NTo], F32) for i in range(2)]
                        dg = [fw.sb(ph, "dg%d" % i, [128, 128], F32) for i in range(2)]
                        dgh = [fw.sb(ph, "dgh%d" % i, [128, 128], BF16) for i in range(2)]
                        dgl = [fw.sb(ph, "dgl%d" % i, [128, 128], BF16) for i in range(2)]
                        actM = [fw.sb(ph, "actM%d" % i, [128, 4 * G], BF16) for i in range(2)]
                        sil = [fw.sb(ph, "silM%d" % i, [128, G], F32) for i in range(2)]
                        tm = [fw.sb(ph, "tmM%d" % i, [128, G], F32) for i in range(2)]
                        g2o = l * 48 + 40
                        it = 0
                        def build_cbc(ex_):
                            cb = cbc[ex_ % 2]
                            cbk = "cbc%d" % (ex_ % 2)
                            for tk in range(NTk):
                                d_ = dg[tk % 2]
                                fw.op("dve", lambda e, tk=tk, d_=d_, ex_=ex_: e.tensor_scalar(
                                    out=d_[:], in0=ident[:], scalar1=comb[:, tk, ex_:ex_ + 1], scalar2=None, op0=ALU.mult),
                                    reads=["ident", "comb"], writes=["dg%d" % (tk % 2)])
                                bank = 6 + (tk % 2)
                                dh_, dl_ = dgh[tk % 2], dgl[tk % 2]
                                fw.op("dve", lambda e, d_=d_, dh_=dh_: e.tensor_copy(out=dh_[:], in_=d_[:]), reads=["dg%d" % (tk % 2)], writes=["dgh%d" % (tk % 2)])
                                fw.op("dve", lambda e, d_=d_, dh_=dh_: e.tensor_tensor(out=d_[:], in0=d_[:], in1=dh_[:], op=ALU.subtract),
                                      reads=["dg%d" % (tk % 2), "dgh%d" % (tk % 2)], writes=["dg%d" % (tk % 2)])
                                fw.op("dve", lambda e, d_=d_, dl_=dl_: e.tensor_copy(out=dl_[:], in_=d_[:]), reads=["dg%d" % (tk % 2)], writes=["dgl%d" % (tk % 2)])
                                fw.op("pe", lambda e, dh_=dh_, bank=bank: e.matmul(pb[bank][:, 0:128], lhsT=ones_bf[:], rhs=dh_[:], start=True, stop=False),
                                      reads=["ones_bf", "dgh%d" % (tk % 2)], writes=[PB[bank]], inc=False)
                                fw.op("pe", lambda e, dl_=dl_, bank=bank: e.matmul(pb[bank][:, 0:128], lhsT=ones_bf[:], rhs=dl_[:], start=False, stop=True),
                                      reads=["ones_bf", "dgl%d" % (tk % 2)], writes=[PB[bank]])
                                fw.op("act", lambda e, tk=tk, cb=cb, bank=bank: e.activation(out=cb[:, tk * 128:(tk + 1) * 128], in_=pb[bank][:, 0:128],
                                                                                             func=AF.Copy), reads=[PB[bank]], writes=[cbk])

                        items = [(ex_, ch, g) for ex_ in range(NE) for ch in range(NCH) for g in range(NGo)]
                        loaded = set()

                        def ensure_loaded(ex_, ch):
                            n_ = ex_ * NCH + ch
                            if n_ in loaded:
                                return
                            loaded.add(n_)
                            wb = n_ % 2
                            load_w(wgc[wb], m_wg[0, ex_, :, ch * FC:(ch + 1) * FC], "mwg%d" % wb)
                            load_w(wuc[wb], m_wu[0, ex_, :, ch * FC:(ch + 1) * FC], "mwu%d" % wb)
                            load_w(wdc[wb], m_wd[0, ex_, ch * FC:(ch + 1) * FC, :], "mwd%d" % wb, kchunks=4)

                        def gateup_f(item, f):
                            ex_, ch, g = item
                            wb = (ex_ * NCH + ch) % 2
                            kg_, ku_ = "mwg%d" % wb, "mwu%d" % wb
                            am = actM[g % 2]
                            amk = "actM%d" % (g % 2)
                            bg, bu = (f % 2) * 2, 1 + (f % 2) * 2
                            for c in range(8):
                                fw.op("pe", lambda e: e.matmul(
                                    pb[bg][:], lhsT=wgc[wb][:, c, f * 128:(f + 1) * 128], rhs=hM[:, c, g * G:(g + 1) * G],
                                    start=(c == 0), stop=(c == 7)), reads=[kg_, "hM"], writes=[PB[bg]], inc=(c == 7))
                            for c in range(8):
                                fw.op("pe", lambda e: e.matmul(
                                    pb[bu][:], lhsT=wuc[wb][:, c, f * 128:(f + 1) * 128], rhs=hM[:, c, g * G:(g + 1) * G],
                                    start=(c == 0), stop=(c == 7)), reads=[ku_, "hM"], writes=[PB[bu]], inc=(c == 7))
                            fw.op("act", lambda e: e.activation(out=sil[f % 2][:], in_=pb[bg][:], func=AF.Silu),
                                  reads=[PB[bg]], writes=["silM%d" % (f % 2)])
                            fw.op("dve", lambda e: e.tensor_tensor(out=am[:, f * G:(f + 1) * G], in0=pb[bu][:],
                                                                   in1=sil[f % 2][:], op=ALU.mult),
                                  reads=[PB[bu], "silM%d" % (f % 2)], writes=[amk + "_%d" % f])

                        def down_c(item, c):
                            ex_, ch, g = item
                            wb = (ex_ * NCH + ch) % 2
                            kd_ = "mwd%d" % wb
                            am = actM[g % 2]
                            amk = "actM%d" % (g % 2)
                            cb = cbc[ex_ % 2]
                            cbk = "cbc%d" % (ex_ % 2)
                            bank = 4 + (c % 4)
                            for f in range(4):
                                fw.op("pe", lambda e: e.matmul(
                                    pb[bank][:], lhsT=wdc[wb][:, f, c * 128:(c + 1) * 128], rhs=am[:, f * G:(f + 1) * G],
                                    start=(f == 0), stop=(f == 3)), reads=[kd_, amk + "_%d" % f], writes=[PB[bank]], inc=(f == 3))
                            fw.op("dve", lambda e: e.tensor_tensor(
                                out=tm[c % 2][:], in0=pb[bank][:], in1=cb[:, g * G:(g + 1) * G], op=ALU.mult),
                                reads=[PB[bank], cbk], writes=["tmM%d" % (c % 2)])
                            fw.op("dve", lambda e: e.scalar_tensor_tensor(
                                out=acc[:, c, g * G:(g + 1) * G], in0=tm[c % 2][:], scalar=modT[:, g2o + c:g2o + c + 1],
                                in1=acc[:, c, g * G:(g + 1) * G], op0=ALU.mult, op1=ALU.add),
                                reads=["tmM%d" % (c % 2), "modT", "acc_g%d" % g], writes=["acc_g%d" % g])

                        build_cbc(0)
                        ensure_loaded(items[0][0], items[0][1])
                        for f in range(4):
                            gateup_f(items[0], f)
                        for k_, item in enumerate(items):
                            nxt = items[k_ + 1] if k_ + 1 < len(items) else None
                            if nxt is not None:
                                ensure_loaded(nxt[0], nxt[1])
                            if item[1] == 3 and item[2] == 0 and item[0] + 1 < NE:
                                build_cbc(item[0] + 1)
                            for i in range(4):
                                if nxt is not None:
                                    gateup_f(nxt, i)
                                down_c(item, 2 * i)
                                down_c(item, 2 * i + 1)
                        for g in range(NGo):
                            fw.dma(XT[:, :, g * G:(g + 1) * G].rearrange("c p t -> p c t"), acc[:, :, g * G:(g + 1) * G],
                                   reads=["acc_g%d" % g], writes=["XT_g%d" % g])
                    fw.barrier()

            NGf = out_rows // G
            with ExitStack() as ph:
                xF = [fw.sb(ph, "xF%d" % i, [128, 8 * G], F32) for i in range(2)]
                sqF = fw.sb(ph, "sqF", [128, 8 * G], BF16)
                rsF = fw.sb(ph, "rsF", [128, G], F32)
                yF = fw.sb(ph, "yF", [128, 8 * G], F32)
                oF = [fw.sb(ph, "oF%d" % i, [128, D], F32) for i in range(2)]
                oi = 0
                for g in range(NGf):
                    b = g % 2
                    xk = "xF%d" % b
                    fw.dma(xF[b][:].rearrange("p (c t) -> p c t", c=8), XT[:, :, g * G:(g + 1) * G].rearrange("c p t -> p c t"),
                           reads=["XT_g%d" % g], writes=[xk])
                    fw.op("act", lambda e, b=b: e.activation(out=sqF[:], in_=xF[b][:], func=AF.Square), reads=[xk], writes=["sqF"])
                    for c in range(8):
                        fw.op("pe", lambda e, c=c: e.matmul(pb[0][:], lhsT=ones_bf[:], rhs=sqF[:, c * G:(c + 1) * G], start=(c == 0), stop=(c == 7)),
                              reads=["ones_bf", "sqF"], writes=[PB[0]], inc=(c == 7))
                    fw.op("act", lambda e: e.activation(out=rsF[:], in_=pb[0][:], func=AF.Ln, scale=1.0 / D, bias=epsc[:]),
                          reads=[PB[0], "epsc"], writes=["rsF"])
                    fw.op("act", lambda e: e.activation(out=rsF[:], in_=rsF[:], func=AF.Exp, scale=-0.5), reads=["rsF"], writes=["rsF"])
                    for c in range(8):
                        fw.op("dve", lambda e, c=c, b=b: e.scalar_tensor_tensor(
                            out=yF[:, c * G:(c + 1) * G], in0=xF[b][:, c * G:(c + 1) * G], scalar=vecs[:, V_FG + c:V_FG + c + 1],
                            in1=rsF[:], op0=ALU.mult, op1=ALU.mult), reads=[xk, "vecs", "rsF"], writes=["yF%d" % c])
                    for j in range(4):
                        ob = oi % 2
                        oi += 1
                        for half in range(2):
                            bank = 1 + half + 2 * ob
                            for cc in range(4):
                                c = half * 4 + cc
                                fw.op("pe", lambda e, c=c, cc=cc, j=j, bank=bank: e.transpose(
                                    pb[bank][:, cc * 128:(cc + 1) * 128], yF[:, c * G + j * 128:c * G + (j + 1) * 128], ident[:]),
                                    reads=["yF%d" % c, "ident"], writes=[PB[bank]], inc=(cc == 3))
                            if half == 0:
                                fw.op("act", lambda e, ob=ob, bank=bank: e.activation(out=oF[ob][:, 0:512], in_=pb[bank][:], func=AF.Copy),
                                      reads=[PB[bank]], writes=["oF%d" % ob])
                            else:
                                fw.op("dve", lambda e, ob=ob, bank=bank: e.tensor_copy(out=oF[ob][:, 512:1024], in_=pb[bank][:]),
                                      reads=[PB[bank]], writes=["oF%d" % ob])
                        r0 = g * G + j * 128
                        fw.dma(out_ap[r0:r0 + 128, :], oF[ob][:], reads=["oF%d" % ob], writes=["out_%d" % (r0 // 128)])
                fw.wait_all("sp", ["out_%d" % i for i in range(out_rows // 128)])
        except _Stop:
            fw.barrier()
            for k_ in open_kv:
                k_.__exit__(None, None, None)
        fw.finish()
    return nc


def _pmaj(v, n):
    return np.ascontiguousarray(np.asarray(v, np.float32).reshape(n, 128).T)


def _rope_tables():
    pos = np.arange(T)
    row = (pos // 64).astype(np.float32)
    col = (pos % 64).astype(np.float32)
    n_freq = 32
    inv_freq = np.exp(-math.log(10000.0) * (2.0 * np.arange(n_freq, dtype=np.float32) / 64.0)).astype(np.float32)
    cosT = np.zeros((128, T), np.float32)
    sinT = np.zeros((128, T), np.float32)
    for d in range(128):
        axis, half, f = d // 64, (d % 64) // 32, d % 32
        ang = (row if axis == 0 else col) * inv_freq[f]
        cosT[d] = np.cos(ang)
        sinT[d] = np.sin(ang) * (-1.0 if half == 0 else 1.0)
    return cosT, sinT


_NC_CACHE = {}


def _get_nc(n_layers, debug=False):
    key = (n_layers, debug)
    if key not in _NC_CACHE:
        _NC_CACHE[key] = build_nc(n_layers, debug)
    return _NC_CACHE[key]


def make_in_maps(inputs, n_layers=2):
    f = lambda k: np.asarray(inputs[k], np.float32)
    x, c = f("x"), f("c")
    cosT, sinT = _rope_tables()
    partner = np.array([d + 32 if (d % 64) < 32 else d - 32 for d in range(128)])
    in_maps = []
    shared = {}
    for k in ["w_mod", "w_in", "w_o_attn", "w_o_lru", "w_out", "ffn_w_gate", "ffn_w_up", "ffn_w_down"]:
        shared[k] = np.ascontiguousarray(f(k))
    if n_layers > 1:
        for k in ["router_w", "moe_w_gate", "moe_w_up", "moe_w_down"]:
            shared[k] = np.ascontiguousarray(f(k))
    lwa, lwx = f("lru_w_a"), f("lru_w_x")
    for core in range(8):
        bi, odd = core // 2, core % 2
        vecs = np.zeros((128, NV), np.float32)
        for l in range(2):
            vecs[:, V_BMOD + l * 48:V_BMOD + (l + 1) * 48] = _pmaj(f("b_mod")[l], 48)
            qg, kg = f("q_norm_gain")[l], f("k_norm_gain")[l]
            vecs[:, V_QG + l * 4 + 0] = qg
            vecs[:, V_QG + l * 4 + 1] = qg[partner]
            vecs[:, V_QG + l * 4 + 2] = kg
            vecs[:, V_QG + l * 4 + 3] = kg[partner]
            cw = f("conv_w")[l]
            taps = np.zeros((5, 1024), np.float32)
            if not odd:
                taps[0:4] = cw
            else:
                taps[1:5] = cw[::-1]
            for j in range(5):
                vecs[:, V_CONVW + l * 40 + j * 8:V_CONVW + l * 40 + (j + 1) * 8] = _pmaj(taps[j], 8)
            vecs[:, V_CONVB + l * 8:V_CONVB + (l + 1) * 8] = _pmaj(f("conv_b")[l], 8)
            for d in range(2):
                sd = d if not odd else 1 - d
                o = V_LRU + l * 48 + d * 24
                vecs[:, o:o + 8] = _pmaj(f("lru_b_a")[l, sd], 8)
                vecs[:, o + 8:o + 16] = _pmaj(f("lru_b_x")[l, sd], 8)
                vecs[:, o + 16:o + 24] = _pmaj(f("lru_lambda")[l, sd], 8)
        vecs[:, V_FG:V_FG + 8] = _pmaj(f("final_gain"), 8)
        vecs[:, V_RB:V_RB + 8] = np.broadcast_to(f("router_b")[0][None, :], (128, 8))
        vecs[:, V_C:V_C + 8] = _pmaj(c[bi], 8)
        xs = x[bi]
        m = dict(shared)
        if odd:
            m["x"] = np.ascontiguousarray(xs[::-1])
            m["cosT"] = np.ascontiguousarray(cosT[:, ::-1])
            m["sinT"] = np.ascontiguousarray(sinT[:, ::-1])
            m["lru_wa"] = np.ascontiguousarray(lwa[:, ::-1])
            m["lru_wx"] = np.ascontiguousarray(lwx[:, ::-1])
        else:
            m["x"] = np.ascontiguousarray(xs)
            m["cosT"] = cosT
            m["sinT"] = sinT
            m["lru_wa"] = np.ascontiguousarray(lwa)
            m["lru_wx"] = np.ascontiguousarray(lwx)
        m["vecs"] = vecs
        in_maps.append(m)
    return in_maps


def kernel(**inputs):
    nc = _get_nc(2)
    in_maps = make_in_maps(inputs, 2)
    res = run_bass_kernel_spmd(nc, in_maps, core_ids=list(range(8)))
    out = np.zeros((4, T, D), np.float32)
    for core in range(8):
        bi, odd = core // 2, core % 2
        o = np.asarray(res.results[core]["out"], np.float32)
        if odd:
            out[bi, T // 2:] = o[::-1]
        else:
            out[bi, :T // 2] = o
    return out
```
